# Optimizing a Trainium2 kernel written in Bass

```python
import jax, jax.numpy as jnp
from jax import lax
import numpy as np

D_MODEL = 1024
BATCH = 2
SEQ = 8192
DEPTH = 1

CTX_LEN = 256
GRID_W = 64
D_FOURIER = D_MODEL // 2
FOURIER_GROUPS = 4
FOURIER_GROUP_DIM = D_FOURIER // FOURIER_GROUPS
D_REC = D_MODEL - D_FOURIER
REC_HEADS = 4
REC_HEAD_DIM = D_REC // REC_HEADS
CHUNK = 64
SUB_CHUNK = 16
N_EXPERTS = 16
EC_CAPACITY_FACTOR = 2
D_EXPERT = 2816
NORM_EPS = 1e-6
D_IN = D_FOURIER + 5 * D_REC
N_ADA = 6

kernel_name = "hybrid_fnet_hgrn2_ec_moe_dit"


def rms_norm(x, gain):
    xf = x.astype(jnp.float32)
    y = xf * lax.rsqrt(jnp.mean(xf * xf, axis=-1, keepdims=True) + NORM_EPS)
    return (y * gain.astype(jnp.float32)).astype(x.dtype)


def adaln(cond, w, b):
    mod = (jax.nn.silu(cond) @ w + b)[:, None, :]
    return jnp.split(mod, N_ADA, axis=-1)


def modulate(h, shift, scale):
    return h * (1 + scale) + shift


def fourier_mix(u, w_f):
    b_, l_, _ = u.shape
    z = u.astype(jnp.float32).reshape(b_, l_, FOURIER_GROUPS, FOURIER_GROUP_DIM)
    z = jnp.fft.fft2(z, axes=(1, 3), norm="ortho").real.astype(u.dtype)
    y = jnp.einsum('blgc,gcd->blgd', z, w_f)
    return y.reshape(b_, l_, D_FOURIER)


def _to_chunks(t):
    b_, l_, h_, d_ = t.shape
    return t.reshape(b_, l_ // CHUNK, CHUNK, h_, d_).transpose(1, 0, 3, 2, 4)


def _from_chunks(t):
    n_, b_, h_, c_, d_ = t.shape
    return t.transpose(1, 0, 3, 2, 4).reshape(b_, n_ * c_, h_, d_)


def _chunk_step(S, inp):
    qc, kc, vc, lc = inp
    b_, h_, c_, d_ = qc.shape
    ns = c_ // SUB_CHUNK
    bcum = jnp.cumsum(lc, axis=2)
    o_inter = jnp.einsum('bhcd,bhde->bhce', qc * jnp.exp(bcum), S)
    bs = bcum.reshape(b_, h_, ns, SUB_CHUNK, d_)
    qs = qc.reshape(b_, h_, ns, SUB_CHUNK, d_)
    ks = kc.reshape(b_, h_, ns, SUB_CHUNK, d_)
    vs = vc.reshape(b_, h_, ns, SUB_CHUNK, -1)
    b_ref = jnp.concatenate([jnp.zeros_like(bs[:, :, :1, 0]), bs[:, :, :-1, -1]], axis=2)
    q_hat = qs * jnp.exp(bs - b_ref[:, :, :, None, :])
    prev = jnp.arange(c_)[None, :] < (jnp.arange(ns) * SUB_CHUNK)[:, None]
    k_hat = kc[:, :, None] * jnp.exp(jnp.where(prev[:, :, None],
                                               b_ref[:, :, :, None, :] - bcum[:, :, None], -jnp.inf))
    a_off = jnp.einsum('bhjtd,bhjsd->bhjts', q_hat, k_hat)
    tri = jnp.tril(jnp.ones((SUB_CHUNK, SUB_CHUNK), dtype=bool))
    decay = jnp.exp(jnp.where(tri[:, :, None],
                              bs[:, :, :, :, None, :] - bs[:, :, :, None, :, :], -jnp.inf))
    a_diag = jnp.einsum('bhjtd,bhjtsd,bhjsd->bhjts', qs, decay, ks)
    o_intra = (jnp.einsum('bhjts,bhse->bhjte', a_off, vc)
               + jnp.einsum('bhjts,bhjse->bhjte', a_diag, vs))
    o = o_inter + o_intra.reshape(b_, h_, c_, -1)
    b_last = bcum[:, :, -1]
    S_new = (jnp.exp(b_last)[..., None] * S
             + jnp.einsum('bhcd,bhce->bhde', kc * jnp.exp(b_last[:, :, None] - bcum), vc))
    return S_new, o


def scan_dir(q, k, v, logf, S0, reverse):
    if reverse:
        q, k, v, logf = (jnp.flip(t, axis=1) for t in (q, k, v, logf))
    S_fin, o = lax.scan(_chunk_step, S0, (_to_chunks(q), _to_chunks(k), _to_chunks(v), _to_chunks(logf)))
    o = _from_chunks(o)
    if reverse:
        o = jnp.flip(o, axis=1)
    return o, S_fin


def split_proj(p, lb):
    b_, l_, _ = p.shape
    u, q, v, f_f, f_b, g = jnp.split(p, [D_FOURIER + i * D_REC for i in range(5)], axis=-1)
    heads = lambda t: t.astype(jnp.float32).reshape(b_, l_, REC_HEADS, REC_HEAD_DIM)

    def gates(fp, lbd):
        lbd = lbd.reshape(REC_HEADS, REC_HEAD_DIM)
        fg = lbd + (1 - lbd) * jax.nn.sigmoid(heads(fp))
        return jnp.log(fg), 1 - fg

    return u, jax.nn.silu(heads(q)), heads(v), gates(f_f, lb[0]), gates(f_b, lb[1]), g


def rec_mixer(q, v, gf, gb, g, S0_f, S0_b, gain):
    o_f, S_f = scan_dir(q, gf[1], v, gf[0], S0_f, False)
    o_b, S_b = scan_dir(q, gb[1], v, gb[0], S0_b, True)
    o = o_f + o_b
    o = o * lax.rsqrt(jnp.mean(o * o, axis=-1, keepdims=True) + NORM_EPS)
    o = o * gain.astype(jnp.float32).reshape(REC_HEADS, REC_HEAD_DIM)
    b_, l_ = o.shape[:2]
    y = o.reshape(b_, l_, D_REC).astype(g.dtype) * jax.nn.silu(g)
    return y, S_f, S_b


def ec_moe(h, w_r, w_g, w_u, w_d):
    b_, l_, d_ = h.shape
    cap = EC_CAPACITY_FACTOR * l_ // N_EXPERTS
    affinity = jax.nn.softmax((h @ w_r).astype(jnp.float32), axis=-1)
    gate, idx = lax.top_k(jnp.swapaxes(affinity, 1, 2), cap)
    xs = jax.vmap(lambda hb, ib: hb[ib])(h, idx)
    hid = jax.nn.silu(jnp.einsum('becd,edf->becf', xs, w_g)) * jnp.einsum('becd,edf->becf', xs, w_u)
    ye = jnp.einsum('becf,efd->becd', hid, w_d) * gate[..., None].astype(h.dtype)
    flat = (idx + jnp.arange(b_)[:, None, None] * l_).reshape(-1)
    out = jax.ops.segment_sum(ye.reshape(-1, d_), flat, num_segments=b_ * l_)
    return out.reshape(b_, l_, d_)


def setup_inputs(seed: int = 0) -> dict:
    key = jax.random.key(seed)
    ks = jax.random.split(key, 20)
    nrm = lambda k, shape, s: jax.random.normal(k, shape, jnp.float32) * s
    D = D_MODEL
    return {
        "x": nrm(ks[0], (BATCH, SEQ, D), 1.0),
        "c": nrm(ks[1], (BATCH, D), 1.0),
        "ctx": nrm(ks[2], (BATCH, CTX_LEN, D), 1.0),
        "c_ctx": nrm(ks[3], (D,), 1.0),
        "w_ada": nrm(ks[4], (DEPTH, D, N_ADA * D), 0.5 * D ** -0.5),
        "b_ada": nrm(ks[5], (DEPTH, N_ADA * D), 0.01),
        "g_mix": 1.0 + nrm(ks[6], (DEPTH, D), 0.02),
        "w_in": nrm(ks[7], (DEPTH, D, D_IN), D ** -0.5),
        "w_fourier": nrm(ks[8], (DEPTH, FOURIER_GROUPS, FOURIER_GROUP_DIM, FOURIER_GROUP_DIM), FOURIER_GROUP_DIM ** -0.5),
        "lb_logits": nrm(ks[9], (DEPTH + 1, 2, D_REC), 0.5),
        "g_rec": 1.0 + nrm(ks[10], (DEPTH, D_REC), 0.02),
        "w_out": nrm(ks[11], (DEPTH, D, D), D ** -0.5),
        "g_ffn": 1.0 + nrm(ks[12], (DEPTH, D), 0.02),
        "w_router": nrm(ks[13], (DEPTH, D, N_EXPERTS), D ** -0.5),
        "w_exp_gate": nrm(ks[14], (DEPTH, N_EXPERTS, D, D_EXPERT), D ** -0.5),
        "w_exp_up": nrm(ks[15], (DEPTH, N_EXPERTS, D, D_EXPERT), D ** -0.5),
        "w_exp_down": nrm(ks[16], (DEPTH, N_EXPERTS, D_EXPERT, D), D_EXPERT ** -0.5),
        "g_final": 1.0 + nrm(ks[17], (D,), 0.02),
    }


def reference(x, c, ctx, c_ctx, w_ada, b_ada, g_mix, w_in, w_fourier, lb_logits, g_rec, w_out,
              g_ffn, w_router, w_exp_gate, w_exp_up, w_exp_down, g_final):
    b_ = x.shape[0]
    lb_all = jnp.cumsum(jax.nn.softmax(lb_logits.astype(jnp.float32), axis=0), axis=0)
    for layer in range(DEPTH):
        ctx_out_needed = layer < DEPTH - 1
        sh1, sc1, gt1, sh2, sc2, gt2 = adaln(c, w_ada[layer], b_ada[layer])
        sh1c, sc1c, gt1c, sh2c, sc2c, gt2c = adaln(c_ctx[None], w_ada[layer], b_ada[layer])
        lb = lb_all[layer]

        h_ctx = modulate(rms_norm(ctx, g_mix[layer]), sh1c, sc1c)
        h_lat = modulate(rms_norm(x, g_mix[layer]), sh1, sc1)
        u_c, q_c, v_c, gf_c, gb_c, g_c = split_proj(h_ctx @ w_in[layer], lb)
        u_l, q_l, v_l, gf_l, gb_l, g_l = split_proj(h_lat @ w_in[layer], lb)

        S_zero = jnp.zeros((b_, REC_HEADS, REC_HEAD_DIM, REC_HEAD_DIM), jnp.float32)
        rec_c, S_cf, S_cb = rec_mixer(q_c, v_c, gf_c, gb_c, g_c, S_zero, S_zero, g_rec[layer])
        rec_l, _, _ = rec_mixer(q_l, v_l, gf_l, gb_l, g_l, S_cf, S_cb, g_rec[layer])

        mix_l = jnp.concatenate([fourier_mix(u_l, w_fourier[layer]), rec_l], axis=-1) @ w_out[layer]
        x = x + gt1 * mix_l
        if ctx_out_needed:
            mix_c = jnp.concatenate([fourier_mix(u_c, w_fourier[layer]), rec_c], axis=-1) @ w_out[layer]
            ctx = ctx + gt1c * mix_c

        x = x + gt2 * ec_moe(modulate(rms_norm(x, g_ffn[layer]), sh2, sc2), w_router[layer],
                             w_exp_gate[layer], w_exp_up[layer], w_exp_down[layer])
        if ctx_out_needed:
            ctx = ctx + gt2c * ec_moe(modulate(rms_norm(ctx, g_ffn[layer]), sh2c, sc2c), w_router[layer],
                                      w_exp_gate[layer], w_exp_up[layer], w_exp_down[layer])
    return rms_norm(x, g_final)
```

```python
import os
import numpy as np
from contextlib import ExitStack
import concourse.bass as bass
import concourse.mybir as mybir
from concourse.bass_utils import run_bass_kernel_spmd

F32 = mybir.dt.float32
I32 = mybir.dt.int32
BF16 = mybir.dt.bfloat16
AF = mybir.ActivationFunctionType
ALU = mybir.AluOpType
AX = mybir.AxisListType

D = 1024
L = 8192
LC = 256
TT = LC + L
NE = 16
CAP = 1024
DE = 2816
NFC = DE // 128
EPS = 1e-6
GROUPS = [[0, 1, 2, 3], [4, 5, 6, 7]]


class Sched:
    CE = ('pe', 'dve', 'act', 'pool')

    def __init__(self, nc, stack, ndma=8):
        self.nc = nc
        self.e = dict(pe=nc.tensor, dve=nc.vector, act=nc.scalar, pool=nc.gpsimd, sp=nc.sync)
        self.csem = {k: stack.enter_context(nc.semaphore('cs_' + k)) for k in self.CE}
        self.ccnt = {k: 0 for k in self.CE}
        self.dsem = {q: [stack.enter_context(nc.semaphore('ds_%s%d' % (q, i))) for i in range(ndma)]
                     for q in ('sp', 'pool', 'act')}
        self.dcnt = {q: [0] * ndma for q in self.dsem}
        self.dnext = {q: 0 for q in self.dsem}
        self.ndma = ndma
        self.seen = {k: {} for k in self.e}
        self.lastw = {}
        self.readers = {}
        self.ccsem = stack.enter_context(nc.semaphore('ccsem'))
        self.cccnt = 0
        self.n = 0

    def _wait(self, eng, ev):
        sem, val, src = ev
        d = self.seen[eng]
        if d.get(sem, 0) >= val:
            return
        self.e[eng].wait_ge(sem, val)
        d[sem] = val

    def _deps(self, eng, reads, writes, skip_same_waw):
        deps = []
        for k in reads:
            w = self.lastw.get(k)
            if isinstance(w, list):
                deps.extend(w)
            elif w is not None:
                deps.append(w)
            if isinstance(k, str) and len(k) == 2 and k[0] == 'P' and k[1].isdigit():
                for sem, (val, src) in self.readers.get(k, {}).items():
                    if src != eng:
                        deps.append((sem, val, src))
        for k in writes:
            w = self.lastw.get(k)
            if isinstance(w, list):
                deps.extend(w)
            elif w is not None and not (skip_same_waw and w[2] == eng):
                deps.append(w)
            for sem, (val, src) in self.readers.get(k, {}).items():
                deps.append((sem, val, src))
        for ev in deps:
            self._wait(eng, ev)

    def _record(self, ev, reads, writes):
        sem, val, src = ev
        for k in reads:
            self.readers.setdefault(k, {})[sem] = (val, src)
        for k in writes:
            self.lastw[k] = ev
            self.readers[k] = {}

    def op(self, eng, fn, reads=(), writes=(), acc=False):
        self._deps(eng, reads, writes, acc)
        ins = fn(self.e[eng])
        self.ccnt[eng] += 1
        ins.then_inc(self.csem[eng], 1)
        ev = (self.csem[eng], self.ccnt[eng], eng)
        self._record(ev, reads, writes)
        self.n += 1
        return ev

    def dma(self, q, fn, reads=(), writes=()):
        i = self.dnext[q]
        self.dnext[q] = (i + 1) % self.ndma
        sem = self.dsem[q][i]
        if self.dcnt[q][i] > 0:
            self._wait(q, (sem, self.dcnt[q][i], 'dma'))
        self._deps(q, reads, writes, False)
        ins = fn(self.e[q])
        ins.then_inc(sem, 16)
        self.dcnt[q][i] += 16
        ev = (sem, self.dcnt[q][i], 'dma')
        self._record(ev, reads, writes)
        self.n += 1
        return ev

    def fence(self, key, q):
        self.lastw[key] = [(self.dsem[q][i], self.dcnt[q][i], 'dma') for i in range(self.ndma) if self.dcnt[q][i] > 0]
        self.readers[key] = {}

    def collective(self, kind, op, ins, outs, reads=(), writes=()):
        if self.cccnt > 0:
            self._wait('pool', (self.ccsem, self.cccnt, 'cc'))
        self._deps('pool', reads, writes, False)
        ins_ = self.nc.gpsimd.collective_compute(kind, op, replica_groups=GROUPS, ins=ins, outs=outs)
        ins_.then_inc(self.ccsem, 1)
        self.cccnt += 1
        ev = (self.ccsem, self.cccnt, 'cc')
        self._record(ev, reads, writes)
        return ev

    def barrier(self, cc=False):
        evs = [(self.csem[k], self.ccnt[k], k) for k in self.CE if self.ccnt[k] > 0]
        for q in self.dsem:
            for i in range(self.ndma):
                if self.dcnt[q][i] > 0:
                    evs.append((self.dsem[q][i], self.dcnt[q][i], 'dma'))
        if self.cccnt and cc:
            evs.append((self.ccsem, self.cccnt, 'cc'))
        for eng in self.e:
            for ev in evs:
                self._wait(eng, ev)
        keepw = {k: v for k, v in self.lastw.items() if not isinstance(v, list) and v[2] == 'cc'}
        keepr = {k: {sm: vv for sm, vv in d.items() if vv[1] == 'cc'} for k, d in self.readers.items()}
        self.lastw = keepw if not cc else {}
        self.readers = {k: d for k, d in keepr.items() if d} if not cc else {}


def _const_tables():
    c = {}
    i128 = np.arange(128)
    i64 = np.arange(64)
    c['ident'] = np.eye(128)
    a = 2 * np.pi * np.outer(i128, i128) / 128.0
    c['C128'] = np.cos(a)
    c['S128'] = np.sin(a)
    c['nS128'] = -np.sin(a)
    c['CCs'] = np.cos(a) / 1024.0
    c['SCs'] = np.sin(a) / 1024.0
    tw = 2 * np.pi * np.outer(i128, i64) / 8192.0
    c['Tc'] = np.cos(tw)
    c['Ts'] = np.sin(tw)
    a64 = 2 * np.pi * np.outer(i64, i64) / 64.0
    c['C64'] = np.cos(a64)
    c['nS64'] = -np.sin(a64)
    s = i64[:, None]
    t = i64[None, :]
    c['TriF'] = (s <= t) * 1.0
    c['TriB'] = (s >= t) * 1.0
    c['SLoF'] = (s > t) * 1.0
    c['SLoB'] = (s < t) * 1.0
    c['SU64'] = (s < t) * 1.0
    c['ones16'] = np.ones((16, 16))
    p = i128[:, None]
    q = i128[None, :]
    c['L128'] = (p < q) * 1.0
    c['avg128'] = np.ones((128, 128)) / 128.0
    c['one128'] = np.ones((128, 128))
    eq = (i128 // 2) % 16
    c['G'] = (eq[:, None] == eq[None, :]) * 1.0
    lay = {}
    off = 0
    for k, v in c.items():
        lay[k] = (off, v.shape[0], v.shape[1])
        off += v.shape[1]
    pack = np.zeros((128, off), np.float32)
    for k, v in c.items():
        o, r, w = lay[k]
        pack[:r, o:o + w] = v.astype(np.float32)
    return pack, lay


CPACK, CLAY = _const_tables()

PLAY = {}


def _play():
    off = 0
    for name, w in [('ccT', 16), ('bada_fm', 16), ('gmix_fm', 8), ('grec_fm', 1), ('lbl_fm', 4),
                    ('lbl_row', 512), ('wr', 128), ('gffn_row', 1024), ('gfin_row', 1024)]:
        PLAY[name] = (off, w)
        off += w
    return off


PW = _play()


def build(dbg=None):
    nc = bass.Bass("TRN2", target_bir_lowering=False)

    def din(name, shape, dt=F32):
        return nc.dram_tensor(name, list(shape), dt, kind="ExternalInput").ap()

    x_b = din("x_b", [L, D])
    x_own = din("x_own", [2048, D])
    ctx_b = din("ctx_b", [LC, D])
    w_ada = din("w_ada", [D, 6 * D])
    w_in = din("w_in", [D, 768])
    w_f = din("w_f", [128, 128])
    w_out = din("w_out", [D, D])
    weg = din("weg", [4, D, DE])
    weu = din("weu", [4, D, DE])
    wed = din("wed", [4, DE, D])
    cpack_d = din("cpack", list(CPACK.shape))
    ppack_d = din("ppack", [128, PW])
    bada_rows_d = din("bada_rows", [128, 4096])
    idx_mix_d = din("idx_mix", [128, 8], I32)
    idx_mk_d = din("idx_mk", [64, 4], I32)
    out_d = nc.dram_tensor("out", [2048, D], F32, kind="ExternalOutput").ap()

    def dscr(name, shape, dt=F32):
        return nc.dram_tensor(name, list(shape), dt).ap()

    def ag_mix(i, keys):
        S.collective("AllGather", ALU.bypass, [mix_d[i * 256:(i + 1) * 256, :]], [mixall_d[i * 1024:(i + 1) * 1024, :]],
                     reads=keys, writes=['mixall'])

    uT_d = dscr("uT_d", [128, L])
    qT_d = dscr("qT_d", [128, L])
    sgT_d = dscr("sgT_d", [128, L])
    kT_d = [dscr("kfT_d", [128, TT]), dscr("kbT_d", [128, TT])]
    v_d = dscr("v_d", [TT, 128], BF16)
    k_d = [dscr("kf_d", [TT, 128]), dscr("kb_d", [TT, 128])]
    lf_d = [dscr("lff_d", [TT, 128]), dscr("lfb_d", [TT, 128])]
    Y_d = dscr("Y_d", [2, 128, 64, 128])
    mix_d = dscr("mix_d", [4 * 256, 2048], BF16)
    mixall_d = dscr("mixall_d", [16 * 256, 2048], BF16)
    x1_d = dscr("x1_d", [2048, D])
    h2_d = dscr("h2_d", [2048, D], BF16)
    h2all_d = dscr("h2all_d", [L, D], BF16)
    affT_d = dscr("affT_d", [16, 2048])
    affall_d = dscr("affall_d", [64, 2048])
    mk_d = dscr("mk_d", [128, 1024])
    wg_d = dscr("wg_d", [128, 1024])
    xs_d = [dscr("xs_d%d" % i, [CAP, D], BF16) for i in range(4)]
    ye_d = [dscr("ye_d%d" % i, [CAP, D]) for i in range(4)]
    op_d = dscr("op_d", [L, D], BF16)
    moe_d = dscr("moe_d", [2048, D], BF16)

    dbg_out = {}
    if dbg:
        for name, shape in dbg.items():
            if name.startswith('_'):
                continue
            dbg_out[name] = nc.dram_tensor("dbg_" + name, list(shape), F32, kind="ExternalOutput").ap()

    with ExitStack() as top:
        S = Sched(nc, top)
        sb = lambda st, name, shape, dt=F32: st.enter_context(nc.sbuf_tensor(name, list(shape), dt))
        P = [top.enter_context(nc.psum_tensor("P%d" % i, [128, 512], F32)) for i in range(8)]
        PK = ['P%d' % i for i in range(8)]
        cp = sb(top, "cpack_sb", CPACK.shape)
        pp = sb(top, "ppack_sb", [128, PW])
        S.dma('sp', lambda e: e.dma_start(out=cp[:], in_=cpack_d), writes=['cp'])
        S.dma('sp', lambda e: e.dma_start(out=pp[:], in_=ppack_d), writes=['pp'])

        def C(name, rows=None):
            o, r, w = CLAY[name]
            return cp[0:(rows or r), o:o + w]

        def PP(name, a=0, b=None):
            o, w = PLAY[name]
            return pp[:, o + a:o + (w if b is None else b)]

        ident = C('ident')
        modfm = sb(top, "modfm", [128, 16, 2])
        A1 = sb(top, "A1", [128, 8, 2])
        rows = sb(top, "rows", [128, 4096])
        lbs = sb(top, "lbs", [128, 4])
        omlrow = sb(top, "omlrow", [128, 256])
        scB = sb(top, "scB", [128, 8, 128])

        with ExitStack() as ph:
            scT = sb(ph, "scT", [128, 16])
            wa = [sb(ph, "wa%d" % i, [128, 8, 512]) for i in range(2)]
            S.op('act', lambda e: e.activation(out=scT[:], in_=PP('ccT'), func=AF.Exp, scale=-1.0), reads=['pp'], writes=['scT'])
            S.op('dve', lambda e: e.tensor_scalar(out=scT[:], in0=scT[:], scalar1=1.0, scalar2=None, op0=ALU.add), reads=['scT'], writes=['scT'])
            S.op('dve', lambda e: e.reciprocal(out=scT[:], in_=scT[:]), reads=['scT'], writes=['scT'])
            S.op('dve', lambda e: e.tensor_tensor(out=scT[:], in0=scT[:], in1=PP('ccT'), op=ALU.mult), reads=['scT', 'pp'], writes=['scT'])
            for k in range(8):
                S.op('dve', lambda e, k=k: e.tensor_scalar(out=scB[:, k, :], in0=C('one128'), scalar1=scT[:, 2 * k:2 * k + 1],
                                                            scalar2=None, op0=ALU.mult),
                     reads=['scT', 'cp'], writes=['scB'])
            w_ada_v = w_ada.rearrange("(k p) n -> p k n", p=128)
            for cb in range(4):
                wt = wa[cb % 2]
                wk = 'wa%d' % (cb % 2)
                S.dma('sp' if cb % 2 == 0 else 'act', lambda e, wt=wt, cb=cb: e.dma_start(out=wt[:], in_=w_ada_v[:, :, cb * 512:(cb + 1) * 512]),
                      writes=[wk])
                if cb < 4:
                    for bl in range(4):
                        blk = cb * 4 + bl
                        for k in range(8):
                            S.op('pe', lambda e, k=k, bl=bl, blk=blk, wt=wt: e.matmul(
                                P[0][:, blk * 2:blk * 2 + 2], lhsT=wt[:, k, bl * 128:(bl + 1) * 128],
                                rhs=scT[:, 2 * k:2 * k + 2], start=(k == 0), stop=(k == 7)),
                                reads=[wk, 'scT'], writes=['P0'], acc=True)
            for n in range(2):
                S.op('dve', lambda e, n=n: e.tensor_tensor(out=modfm[:, :, n], in0=P[0][:, 0:32].rearrange("p (b n) -> p b n", n=2)[:, :, n],
                                                           in1=PP('bada_fm'), op=ALU.add),
                     reads=['P0', 'pp'], writes=['modfm'])
            for n in range(2):
                S.op('dve', lambda e, n=n: e.scalar_tensor_tensor(out=A1[:, :, n], in0=modfm[:, 8:16, n], scalar=1.0, in1=PP('gmix_fm'),
                                                                  op0=ALU.add, op1=ALU.mult),
                     reads=['modfm', 'pp'], writes=['A1'])
            S.op('dve', lambda e: e.tensor_tensor(out=lbs[:, 2:4], in0=PP('lbl_fm', 2, 4), in1=PP('lbl_fm', 0, 2), op=ALU.subtract),
                 reads=['pp'], writes=['lbs'])
            S.op('act', lambda e: e.activation(out=lbs[:, 0:2], in_=lbs[:, 2:4], func=AF.Exp, scale=-1.0), reads=['lbs'], writes=['lbs'])
            S.op('dve', lambda e: e.tensor_scalar(out=lbs[:, 0:2], in0=lbs[:, 0:2], scalar1=1.0, scalar2=None, op0=ALU.add), reads=['lbs'], writes=['lbs'])
            S.op('dve', lambda e: e.tensor_tensor(out=omlrow[:], in0=PP('lbl_row', 256, 512), in1=PP('lbl_row', 0, 256), op=ALU.subtract),
                 reads=['pp'], writes=['omlrow'])
            S.op('act', lambda e: e.activation(out=omlrow[:], in_=omlrow[:], func=AF.Exp, scale=-1.0), reads=['omlrow'], writes=['omlrow'])
            S.op('dve', lambda e: e.tensor_scalar(out=omlrow[:], in0=omlrow[:], scalar1=1.0, scalar2=None, op0=ALU.add), reads=['omlrow'], writes=['omlrow'])
            if dbg and 'modfm' in dbg:
                S.dma('sp', lambda e: e.dma_start(out=dbg_out['modfm'], in_=modfm[:].rearrange("p a b -> p (a b)")), reads=['modfm'])
            S.barrier()

        with ExitStack() as ph:
            win = sb(ph, "win", [128, 8, 768], BF16)
            with ExitStack() as ph0:
                winf = sb(ph0, "winf", [128, 8, 768])
                S.dma('act', lambda e: e.dma_start(out=winf[:], in_=w_in.rearrange("(k p) n -> p k n", p=128)), writes=['winf'])
                for k in range(8):
                    if k % 2 == 0:
                        S.op('act', lambda e: e.copy(out=win[:, k, :], in_=winf[:, k, :]), reads=['winf'], writes=['win'])
                    else:
                        S.op('dve', lambda e: e.tensor_copy(out=win[:, k, :], in_=winf[:, k, :]), reads=['winf'], writes=['win'])
                S.barrier()
            xt = [sb(ph, "xt%d" % i, [128, D]) for i in range(4)]
            xn = [sb(ph, "xn%d" % i, [128, D], BF16) for i in range(2)]
            identb1 = sb(ph, "identb1", [128, 128], BF16)
            S.op('dve', lambda e: e.tensor_copy(out=identb1[:], in_=ident), reads=['cp'], writes=['identb1'])
            junk = sb(ph, "junk", [128, D])
            hT = [sb(ph, "hT%d" % i, [128, 8, 512], BF16) for i in range(2)]
            ofm = [sb(ph, "ofm%d" % i, [128, 512]) for i in range(4)]
            otm = [sb(ph, "otm%d" % i, [128, 128], BF16) for i in range(6)]
            ofm_i = [0]
            otm_i = [0]
            tcount = [0]

            st4 = sb(ph, "st4r", [128, 4, 4])
            otw = [sb(ph, "otw%d" % i, [128, 256]) for i in range(4)]
            otw_i = [0]

            def stageA(gi, src, ntile, n):
                hb = hT[gi % 2]
                hk = 'hT%d' % (gi % 2)
                for i in range(ntile):
                    ti = tcount[0]
                    tcount[0] += 1
                    xb, xk = xt[ti % 4], 'xt%d' % (ti % 4)
                    nb, nk = xn[ti % 2], 'xn%d' % (ti % 2)
                    sk = 'st4_%d' % i
                    S.dma('sp', lambda e: e.dma_start(out=xb[:], in_=src[i * 128:(i + 1) * 128, :]), writes=[xk])
                    S.op('act', lambda e: e.activation(out=junk[:], in_=xb[:], func=AF.Square, accum_out=st4[:, i, 0:1]),
                         reads=[xk], writes=['junk', sk])
                    S.op('act', lambda e: e.activation(out=st4[:, i, 1:2], in_=st4[:, i, 0:1], func=AF.Ln, scale=1.0 / D, bias=EPS),
                         reads=[sk], writes=[sk])
                    S.op('act', lambda e: e.activation(out=st4[:, i, 2:3], in_=st4[:, i, 1:2], func=AF.Exp, scale=-0.5),
                         reads=[sk], writes=[sk])
                    S.op('dve', lambda e: e.tensor_scalar(out=nb[:], in0=xb[:], scalar1=st4[:, i, 2:3], scalar2=None, op0=ALU.mult),
                         reads=[xk, sk], writes=[nk])
                    for half in range(2):
                        bi_ = half + 6 * (i % 2)
                        pb, pk = P[bi_][:, :].bitcast(BF16), PK[bi_]
                        for kk in range(4):
                            k = half * 4 + kk
                            S.op('pe', lambda e: e.transpose(pb[:, kk * 128:(kk + 1) * 128], nb[:, k * 128:(k + 1) * 128], identb1[:]),
                                 reads=[nk, 'identb1'], writes=[pk], acc=True)
                        for kk in range(4):
                            k = half * 4 + kk
                            if half == 0:
                                S.op('dve', lambda e: e.tensor_scalar(
                                    out=hb[:, k, i * 128:(i + 1) * 128], in0=pb[:, kk * 128:(kk + 1) * 128],
                                    scalar1=A1[:, k, n:n + 1], scalar2=modfm[:, k, n:n + 1], op0=ALU.mult, op1=ALU.add),
                                    reads=[pk, 'A1', 'modfm'], writes=[(hk, k)])
                            else:
                                S.op('act', lambda e: e.activation(
                                    out=hb[:, k, i * 128:(i + 1) * 128], in_=pb[:, kk * 128:(kk + 1) * 128], func=AF.Identity,
                                    scale=A1[:, k, n:n + 1], bias=modfm[:, k, n:n + 1]),
                                    reads=[pk, 'A1', 'modfm'], writes=[(hk, k)])
                    yield

            def stageB(gi, ntile, col0, latent):
                hb = hT[gi % 2]
                hk = 'hT%d' % (gi % 2)
                ncol = ntile * 128
                blks = [0, 1, 3, 4, 5] if latent else [3, 4]
                for bi, blk in enumerate(blks):
                    pb, pk = P[2 + bi % 2], PK[2 + bi % 2]
                    for k in range(8):
                        S.op('pe', lambda e: e.matmul(pb[:, 0:ncol], lhsT=win[:, k, blk * 128:(blk + 1) * 128],
                                                      rhs=hb[:, k, 0:ncol], start=(k == 0), stop=(k == 7)),
                             reads=['win', (hk, k)], writes=[pk], acc=True)
                    ob, ok = ofm[ofm_i[0] % 4], 'ofm%d' % (ofm_i[0] % 4)
                    ofm_i[0] += 1
                    if blk == 0:
                        S.op('act', lambda e: e.copy(out=ob[:, 0:ncol], in_=pb[:, 0:ncol]), reads=[pk], writes=[ok])
                        dst = uT_d[:, col0 - LC:col0 - LC + ncol]
                    elif blk in (1, 5):
                        S.op('act', lambda e: e.activation(out=ob[:, 0:ncol], in_=pb[:, 0:ncol], func=AF.Exp, scale=-1.0), reads=[pk], writes=[ok])
                        S.op('dve', lambda e: e.tensor_scalar(out=ob[:, 0:ncol], in0=ob[:, 0:ncol], scalar1=1.0, scalar2=None, op0=ALU.add),
                             reads=[ok], writes=[ok])
                        S.op('act', lambda e: e.activation(out=ob[:, 0:ncol], in_=ob[:, 0:ncol], func=AF.Ln), reads=[ok], writes=[ok])
                        S.op('act', lambda e: e.activation(out=ob[:, 0:ncol], in_=ob[:, 0:ncol], func=AF.Exp, scale=-1.0), reads=[ok], writes=[ok])
                        S.op('dve', lambda e: e.tensor_tensor(out=ob[:, 0:ncol], in0=ob[:, 0:ncol], in1=pb[:, 0:ncol], op=ALU.mult),
                             reads=[ok, pk], writes=[ok])
                        dst = (qT_d if blk == 1 else sgT_d)[:, col0 - LC:col0 - LC + ncol]
                    else:
                        d_ = blk - 3
                        S.op('act', lambda e: e.activation(out=ob[:, 0:ncol], in_=pb[:, 0:ncol], func=AF.Exp), reads=[pk], writes=[ok])
                        S.op('dve', lambda e: e.tensor_scalar(out=ob[:, 0:ncol], in0=ob[:, 0:ncol], scalar1=lbs[:, d_:d_ + 1], scalar2=lbs[:, d_:d_ + 1],
                                                              op0=ALU.mult, op1=ALU.add),
                             reads=[ok, 'lbs'], writes=[ok])
                        S.op('act', lambda e: e.activation(out=ob[:, 0:ncol], in_=ob[:, 0:ncol], func=AF.Ln), reads=[ok], writes=[ok])
                        S.op('act', lambda e: e.activation(out=ob[:, 0:ncol], in_=ob[:, 0:ncol], func=AF.Exp, scale=-1.0), reads=[ok], writes=[ok])
                        dst = kT_d[d_][:, col0:col0 + ncol]
                    S.dma('pool', lambda e: e.dma_start(out=dst, in_=ob[:, 0:ncol]), reads=[ok])
                    yield
                for i in range(ntile):
                    pb, pk = P[4 + i % 2], PK[4 + i % 2]
                    for k in range(8):
                        S.op('pe', lambda e: e.matmul(pb[:, 0:384], lhsT=hb[:, k, i * 128:(i + 1) * 128], rhs=win[:, k, 256:640],
                                                      start=(k == 0), stop=(k == 7)),
                             reads=['win', (hk, k)], writes=[pk], acc=True)
                    r0 = col0 + i * 128
                    ob, ok = otm[otm_i[0] % 6], 'otm%d' % (otm_i[0] % 6)
                    otm_i[0] += 1
                    S.op('act', lambda e: e.copy(out=ob[:], in_=pb[:, 0:128]), reads=[pk], writes=[ok])
                    S.dma('pool', lambda e: e.dma_start(out=v_d[r0:r0 + 128, :], in_=ob[:]), reads=[ok])
                    kb, kk_ = otw[otw_i[0] % 4], 'otw%d' % (otw_i[0] % 4)
                    lb_, lk_ = otw[(otw_i[0] + 1) % 4], 'otw%d' % ((otw_i[0] + 1) % 4)
                    otw_i[0] += 2
                    S.op('act', lambda e: e.activation(out=kb[:], in_=pb[:, 128:384], func=AF.Exp), reads=[pk], writes=[kk_])
                    S.op('dve', lambda e: e.scalar_tensor_tensor(out=kb[:], in0=kb[:], scalar=1.0, in1=omlrow[:], op0=ALU.add, op1=ALU.mult),
                         reads=[kk_, 'omlrow'], writes=[kk_])
                    S.op('act', lambda e: e.activation(out=kb[:], in_=kb[:], func=AF.Ln), reads=[kk_], writes=[kk_])
                    S.op('act', lambda e: e.activation(out=kb[:], in_=kb[:], func=AF.Exp, scale=-1.0), reads=[kk_], writes=[kk_])
                    S.op('act', lambda e: e.activation(out=lb_[:], in_=kb[:], func=AF.Ln, scale=-1.0, bias=1.0), reads=[kk_], writes=[lk_])
                    for d_ in range(2):
                        S.dma('pool', lambda e: e.dma_start(out=k_d[d_][r0:r0 + 128, :], in_=kb[:, d_ * 128:(d_ + 1) * 128]), reads=[kk_])
                        S.dma('pool', lambda e: e.dma_start(out=lf_d[d_][r0:r0 + 128, :], in_=lb_[:, d_ * 128:(d_ + 1) * 128]), reads=[lk_])
                    yield

            ngl = 16 if not (dbg and dbg.get('_short')) else 1
            plan = [(ctx_b, 2, 1, 0, False)] + [(x_b[g * 512:(g + 1) * 512, :], 4, 0, LC + g * 512, True) for g in range(ngl)]
            def drain(gen, n=10 ** 9):
                for _ in range(n):
                    try:
                        next(gen)
                    except StopIteration:
                        return True
                return False

            drain(stageA(0, plan[0][0], plan[0][1], plan[0][2]))
            for gi in range(len(plan)):
                ga = stageA(gi + 1, plan[gi + 1][0], plan[gi + 1][1], plan[gi + 1][2]) if gi + 1 < len(plan) else iter(())
                gb = stageB(gi, plan[gi][1], plan[gi][3], plan[gi][4])
                da = db = False
                while not (da and db):
                    if not da:
                        da = drain(ga, 1)
                    if not db:
                        db = drain(gb, 2)
            S.barrier()

        def dbgdump(name, src_ap, rows=None):
            if dbg and name in dbg:
                S.barrier(cc=True)
                S.dma('sp', lambda e: e.dma_start(out=dbg_out[name], in_=src_ap))
                S.barrier()

        stop = (dbg or {}).get('_stop', 99)
        if stop >= 2:
          with ExitStack() as ph:
            Xs = sb(ph, "Xs", [128, 64, 256])
            Wcat = sb(ph, "Wcat", [128, 256])
            wf = sb(ph, "wf", [128, 128])
            ft = [sb(ph, "ft%d" % i, [128, 512]) for i in range(6)]
            with ExitStack() as ph2:
                uT = sb(ph2, "uT", [128, L])
                for i in range(4):
                    S.dma('sp' if i % 2 == 0 else 'act', lambda e: e.dma_start(out=uT[:, i * 2048:(i + 1) * 2048], in_=uT_d[:, i * 2048:(i + 1) * 2048]),
                          writes=['uT'])
                S.dma('act', lambda e: e.dma_start(out=wf[:], in_=w_f), writes=['wf'])
                S.op('pe', lambda e: e.matmul(P[0][:, 0:128], lhsT=C('CCs'), rhs=wf[:], start=True, stop=True), reads=['cp', 'wf'], writes=['P0'], acc=True)
                S.op('pe', lambda e: e.matmul(P[0][:, 128:256], lhsT=C('SCs'), rhs=wf[:], start=True, stop=True), reads=['cp', 'wf'], writes=['P0'], acc=True)
                S.op('dve', lambda e: e.tensor_copy(out=Wcat[:], in_=P[0][:, 0:256]), reads=['P0'], writes=['Wcat'])
                uT3 = uT[:].rearrange("p (t1 t2) -> p t2 t1", t2=64)
                for t2p in range(32):
                    pb, pk = P[1 + t2p % 2], PK[1 + t2p % 2]
                    for h in range(2):
                        S.op('pe', lambda e: e.matmul(pb[:, h * 256:(h + 1) * 256], lhsT=uT3[:, 2 * t2p + h, :], rhs=Wcat[:], start=True, stop=True),
                             reads=['uT', 'Wcat'], writes=[pk], acc=True)
                    dst = Xs[:, 2 * t2p:2 * t2p + 2, :]
                    src = pb[:, 0:512].rearrange("p (a b) -> p a b", b=256)
                    if t2p % 2 == 0:
                        S.op('dve', lambda e: e.tensor_copy(out=dst, in_=src), reads=[pk], writes=['Xs0'])
                    else:
                        S.op('act', lambda e: e.copy(out=dst, in_=src), reads=[pk], writes=['Xs1'])
                S.barrier()
            yT = sb(ph, "yT", [128, L], BF16)
            for q in range(16):
                XA = Xs[:, 4 * q:4 * q + 4, 0:128]
                XB = Xs[:, 4 * q:4 * q + 4, 128:256]
                pr, prk = P[3 + (q % 2) * 2], PK[3 + (q % 2) * 2]
                pi, pik = P[4 + (q % 2) * 2], PK[4 + (q % 2) * 2]
                pr3 = pr[:, :].rearrange("p (a c) -> p a c", c=128)
                pi3 = pi[:, :].rearrange("p (a c) -> p a c", c=128)
                S.op('pe', lambda e: e.matmul(pr3, lhsT=C('C128'), rhs=XA, start=True, stop=False), reads=['cp', 'Xs0', 'Xs1'], writes=[prk], acc=True)
                S.op('pe', lambda e: e.matmul(pr3, lhsT=C('nS128'), rhs=XB, start=False, stop=True), reads=['cp', 'Xs0', 'Xs1'], writes=[prk], acc=True)
                S.op('pe', lambda e: e.matmul(pi3, lhsT=C('C128'), rhs=XB, start=True, stop=False), reads=['cp', 'Xs0', 'Xs1'], writes=[pik], acc=True)
                S.op('pe', lambda e: e.matmul(pi3, lhsT=C('S128'), rhs=XA, start=False, stop=True), reads=['cp', 'Xs0', 'Xs1'], writes=[pik], acc=True)
                Tcb = C('Tc')[:, 4 * q:4 * q + 4].unsqueeze(2).to_broadcast([128, 4, 128])
                Tsb = C('Ts')[:, 4 * q:4 * q + 4].unsqueeze(2).to_broadcast([128, 4, 128])
                f3 = [t[:].rearrange("p (a c) -> p a c", c=128) for t in ft]
                S.op('dve', lambda e: e.tensor_tensor(out=f3[0], in0=pr3, in1=Tcb, op=ALU.mult), reads=[prk, 'cp'], writes=['ft0'])
                S.op('dve', lambda e: e.tensor_tensor(out=f3[1], in0=pi3, in1=Tsb, op=ALU.mult), reads=[pik, 'cp'], writes=['ft1'])
                S.op('pool', lambda e: e.tensor_tensor(out=f3[2], in0=f3[0], in1=f3[1], op=ALU.subtract), reads=['ft0', 'ft1'], writes=['ft2'])
                S.op('dve', lambda e: e.tensor_tensor(out=f3[3], in0=pi3, in1=Tcb, op=ALU.mult), reads=[pik, 'cp'], writes=['ft3'])
                S.op('dve', lambda e: e.tensor_tensor(out=f3[4], in0=pr3, in1=Tsb, op=ALU.mult), reads=[prk, 'cp'], writes=['ft4'])
                S.op('pool', lambda e: e.tensor_tensor(out=f3[5], in0=f3[3], in1=f3[4], op=ALU.add), reads=['ft3', 'ft4'], writes=['ft5'])
                S.dma('pool', lambda e: e.dma_start(out=Y_d[0][:, 4 * q:4 * q + 4, :], in_=f3[2]), reads=['ft2'])
                S.dma('pool', lambda e: e.dma_start(out=Y_d[1][:, 4 * q:4 * q + 4, :], in_=f3[5]), reads=['ft5'])
            S.barrier()
            yr = [sb(ph, "yr%d" % i, [64, 8, 128]) for i in range(2)]
            yi = [sb(ph, "yi%d" % i, [64, 8, 128]) for i in range(2)]
            Yv = [Y_d[i].rearrange("k t c -> t k c") for i in range(2)]
            yTv = yT[:].rearrange("p (k2 k1) -> p k1 k2", k1=128)
            for g in range(16):
                a_, ak = yr[g % 2], 'yr%d' % (g % 2)
                b_, bk = yi[g % 2], 'yi%d' % (g % 2)
                S.dma('sp', lambda e: e.dma_start(out=a_[:], in_=Yv[0][:, 8 * g:8 * g + 8, :]), writes=[ak])
                S.dma('sp', lambda e: e.dma_start(out=b_[:], in_=Yv[1][:, 8 * g:8 * g + 8, :]), writes=[bk])
                pb, pk = P[g % 2], PK[g % 2]
                for kk in range(8):
                    S.op('pe', lambda e: e.matmul(pb[:, kk * 64:(kk + 1) * 64], lhsT=a_[:, kk, :], rhs=C('C64'), start=True, stop=False),
                         reads=[ak, 'cp'], writes=[pk], acc=True)
                    S.op('pe', lambda e: e.matmul(pb[:, kk * 64:(kk + 1) * 64], lhsT=b_[:, kk, :], rhs=C('nS64'), start=False, stop=True),
                         reads=[bk, 'cp'], writes=[pk], acc=True)
                k1a = 8 * g
                src = pb[:, 0:512].rearrange("p (a b) -> p a b", b=64)
                if g % 2 == 0:
                    S.op('dve', lambda e: e.tensor_copy(out=yTv[:, k1a:k1a + 8, :], in_=src), reads=[pk], writes=['yT0'])
                else:
                    S.op('act', lambda e: e.copy(out=yTv[:, k1a:k1a + 8, :], in_=src), reads=[pk], writes=['yT1'])
            for jj in range(4):
                S.dma('sp', lambda e: e.dma_start(out=mix_d[jj * 128:jj * 128 + 128, :], in_=yT[:, jj * 2048:(jj + 1) * 2048]), reads=['yT0', 'yT1'],
                      writes=['mixf%d' % jj])
            if stop >= 3.5:
                ag_mix(0, ['mixf0', 'mixf1'])
                ag_mix(1, ['mixf2', 'mixf3'])
            S.barrier()

        if stop >= 3:
          with ExitStack() as ph:
            oT = sb(ph, "oT", [128, L])
            Sst = [sb(ph, "Sst%d" % i, [128, 128]) for i in range(2)]
            qTg = [sb(ph, "qTg%d" % i, [128, 512]) for i in range(2)]
            kTg = [sb(ph, "kTg%d" % i, [128, 512]) for i in range(2)]
            vg = [sb(ph, "vg%d" % i, [64, 8, 128], BF16) for i in range(2)]
            Sb = [sb(ph, "Sb%d" % i, [128, 128], BF16) for i in range(2)]
            lfg = [sb(ph, "lfg%d" % i, [64, 8, 128]) for i in range(2)]
            ktg = [sb(ph, "ktg%d" % i, [64, 8, 128]) for i in range(2)]
            eB = [sb(ph, "eB%d" % i, [128, 512]) for i in range(2)]
            enB = [sb(ph, "enB%d" % i, [128, 512]) for i in range(2)]
            eR = [sb(ph, "eR%d" % i, [64, 8, 128]) for i in range(2)]
            Qt = [sb(ph, "Qt%d" % i, [128, 512], BF16) for i in range(2)]
            Kt = [sb(ph, "Kt%d" % i, [128, 512], BF16) for i in range(2)]
            Kh = [sb(ph, "Kh%d" % i, [64, 8, 128], BF16) for i in range(2)]
            ATm = [sb(ph, "ATm%d" % i, [64, 8, 64], BF16) for i in range(2)]
            ngl = 16 if not (dbg and dbg.get('_short')) else 1
            scur = [0]

            def pre1(g):
                gi, nch, latent, tok0, d_ = g['gi'], g['nch'], g['latent'], g['tok0'], g['d']
                nt = nch * 64
                tri = C('TriF') if d_ == 0 else C('TriB')
                slo = C('SLoF') if d_ == 0 else C('SLoB')
                S.dma('sp', lambda e: e.dma_start(out=lfg[gi][:, 0:nch, :], in_=lf_d[d_][tok0:tok0 + nt, :].rearrange("(c s) d -> s c d", s=64)),
                      writes=['lfg%d' % gi])
                S.dma('sp', lambda e: e.dma_start(out=ktg[gi][:, 0:nch, :], in_=k_d[d_][tok0:tok0 + nt, :].rearrange("(c s) d -> s c d", s=64)),
                      writes=['ktg%d' % gi])
                S.dma('sp', lambda e: e.dma_start(out=vg[gi][:, 0:nch, :], in_=v_d[tok0:tok0 + nt, :].rearrange("(c s) d -> s c d", s=64)),
                      writes=['vg%d' % gi])
                if latent:
                    S.dma('sp', lambda e: e.dma_start(out=kTg[gi][:, 0:nt], in_=kT_d[d_][:, tok0:tok0 + nt]), writes=['kTg%d' % gi])
                    S.dma('sp', lambda e: e.dma_start(out=qTg[gi][:, 0:nt], in_=qT_d[:, tok0 - LC:tok0 - LC + nt]), writes=['qTg%d' % gi])
                for ch in range(nch):
                    S.op('pe', lambda e: e.matmul(P[0][:, ch * 64:(ch + 1) * 64], lhsT=lfg[gi][:, ch, :], rhs=tri, start=True, stop=True),
                         reads=['lfg%d' % gi, 'cp'], writes=['P0'], acc=True)
                S.op('act', lambda e: e.activation(out=eB[gi][:, 0:nt], in_=P[0][:, 0:nt], func=AF.Exp), reads=['P0'], writes=['eB%d' % gi])
                if latent:
                    S.op('act', lambda e: e.activation(out=enB[gi][:, 0:nt], in_=P[0][:, 0:nt], func=AF.Exp, scale=-1.0), reads=['P0'], writes=['enB%d' % gi])
                for hf in range((nch + 3) // 4):
                    n4 = min(4, nch - hf * 4)
                    for c4 in range(n4):
                        S.op('pe', lambda e: e.matmul(P[1][0:64, c4 * 128:(c4 + 1) * 128], lhsT=slo, rhs=lfg[gi][:, hf * 4 + c4, :], start=True, stop=True),
                             reads=['lfg%d' % gi, 'cp'], writes=['P1'], acc=True)
                    S.op('act', lambda e: e.activation(out=eR[gi][:, hf * 4:hf * 4 + n4, :],
                                                       in_=P[1][0:64, 0:n4 * 128].rearrange("p (a b) -> p a b", b=128), func=AF.Exp),
                         reads=['P1'], writes=['eR%d' % gi])
                S.op('dve', lambda e: e.tensor_tensor(out=Kh[gi][:, 0:nch, :], in0=ktg[gi][:, 0:nch, :], in1=eR[gi][:, 0:nch, :], op=ALU.mult),
                     reads=['ktg%d' % gi, 'eR%d' % gi], writes=['Kh%d' % gi])
                if latent:
                    S.op('dve', lambda e: e.tensor_tensor(out=Qt[gi][:, 0:nt], in0=qTg[gi][:, 0:nt], in1=eB[gi][:, 0:nt], op=ALU.mult),
                         reads=['qTg%d' % gi, 'eB%d' % gi], writes=['Qt%d' % gi])
                    S.op('dve', lambda e: e.tensor_tensor(out=Kt[gi][:, 0:nt], in0=kTg[gi][:, 0:nt], in1=enB[gi][:, 0:nt], op=ALU.mult),
                         reads=['kTg%d' % gi, 'enB%d' % gi], writes=['Kt%d' % gi])

            def pre2(g):
                gi, nch, latent, d_ = g['gi'], g['nch'], g['latent'], g['d']
                tri = C('TriF') if d_ == 0 else C('TriB')
                if latent:
                    for ch in range(nch):
                        cs = slice(ch * 64, (ch + 1) * 64)
                        S.op('pe', lambda e: e.matmul(P[2][0:64, cs], lhsT=Kt[gi][:, cs], rhs=Qt[gi][:, cs], start=True, stop=True),
                             reads=['Kt%d' % gi, 'Qt%d' % gi], writes=['P2'], acc=True)
                    S.op('dve', lambda e: e.tensor_tensor(out=ATm[gi][:, 0:nch, :], in0=P[2][0:64, 0:nch * 64].rearrange("p (a b) -> p a b", b=64),
                                                          in1=tri.unsqueeze(1).to_broadcast([64, nch, 64]), op=ALU.mult),
                         reads=['P2', 'cp'], writes=['ATm%d' % gi])
                ub = g['ub']
                for ch in range(nch):
                    S.op('pe', lambda e: e.matmul(P[ub + ch // 4][:, (ch % 4) * 128:(ch % 4 + 1) * 128], lhsT=Kh[gi][:, ch, :], rhs=vg[gi][:, ch, :],
                                                  start=True, stop=True),
                         reads=['Kh%d' % gi, 'vg%d' % gi], writes=[PK[ub + ch // 4]], acc=True)

            def seq(g, chs):
                gi, latent, d_, ub = g['gi'], g['latent'], g['d'], g['ub']
                for ch in chs:
                    cs = slice(ch * 64, (ch + 1) * 64)
                    cur = scur[0]
                    if latent:
                        S.op('act', lambda e: e.copy(out=Sb[cur][:], in_=Sst[cur][:]), reads=['S%d' % cur], writes=['Sb%d' % cur])
                        S.op('pe', lambda e: e.matmul(P[7][:, cs], lhsT=Sb[cur][:], rhs=Qt[gi][:, cs], start=True, stop=False),
                             reads=['Sb%d' % cur, 'Qt%d' % gi], writes=['P7'], acc=True)
                        S.op('pe', lambda e: e.matmul(P[7][:, cs], lhsT=vg[gi][:, ch, :], rhs=ATm[gi][:, ch, :], start=False, stop=True),
                             reads=['vg%d' % gi, 'ATm%d' % gi], writes=['P7'], acc=True)
                    dc = ch * 64 + (63 if d_ == 0 else 0)
                    S.op('dve', lambda e: e.scalar_tensor_tensor(out=Sst[1 - cur][:], in0=Sst[cur][:], scalar=eB[gi][:, dc:dc + 1],
                                                                 in1=P[ub + ch // 4][:, (ch % 4) * 128:(ch % 4 + 1) * 128], op0=ALU.mult, op1=ALU.add),
                         reads=['S%d' % cur, 'eB%d' % gi, PK[ub + ch // 4]], writes=['S%d' % (1 - cur)])
                    scur[0] = 1 - cur

            def fin(g):
                if not g['latent']:
                    return
                nt = g['nch'] * 64
                c0 = g['tok0'] - LC
                if g['d'] == 0:
                    S.op('act', lambda e: e.copy(out=oT[:, c0:c0 + nt], in_=P[7][:, 0:nt]), reads=['P7'], writes=['oT'])
                else:
                    S.op('dve', lambda e: e.tensor_tensor(out=oT[:, c0:c0 + nt], in0=oT[:, c0:c0 + nt], in1=P[7][:, 0:nt], op=ALU.add),
                         reads=['P7', 'oT'], writes=['oT'])

            gcount = 0
            for d_ in range(2):
                S.op('dve', lambda e: e.memset(Sst[scur[0]][:], 0.0), writes=['S%d' % scur[0]])
                glist = []
                for (tok0, nch, latent) in [(0, 4, False)] + [(LC + g * 512, 8, True) for g in (range(ngl) if d_ == 0 else range(ngl - 1, -1, -1))]:
                    glist.append(dict(gi=gcount % 2, ub=3 + 2 * (gcount % 2), nch=nch, latent=latent, tok0=tok0, d=d_))
                    gcount += 1
                pre1(glist[0])
                pre2(glist[0])
                for ix, g in enumerate(glist):
                    order = list(range(g['nch'])) if d_ == 0 else list(range(g['nch'] - 1, -1, -1))
                    hlf = len(order) // 2
                    nxt = glist[ix + 1] if ix + 1 < len(glist) else None
                    if nxt:
                        pre1(nxt)
                    seq(g, order[:hlf])
                    if nxt:
                        pre2(nxt)
                    seq(g, order[hlf:])
                    fin(g)
            rt = [sb(ph, "rt%d" % i, [128, 512]) for i in range(3)]
            rb = [sb(ph, "rb%d" % i, [128, 512], BF16) for i in range(2)]
            sgt = [sb(ph, "sgt%d" % i, [128, 512]) for i in range(2)]
            for g in range(ngl):
                cs = slice(g * 512, (g + 1) * 512)
                S.dma('sp', lambda e: e.dma_start(out=sgt[g % 2][:], in_=sgT_d[:, cs]), writes=['sgt%d' % (g % 2)])
                S.op('act', lambda e: e.activation(out=rt[0][:], in_=oT[:, cs], func=AF.Square), reads=['oT'], writes=['rt0'])
                S.op('pe', lambda e: e.matmul(P[5][:, :], lhsT=C('avg128'), rhs=rt[0][:], start=True, stop=True), reads=['cp', 'rt0'], writes=['P5'], acc=True)
                S.op('act', lambda e: e.activation(out=rt[1][:], in_=P[5][:, :], func=AF.Ln, bias=EPS, scale=1.0), reads=['P5'], writes=['rt1'])
                S.op('act', lambda e: e.activation(out=rt[1][:], in_=rt[1][:], func=AF.Exp, scale=-0.5), reads=['rt1'], writes=['rt1'])
                S.op('dve', lambda e: e.tensor_tensor(out=rt[2][:], in0=oT[:, cs], in1=rt[1][:], op=ALU.mult), reads=['oT', 'rt1'], writes=['rt2'])
                ob = rb[g % 2]
                okk = 'rb%d' % (g % 2)
                S.op('dve', lambda e: e.scalar_tensor_tensor(out=ob[:], in0=rt[2][:], scalar=PP('grec_fm'), in1=sgt[g % 2][:], op0=ALU.mult, op1=ALU.mult),
                     reads=['rt2', 'pp', 'sgt%d' % (g % 2)], writes=[okk])
                jj = g // 4
                S.dma('sp', lambda e: e.dma_start(out=mix_d[512 + jj * 128:512 + jj * 128 + 128, (g % 4) * 512:(g % 4 + 1) * 512], in_=ob[:]),
                      reads=[okk], writes=['mixr%d' % g])
                if stop >= 3.5 and g % 8 == 7:
                    ag_mix(2 + g // 8, ['mixr%d' % gg for gg in range(g - 7, g + 1)])
            S.barrier()

        if stop >= 4:
          with ExitStack() as ph:
            wo = sb(ph, "wo", [128, 8, D], BF16)
            imix = sb(ph, "imix", [128, 8], I32)
            mT = sb(ph, "mT", [128, 8, 2048], BF16)
            affT = sb(ph, "affT", [16, 2048])
            xt3 = [sb(ph, "x3t%d" % i, [128, D]) for i in range(2)]
            x1t = [sb(ph, "x1t%d" % i, [128, D]) for i in range(2)]
            h2t = [sb(ph, "h2t%d" % i, [128, D]) for i in range(2)]
            h2b = [sb(ph, "h2b%d" % i, [128, D], BF16) for i in range(2)]
            h2T = sb(ph, "h2T", [128, 8, 128])
            junk3 = sb(ph, "junk3", [128, D])
            st3 = sb(ph, "st3", [128, 8])
            eT = sb(ph, "eT", [16, 128])
            rs16 = sb(ph, "rs16", [16, 128])
            S.dma('sp', lambda e: e.dma_start(out=imix[:], in_=idx_mix_d), writes=['imix'])
            with ExitStack() as ph0:
                wa = [sb(ph0, "wa3%d" % i, [128, 8, 512]) for i in range(2)]
                brows = sb(ph0, "brows", [128, 4096])
                S.dma('act', lambda e: e.dma_start(out=brows[:], in_=bada_rows_d), writes=['brows'])
                w_ada_v = w_ada.rearrange("(k p) n -> p k n", p=128)
                for cb in range(4, 12):
                    wt = wa[cb % 2]
                    wk = 'wa%d' % (cb % 2)
                    S.dma('sp' if cb % 2 == 0 else 'act', lambda e: e.dma_start(out=wt[:], in_=w_ada_v[:, :, cb * 512:(cb + 1) * 512]), writes=[wk])
                    pb = P[1 + (cb % 2)]
                    pk = PK[1 + (cb % 2)]
                    for k in range(8):
                        S.op('pe', lambda e: e.matmul(pb[:, :], lhsT=scB[:, k, :], rhs=wt[:, k, :], start=(k == 0), stop=(k == 7)),
                             reads=[wk, 'scB'], writes=[pk], acc=True)
                    o = (cb - 4) * 512
                    S.op('dve', lambda e: e.tensor_tensor(out=rows[:, o:o + 512], in0=pb[:, :], in1=brows[:, o:o + 512], op=ALU.add),
                         reads=[pk, 'brows'], writes=['rows'])
                S.op('dve', lambda e: e.scalar_tensor_tensor(out=rows[:, 2048:3072], in0=rows[:, 2048:3072], scalar=1.0, in1=PP('gffn_row'),
                                                             op0=ALU.add, op1=ALU.mult),
                     reads=['rows', 'pp'], writes=['rows'])
                S.barrier()
            with ExitStack() as ph0:
                wof = sb(ph0, "wof", [128, 8, D])
                S.dma('sp', lambda e: e.dma_start(out=wof[:], in_=w_out.rearrange("(k p) n -> p k n", p=128)), writes=['wof'])
                for k in range(8):
                    if k % 2 == 0:
                        S.op('act', lambda e: e.copy(out=wo[:, k, :], in_=wof[:, k, :]), reads=['wof'], writes=['wo'])
                    else:
                        S.op('dve', lambda e: e.tensor_copy(out=wo[:, k, :], in_=wof[:, k, :]), reads=['wof'], writes=['wo'])
                S.barrier()
            for k in range(8):
                S.dma('pool', lambda e: e.indirect_dma_start(out=mT[:, k, :], out_offset=None, in_=mixall_d,
                                                            in_offset=bass.IndirectOffsetOnAxis(ap=imix[:, k:k + 1], axis=0)),
                      reads=['imix', 'mixall'], writes=['mT'])
            wr = PP('wr').rearrange("p (k e) -> p k e", e=16)
            def p3A(i):
                xb, xk = xt3[i % 2], 'x3t%d' % (i % 2)
                x1, x1k = x1t[i % 2], 'x1t%d' % (i % 2)
                h2, h2k = h2t[i % 2], 'h2t%d' % (i % 2)
                sk = 'st3_%d' % (i % 2)
                st = st3[:, (i % 2) * 4:(i % 2) * 4 + 4]
                ts_ = slice(i * 128, (i + 1) * 128)
                S.dma('sp', lambda e: e.dma_start(out=xb[:], in_=x_own[ts_, :]), writes=[xk])
                for half in range(2):
                    for k in range(8):
                        S.op('pe', lambda e: e.matmul(P[half][:, :], lhsT=mT[:, k, ts_], rhs=wo[:, k, half * 512:(half + 1) * 512],
                                                      start=(k == 0), stop=(k == 7)),
                             reads=['mT', 'wo'], writes=[PK[half]], acc=True)
                    hs = slice(half * 512, (half + 1) * 512)
                    S.op('dve', lambda e: e.tensor_tensor(out=x1[:, hs], in0=P[half][:, :], in1=rows[:, half * 512:(half + 1) * 512], op=ALU.mult),
                         reads=[PK[half], 'rows'], writes=[x1k])
                S.op('dve', lambda e: e.tensor_tensor(out=x1[:], in0=x1[:], in1=xb[:], op=ALU.add), reads=[x1k, xk], writes=[x1k])
                S.dma('sp', lambda e: e.dma_start(out=x1_d[ts_, :], in_=x1[:]), reads=[x1k])
                S.op('act', lambda e: e.activation(out=junk3[:], in_=x1[:], func=AF.Square, accum_out=st[:, 0:1]), reads=[x1k], writes=['junk3', sk])
                S.op('act', lambda e: e.activation(out=st[:, 1:2], in_=st[:, 0:1], func=AF.Ln, scale=1.0 / D, bias=EPS), reads=[sk], writes=[sk])
                S.op('act', lambda e: e.activation(out=st[:, 2:3], in_=st[:, 1:2], func=AF.Exp, scale=-0.5), reads=[sk], writes=[sk])
                S.op('dve', lambda e: e.scalar_tensor_tensor(out=h2[:], in0=x1[:], scalar=st[:, 2:3], in1=rows[:, 2048:3072], op0=ALU.mult, op1=ALU.mult),
                     reads=[x1k, sk, 'rows'], writes=[h2k])
                S.op('dve', lambda e: e.tensor_tensor(out=h2[:], in0=h2[:], in1=rows[:, 1024:2048], op=ALU.add), reads=[h2k, 'rows'], writes=[h2k])
                S.op('act', lambda e: e.copy(out=h2b[i % 2][:], in_=h2[:]), reads=[h2k], writes=['h2b%d' % (i % 2)])
                S.dma('sp', lambda e: e.dma_start(out=h2_d[ts_, :], in_=h2b[i % 2][:]), reads=['h2b%d' % (i % 2)], writes=['h2_dt%d' % i])
                if stop >= 5 and i % 4 == 3:
                    S.collective("AllGather", ALU.bypass, [h2_d[(i // 4) * 512:(i // 4 + 1) * 512, :]],
                                 [h2all_d[(i // 4) * 2048:(i // 4 + 1) * 2048, :]], reads=['h2_dt%d' % ii for ii in range(i - 3, i + 1)],
                                 writes=['h2all%d' % (i // 4)])

            def p3B(i):
                h2, h2k = h2t[i % 2], 'h2t%d' % (i % 2)
                ts_ = slice(i * 128, (i + 1) * 128)
                for half in range(2):
                    pb, pk = P[2 + half], PK[2 + half]
                    for kk in range(4):
                        k = half * 4 + kk
                        S.op('pe', lambda e: e.transpose(pb[:, kk * 128:(kk + 1) * 128], h2[:, k * 128:(k + 1) * 128], ident),
                             reads=[h2k, 'cp'], writes=[pk], acc=True)
                    dst = h2T[:, half * 4:(half + 1) * 4, :]
                    src = pb[:, :].rearrange("p (a b) -> p a b", b=128)
                    if half == 0:
                        S.op('act', lambda e: e.copy(out=dst, in_=src), reads=[pk], writes=['h2T0'])
                    else:
                        S.op('dve', lambda e: e.tensor_copy(out=dst, in_=src), reads=[pk], writes=['h2T1'])
                for k in range(8):
                    S.op('pe', lambda e: e.matmul(P[4][0:16, 0:128], lhsT=wr[:, k, :], rhs=h2T[:, k, :], start=(k == 0), stop=(k == 7)),
                         reads=['pp', 'h2T%d' % (k // 4)], writes=['P4'], acc=True)
                S.op('act', lambda e: e.activation(out=eT[:], in_=P[4][0:16, 0:128], func=AF.Exp), reads=['P4'], writes=['eT'])
                S.op('pe', lambda e: e.matmul(P[5][0:16, 0:128], lhsT=C('ones16'), rhs=eT[:], start=True, stop=True), reads=['cp', 'eT'], writes=['P5'], acc=True)
                S.op('dve', lambda e: e.reciprocal(out=rs16[:], in_=P[5][0:16, 0:128]), reads=['P5'], writes=['rs16'])
                S.op('dve', lambda e: e.tensor_tensor(out=affT[:, ts_], in0=eT[:], in1=rs16[:], op=ALU.mult), reads=['eT', 'rs16'], writes=['affT'])

            p3A(0)
            for i in range(16):
                if i + 1 < 16:
                    p3A(i + 1)
                p3B(i)
            S.dma('sp', lambda e: e.dma_start(out=affT_d, in_=affT[:]), reads=['affT'], writes=['affT_d'])
            if stop >= 5:
                S.collective("AllGather", ALU.bypass, [affT_d], [affall_d], reads=['affT_d'], writes=['affall'])
            S.barrier()
        dbgdump('x1', x1_d)
        dbgdump('h2', h2_d)
        dbgdump('affT', affT_d)

        si = [sb(top, "si%d" % i, [128, 64], I32) for i in range(4)]
        gidx = [sb(top, "gi%d" % i, [128, 64], I32) for i in range(4)]
        wpc = [sb(top, "wpc%d" % i, [128, 64]) for i in range(4)]
        if stop >= 5:
          with ExitStack() as ph:
            af = sb(ph, "af", [128, 1024])
            junk4 = sb(ph, "junk4", [128, 1024])
            bis = sb(ph, "bis", [128, 8])
            lo, hi, mid, cnt, ge, dd = [bis[:, i:i + 1] for i in range(6)]
            S.dma('sp', lambda e: e.dma_start(out=af[:], in_=affall_d.rearrange("a (h t) -> (a h) t", h=2)), reads=['affall'], writes=['af'])
            S.op('dve', lambda e: e.memset(lo, 0.0), writes=['bis'])
            S.op('dve', lambda e: e.memset(hi, 1.5), writes=['bis'])
            for it in range(28):
                S.op('dve', lambda e: e.tensor_tensor(out=mid, in0=lo, in1=hi, op=ALU.add), reads=['bis'], writes=['bis'])
                S.op('dve', lambda e: e.tensor_scalar(out=mid, in0=mid, scalar1=0.5, scalar2=None, op0=ALU.mult), reads=['bis'], writes=['bis'])
                S.op('dve', lambda e: e.tensor_scalar(out=junk4[:], in0=af[:], scalar1=mid, scalar2=0.0, op0=ALU.is_ge, op1=ALU.add, accum_out=cnt),
                     reads=['bis', 'af'], writes=['junk4', 'bis'])
                S.op('pe', lambda e: e.matmul(P[0][:, 0:1], lhsT=C('G'), rhs=cnt, start=True, stop=True), reads=['cp', 'bis'], writes=['P0'], acc=True)
                S.op('dve', lambda e: e.tensor_scalar(out=ge, in0=P[0][:, 0:1], scalar1=CAP - 0.5, scalar2=None, op0=ALU.is_ge), reads=['P0'], writes=['bis'])
                S.op('dve', lambda e: e.tensor_tensor(out=dd, in0=mid, in1=lo, op=ALU.subtract), reads=['bis'], writes=['bis'])
                S.op('dve', lambda e: e.scalar_tensor_tensor(out=lo, in0=dd, scalar=ge, in1=lo, op0=ALU.mult, op1=ALU.add), reads=['bis'], writes=['bis'])
                S.op('dve', lambda e: e.tensor_tensor(out=dd, in0=hi, in1=mid, op=ALU.subtract), reads=['bis'], writes=['bis'])
                S.op('dve', lambda e: e.scalar_tensor_tensor(out=hi, in0=dd, scalar=ge, in1=mid, op0=ALU.mult, op1=ALU.add), reads=['bis'], writes=['bis'])
            S.op('dve', lambda e: e.tensor_scalar(out=junk4[:], in0=af[:], scalar1=lo, scalar2=None, op0=ALU.is_ge), reads=['bis', 'af'], writes=['junk4'])
            S.op('dve', lambda e: e.tensor_tensor(out=af[:], in0=af[:], in1=junk4[:], op=ALU.mult), reads=['junk4', 'af'], writes=['af'])
            S.dma('sp', lambda e: e.dma_start(out=mk_d, in_=junk4[:]), reads=['junk4'], writes=['mk_d'])
            S.dma('act', lambda e: e.dma_start(out=wg_d, in_=af[:]), reads=['af'], writes=['wg_d'])
            imk = sb(ph, "imk", [64, 4], I32)
            S.dma('sp', lambda e: e.dma_start(out=imk[:], in_=idx_mk_d), writes=['imk'])
            mkT = sb(ph, "mkT", [64, 128])
            wgT = sb(ph, "wgT", [64, 128])
            mk = sb(ph, "mk", [128, 64])
            totb = sb(ph, "totb", [128, 64])
            slf = sb(ph, "slf", [128, 64])
            sl2 = sb(ph, "sl2", [128, 64])
            tot = sb(ph, "tot", [128, 1])
            mkv = mk_d.rearrange("q (c p) -> (q c) p", p=128)
            wgv = wg_d.rearrange("q (c p) -> (q c) p", p=128)
            for el in range(4):
                S.dma('pool', lambda e: e.indirect_dma_start(out=mkT[:, :], out_offset=None, in_=mkv,
                                                            in_offset=bass.IndirectOffsetOnAxis(ap=imk[:, el:el + 1], axis=0)),
                      reads=['imk', 'mk_d'], writes=['mkT'])
                S.dma('pool', lambda e: e.indirect_dma_start(out=wgT[:, :], out_offset=None, in_=wgv,
                                                            in_offset=bass.IndirectOffsetOnAxis(ap=imk[:, el:el + 1], axis=0)),
                      reads=['imk', 'wg_d'], writes=['wgT'])
                S.op('pe', lambda e: e.transpose(P[1][:, 0:64], mkT[:, :], C('ident', 64)[:, 0:64]), reads=['mkT', 'cp'], writes=['P1'], acc=True)
                S.op('pe', lambda e: e.transpose(P[2][:, 0:64], wgT[:, :], C('ident', 64)[:, 0:64]), reads=['wgT', 'cp'], writes=['P2'], acc=True)
                S.op('dve', lambda e: e.tensor_copy(out=mk[:], in_=P[1][:, 0:64]), reads=['P1'], writes=['mk'])
                S.op('act', lambda e: e.copy(out=wpc[el][:], in_=P[2][:, 0:64]), reads=['P2'], writes=['wpc%d' % el])
                S.op('dve', lambda e: e.reduce_sum(out=tot[:], in_=mk[:], axis=AX.X), reads=['mk'], writes=['tot'])
                S.op('dve', lambda e: e.tensor_scalar(out=totb[:], in0=C('one128')[:, 0:64], scalar1=tot[:, 0:1], scalar2=None, op0=ALU.mult),
                     reads=['tot', 'cp'], writes=['totb'])
                S.op('pe', lambda e: e.matmul(P[3][:, 0:64], lhsT=mkT[:, :], rhs=C('SU64'), start=True, stop=False), reads=['mkT', 'cp'], writes=['P3'], acc=True)
                S.op('pe', lambda e: e.matmul(P[3][:, 0:64], lhsT=C('L128'), rhs=totb[:], start=False, stop=True), reads=['totb', 'cp'], writes=['P3'], acc=True)
                S.op('dve', lambda e: e.tensor_copy(out=slf[:], in_=P[3][:, 0:64]), reads=['P3'], writes=['slf'])
                S.op('dve', lambda e: e.tensor_tensor(out=sl2[:], in0=slf[:], in1=mk[:], op=ALU.mult), reads=['slf', 'mk'], writes=['sl2'])
                S.op('dve', lambda e: e.tensor_scalar(out=sl2[:], in0=sl2[:], scalar1=float(CAP - 1), scalar2=None, op0=ALU.min), reads=['sl2'], writes=['sl2'])
                S.op('dve', lambda e: e.tensor_copy(out=gidx[el][:], in_=sl2[:]), reads=['sl2'], writes=['gi%d' % el])
                S.op('dve', lambda e: e.tensor_scalar(out=sl2[:], in0=slf[:], scalar1=-5000.0, scalar2=None, op0=ALU.add), reads=['slf'], writes=['sl2'])
                S.op('dve', lambda e: e.tensor_tensor(out=sl2[:], in0=sl2[:], in1=mk[:], op=ALU.mult), reads=['sl2', 'mk'], writes=['sl2'])
                S.op('dve', lambda e: e.tensor_scalar(out=sl2[:], in0=sl2[:], scalar1=5000.0, scalar2=None, op0=ALU.add), reads=['sl2'], writes=['sl2'])
                S.op('dve', lambda e: e.tensor_copy(out=si[el][:], in_=sl2[:]), reads=['sl2'], writes=['si%d' % el])
            if dbg and 'slots' in dbg:
                for el in range(4):
                    S.op('dve', lambda e: e.tensor_copy(out=junk4[:, el * 64:(el + 1) * 64], in_=si[el][:]), reads=['si%d' % el], writes=['junk4'])
                    S.op('dve', lambda e: e.tensor_copy(out=junk4[:, 256 + el * 64:256 + (el + 1) * 64], in_=wpc[el][:]), reads=['wpc%d' % el], writes=['junk4'])
                S.dma('sp', lambda e: e.dma_start(out=dbg_out['slots'], in_=junk4[:, 0:512]), reads=['junk4'])
            S.barrier()

        if stop >= 7:
          with ExitStack() as ph:
            xsT = sb(ph, "xsT", [128, 8, CAP], BF16)
            hid = sb(ph, "hid", [128, NFC, CAP], BF16)
            wgf = [sb(ph, "wgf%d" % i, [128, 8, 256]) for i in range(2)]
            wuf = [sb(ph, "wuf%d" % i, [128, 8, 256]) for i in range(2)]
            wgb = [sb(ph, "wgb%d" % i, [128, 8, 256], BF16) for i in range(2)]
            wub = [sb(ph, "wub%d" % i, [128, 8, 256], BF16) for i in range(2)]
            wdf = [sb(ph, "wdf%d" % i, [128, D]) for i in range(3)]
            wdb = [sb(ph, "wdb%d" % i, [128, D], BF16) for i in range(3)]
            xr = [sb(ph, "xr%d" % i, [128, D], BF16) for i in range(2)]
            yt = [sb(ph, "yt%d" % i, [128, D]) for i in range(2)]
            tmp6 = [sb(ph, "tmp6%d" % i, [128, 512]) for i in range(2)]
            identb = sb(ph, "identb", [128, 128], BF16)
            S.op('dve', lambda e: e.tensor_copy(out=identb[:], in_=ident), reads=['cp'], writes=['identb'])
            nexp = 4 if not (dbg and dbg.get('_short')) else 1
            hb = [sb(ph, "hb%d" % i, [128, D], BF16) for i in range(4)]
            breg = nc.gpsimd.to_reg(CAP - 1)
            hcnt = [0]

            def dispatch(el):
                def load(blk):
                    i_ = (hcnt[0] + blk) % 4
                    S.dma('pool', lambda e: e.dma_start(out=hb[i_][:], in_=h2all_d[blk * 128:(blk + 1) * 128, :]),
                          reads=['h2all%d' % (blk // 16)], writes=['hb%d' % i_])
                load(0)
                load(1)
                for blk in range(64):
                    if blk + 2 < 64:
                        load(blk + 2)
                    i_ = (hcnt[0] + blk) % 4
                    tb = ((blk % 16) // 4) * 16 + (blk // 16) * 4 + (blk % 4)
                    S.dma('pool', lambda e: e.indirect_dma_start(out=xs_d[el], out_offset=bass.IndirectOffsetOnAxis(ap=si[el][:, tb:tb + 1], axis=0),
                                                                in_=hb[i_][:, :], in_offset=None, bounds_check=breg, oob_is_err=False),
                          reads=['hb%d' % i_, 'si%d' % el])
                hcnt[0] += 64
                S.fence('xsd%d' % el, 'pool')

            dispatch(0)
            wcnt = 0
            dcnt = 0
            tcnt = 0
            xcnt = 0
            for el in range(nexp):
                if el + 1 < nexp:
                    dispatch(el + 1)
                wgv_ = weg[el].rearrange("(k p) f -> p k f", p=128)
                wuv_ = weu[el].rearrange("(k p) f -> p k f", p=128)
                for st in range(8):
                    r0 = st * 128
                    xb, xk = xr[xcnt % 2], 'xr%d' % (xcnt % 2)
                    xcnt += 1
                    S.dma('sp', lambda e: e.dma_start(out=xb[:], in_=xs_d[el][r0:r0 + 128, :]), reads=['xsd%d' % el], writes=[xk])
                    for h2_ in range(2):
                        pb, pk = P[4 + h2_][:, :].bitcast(BF16), PK[4 + h2_]
                        for kk in range(4):
                            k = h2_ * 4 + kk
                            S.op('pe', lambda e: e.transpose(pb[:, kk * 128:(kk + 1) * 128], xb[:, k * 128:(k + 1) * 128], identb[:]),
                                 reads=[xk, 'identb'], writes=[pk], acc=True)
                        dst = xsT[:, h2_ * 4:(h2_ + 1) * 4, st * 128:(st + 1) * 128]
                        src = pb[:, 0:512].rearrange("p (a b) -> p a b", b=128)
                        if h2_ == 0:
                            S.op('act', lambda e: e.copy(out=dst, in_=src), reads=[pk], writes=['xsT0'])
                        else:
                            S.op('dve', lambda e: e.tensor_copy(out=dst, in_=src), reads=[pk], writes=['xsT1'])
                for fg in range(11):
                    wi = wcnt % 2
                    wcnt += 1
                    S.dma('sp', lambda e: e.dma_start(out=wgf[wi][:], in_=wgv_[:, :, fg * 256:(fg + 1) * 256]), writes=['wgf%d' % wi])
                    S.dma('sp', lambda e: e.dma_start(out=wuf[wi][:], in_=wuv_[:, :, fg * 256:(fg + 1) * 256]), writes=['wuf%d' % wi])
                    S.op('act', lambda e: e.copy(out=wgb[wi][:], in_=wgf[wi][:]), reads=['wgf%d' % wi], writes=['wgb%d' % wi])
                    S.op('dve', lambda e: e.tensor_copy(out=wub[wi][:], in_=wuf[wi][:]), reads=['wuf%d' % wi], writes=['wub%d' % wi])
                    for fc in range(2):
                        for half in range(2):
                            pg, pgk = P[half], PK[half]
                            pu, puk = P[2 + half], PK[2 + half]
                            hs = slice(half * 512, (half + 1) * 512)
                            for k in range(8):
                                S.op('pe', lambda e: e.matmul(pg[:, :], lhsT=wgb[wi][:, k, fc * 128:(fc + 1) * 128], rhs=xsT[:, k, hs],
                                                              start=(k == 0), stop=(k == 7)),
                                     reads=['wgb%d' % wi, 'xsT%d' % (k // 4)], writes=[pgk], acc=True)
                            for k in range(8):
                                S.op('pe', lambda e: e.matmul(pu[:, :], lhsT=wub[wi][:, k, fc * 128:(fc + 1) * 128], rhs=xsT[:, k, hs],
                                                              start=(k == 0), stop=(k == 7)),
                                     reads=['wub%d' % wi, 'xsT%d' % (k // 4)], writes=[puk], acc=True)
                            ti = tcnt % 2
                            tcnt += 1
                            S.op('act', lambda e: e.activation(out=tmp6[ti][:], in_=pg[:, :], func=AF.Silu), reads=[pgk], writes=['tmp6%d' % ti])
                            S.op('dve', lambda e: e.tensor_tensor(out=hid[:, fg * 2 + fc, hs], in0=tmp6[ti][:], in1=pu[:, :], op=ALU.mult),
                                 reads=['tmp6%d' % ti, puk], writes=['hid'])
                for tg in range(2):
                    for fch in range(NFC):
                        di = dcnt % 3
                        dcnt += 1
                        S.dma('sp', lambda e: e.dma_start(out=wdf[di][:], in_=wed[el][fch * 128:(fch + 1) * 128, :]), writes=['wdf%d' % di])
                        if fch % 2 == 0:
                            S.op('act', lambda e: e.copy(out=wdb[di][:], in_=wdf[di][:]), reads=['wdf%d' % di], writes=['wdb%d' % di])
                        else:
                            S.op('dve', lambda e: e.tensor_copy(out=wdb[di][:], in_=wdf[di][:]), reads=['wdf%d' % di], writes=['wdb%d' % di])
                        for st in range(4):
                            for dh in range(2):
                                S.op('pe', lambda e: e.matmul(P[st * 2 + dh][:, :], lhsT=hid[:, fch, (tg * 4 + st) * 128:(tg * 4 + st + 1) * 128],
                                                              rhs=wdb[di][:, dh * 512:(dh + 1) * 512], start=(fch == 0), stop=(fch == NFC - 1)),
                                     reads=['hid', 'wdb%d' % di], writes=[PK[st * 2 + dh]], acc=True)
                    for st in range(4):
                        yb, yk = yt[st % 2], 'yt%d' % (st % 2)
                        S.op('act', lambda e: e.copy(out=yb[:, 0:512], in_=P[st * 2][:, :]), reads=[PK[st * 2]], writes=[yk])
                        S.op('dve', lambda e: e.tensor_copy(out=yb[:, 512:1024], in_=P[st * 2 + 1][:, :]), reads=[PK[st * 2 + 1]], writes=[yk])
                        r0 = (tg * 4 + st) * 128
                        S.dma('act', lambda e: e.dma_start(out=ye_d[el][r0:r0 + 128, :], in_=yb[:]), reads=[yk])
            S.barrier()
        dbgdump('ye0', ye_d[0])

        if stop >= 8:
          with ExitStack() as ph:
            gt_ = [sb(ph, "gt%d" % i, [128, D]) for i in range(4)]
            acc = [sb(ph, "acc%d" % i, [128, D]) for i in range(2)]
            accb = [sb(ph, "accb%d" % i, [128, D], BF16) for i in range(2)]
            gc = 0
            breg7 = nc.gpsimd.to_reg(CAP - 1)
            for i in range(4):
                S.op('pool', lambda e: e.memset(gt_[i][:], 0.0), writes=['gt%d' % i])
            for blk in range(64):
                ab, ak = acc[blk % 2], 'acc%d' % (blk % 2)
                tb7 = ((blk % 32) // 8) * 16 + (blk // 32) * 8 + (blk % 8)
                for el in range(4):
                    g_, gk = gt_[gc % 4], 'gt%d' % (gc % 4)
                    gc += 1
                    S.dma('pool', lambda e: e.indirect_dma_start(out=g_[:, :], out_offset=None, in_=ye_d[el],
                                                                in_offset=bass.IndirectOffsetOnAxis(ap=si[el][:, tb7:tb7 + 1], axis=0),
                                                                bounds_check=breg7, oob_is_err=False),
                          reads=['si%d' % el], writes=[gk])
                    if el == 0:
                        S.op('dve', lambda e: e.tensor_scalar(out=ab[:], in0=g_[:], scalar1=wpc[el][:, tb7:tb7 + 1], scalar2=None, op0=ALU.mult),
                             reads=[gk, 'wpc%d' % el], writes=[ak])
                    elif el < 3:
                        S.op('dve', lambda e: e.scalar_tensor_tensor(out=ab[:], in0=g_[:], scalar=wpc[el][:, tb7:tb7 + 1], in1=ab[:], op0=ALU.mult, op1=ALU.add),
                             reads=[gk, 'wpc%d' % el, ak], writes=[ak])
                    else:
                        S.op('dve', lambda e: e.scalar_tensor_tensor(out=accb[blk % 2][:], in0=g_[:], scalar=wpc[el][:, tb7:tb7 + 1], in1=ab[:],
                                                                     op0=ALU.mult, op1=ALU.add),
                             reads=[gk, 'wpc%d' % el, ak], writes=['accb%d' % (blk % 2)])
                S.dma('sp', lambda e: e.dma_start(out=op_d[blk * 128:(blk + 1) * 128, :], in_=accb[blk % 2][:]), reads=['accb%d' % (blk % 2)],
                      writes=['op_b%d' % blk])
                if blk % 32 == 31:
                    c_ = blk // 32
                    S.collective("ReduceScatter", ALU.add, [op_d[c_ * 4096:(c_ + 1) * 4096, :]], [moe_d[c_ * 1024:(c_ + 1) * 1024, :]],
                                 reads=['op_b%d' % bb for bb in range(blk - 31, blk + 1)], writes=['moe%d' % c_])
            S.barrier()
        dbgdump('moe', moe_d)

        if stop >= 9:
          with ExitStack() as ph:
            a8 = [sb(ph, "a8%d" % i, [128, D]) for i in range(2)]
            m8 = [sb(ph, "m8%d" % i, [128, D], BF16) for i in range(2)]
            m8f = [sb(ph, "m8f%d" % i, [128, D]) for i in range(2)]
            o8 = [sb(ph, "o8%d" % i, [128, D]) for i in range(2)]
            junk8 = sb(ph, "junk8", [128, D])
            st8 = sb(ph, "st8", [128, 8])
            for i in range(16):
                ts_ = slice(i * 128, (i + 1) * 128)
                a_, ak = a8[i % 2], 'a8%d' % (i % 2)
                m_, mk_ = m8[i % 2], 'm8%d' % (i % 2)
                o_, ok_ = o8[i % 2], 'o8%d' % (i % 2)
                S.dma('sp', lambda e: e.dma_start(out=a_[:], in_=x1_d[ts_, :]), writes=[ak])
                S.dma('act', lambda e: e.dma_start(out=m_[:], in_=moe_d[ts_, :]), reads=['moe%d' % (i // 8)], writes=[mk_])
                mf_, mfk = m8f[i % 2], 'm8f%d' % (i % 2)
                S.op('dve', lambda e: e.tensor_tensor(out=mf_[:], in0=m_[:], in1=rows[:, 3072:4096], op=ALU.mult), reads=[mk_, 'rows'], writes=[mfk])
                S.op('dve', lambda e: e.tensor_tensor(out=a_[:], in0=a_[:], in1=mf_[:], op=ALU.add), reads=[ak, mfk], writes=[ak])
                S.op('act', lambda e: e.activation(out=junk8[:], in_=a_[:], func=AF.Square, accum_out=st8[:, 0:1]), reads=[ak], writes=['junk8', 'st8'])
                S.op('act', lambda e: e.activation(out=st8[:, 1:2], in_=st8[:, 0:1], func=AF.Ln, scale=1.0 / D, bias=EPS), reads=['st8'], writes=['st8'])
                S.op('act', lambda e: e.activation(out=st8[:, 2:3], in_=st8[:, 1:2], func=AF.Exp, scale=-0.5), reads=['st8'], writes=['st8'])
                S.op('dve', lambda e: e.scalar_tensor_tensor(out=o_[:], in0=a_[:], scalar=st8[:, 2:3], in1=PP('gfin_row'), op0=ALU.mult, op1=ALU.mult),
                     reads=[ak, 'st8', 'pp'], writes=[ok_])
                S.dma('sp', lambda e: e.dma_start(out=out_d[ts_, :], in_=o_[:]), reads=[ok_], writes=['out'])
        S.barrier(cc=True)
    return nc


def _make_inputs(x, c, ctx, c_ctx, w_ada, b_ada, g_mix, w_in, w_fourier, lb_logits, g_rec, w_out,
                 g_ffn, w_router, w_exp_gate, w_exp_up, w_exp_down, g_final):
    f = lambda a: np.ascontiguousarray(np.asarray(a, dtype=np.float32))
    x, c, ctx, c_ctx = f(x), f(c), f(ctx), f(c_ctx)
    w_ada, b_ada, g_mix, w_in = f(w_ada)[0], f(b_ada)[0], f(g_mix)[0], f(w_in)[0]
    w_fourier, lb_logits, g_rec, w_out = f(w_fourier)[0], f(lb_logits), f(g_rec)[0], f(w_out)[0]
    g_ffn, w_router = f(g_ffn)[0], f(w_router)[0]
    weg, weu, wed, g_final = f(w_exp_gate)[0], f(w_exp_up)[0], f(w_exp_down)[0], f(g_final)
    perm = np.concatenate([np.concatenate([np.arange(r * 128, (r + 1) * 128), 512 + np.arange(r * 128, (r + 1) * 128)]) for r in range(4)])
    w_out_p = np.ascontiguousarray(w_out[perm, :])
    in_maps = []
    for core in range(8):
        b, j = core // 4, core % 4
        cols = np.concatenate([np.arange(j * 128, (j + 1) * 128)] + [512 + i * 512 + np.arange(j * 128, (j + 1) * 128) for i in range(5)])
        pk = np.zeros((128, PW), np.float32)

        def put(name, arr):
            o, w = PLAY[name]
            pk[:, o:o + w] = arr.reshape(128, w)
        cc = np.stack([c[b], c_ctx], 0)
        put('ccT', cc.reshape(2, 8, 128).transpose(2, 1, 0))
        put('bada_fm', b_ada[:2048].reshape(16, 128).T)
        put('gmix_fm', g_mix.reshape(8, 128).T)
        put('grec_fm', g_rec[j * 128:(j + 1) * 128].reshape(128, 1))
        lbl = lb_logits[:, :, j * 128:(j + 1) * 128]
        put('lbl_fm', lbl.reshape(4, 128).T)
        put('lbl_row', np.broadcast_to(lbl.reshape(1, 512), (128, 512)))
        put('wr', w_router.reshape(8, 128, 16).transpose(1, 0, 2))
        put('gffn_row', np.broadcast_to(g_ffn.reshape(1, 1024), (128, 1024)))
        put('gfin_row', np.broadcast_to(g_final.reshape(1, 1024), (128, 1024)))
        feat = np.arange(1024)
        r_, fh_, p_ = feat // 256, (feat // 128) % 2, feat % 128
        rowidx = (fh_ * 2 + j // 2) * 1024 + r_ * 256 + (j % 2) * 128 + p_
        idx_mix = np.ascontiguousarray(rowidx.reshape(8, 128).T.astype(np.int32))
        idx_mk = np.zeros((64, 4), np.int32)
        for el in range(4):
            e_ = 4 * j + el
            cidx = np.arange(64)
            rr, hh, cc_ = cidx // 16, (cidx // 8) % 2, cidx % 8
            idx_mk[:, el] = (rr * 32 + e_ * 2 + hh) * 8 + cc_
        in_maps.append({
            "x_b": x[b], "x_own": np.ascontiguousarray(x[b, j * 2048:(j + 1) * 2048]), "ctx_b": ctx[b],
            "w_ada": w_ada, "w_in": np.ascontiguousarray(w_in[:, cols]), "w_f": w_fourier[j],
            "w_out": w_out_p, "weg": np.ascontiguousarray(weg[4 * j:4 * j + 4]), "weu": np.ascontiguousarray(weu[4 * j:4 * j + 4]),
            "wed": np.ascontiguousarray(wed[4 * j:4 * j + 4]), "cpack": CPACK, "ppack": pk,
            "bada_rows": np.ascontiguousarray(np.broadcast_to(b_ada[2048:].reshape(1, 4096), (128, 4096))),
            "idx_mix": idx_mix, "idx_mk": idx_mk,
        })
    return in_maps


def kernel(**inputs):
    in_maps = _make_inputs(**inputs)
    nc = build()
    res = run_bass_kernel_spmd(nc, in_maps, core_ids=list(range(8)))
    out = np.zeros((2, L, D), np.float32)
    for core in range(8):
        b, j = core // 4, core % 4
        out[b, j * 2048:(j + 1) * 2048] = res.results[core]["out"]
    return out
```

```python
import os
import numpy as np
from contextlib import ExitStack
import concourse.bass as bass
import concourse.mybir as mybir
from concourse.bass_utils import run_bass_kernel_spmd

F32 = mybir.dt.float32
I32 = mybir.dt.int32
BF16 = mybir.dt.bfloat16
AF = mybir.ActivationFunctionType
ALU = mybir.AluOpType
AX = mybir.AxisListType

D = 1024
L = 8192
LC = 256
TT = LC + L
NE = 16
CAP = 1024
DE = 2816
NFC = DE // 128
EPS = 1e-6
GROUPS = [[0, 1, 2, 3], [4, 5, 6, 7]]


class Sched:
    CE = ('pe', 'dve', 'act', 'pool')

    def __init__(self, nc, stack, ndma=8):
        self.nc = nc
        self.e = dict(pe=nc.tensor, dve=nc.vector, act=nc.scalar, pool=nc.gpsimd, sp=nc.sync)
        self.csem = {k: stack.enter_context(nc.semaphore('cs_' + k)) for k in self.CE}
        self.ccnt = {k: 0 for k in self.CE}
        self.dsem = {q: [stack.enter_context(nc.semaphore('ds_%s%d' % (q, i))) for i in range(ndma)]
                     for q in ('sp', 'pool', 'act')}
        self.dcnt = {q: [0] * ndma for q in self.dsem}
        self.dnext = {q: 0 for q in self.dsem}
        self.ndma = ndma
        self.seen = {k: {} for k in self.e}
        self.lastw = {}
        self.readers = {}
        self.ccsem = stack.enter_context(nc.semaphore('ccsem'))
        self.cccnt = 0
        self.n = 0

    def _wait(self, eng, ev):
        sem, val, src = ev
        d = self.seen[eng]
        if d.get(sem, 0) >= val:
            return
        self.e[eng].wait_ge(sem, val)
        d[sem] = val

    def _deps(self, eng, reads, writes, skip_same_waw):
        deps = []
        for k in reads:
            w = self.lastw.get(k)
            if isinstance(w, list):
                deps.extend(w)
            elif w is not None:
                deps.append(w)
            if isinstance(k, str) and len(k) == 2 and k[0] == 'P' and k[1].isdigit():
                for sem, (val, src) in self.readers.get(k, {}).items():
                    if src != eng:
                        deps.append((sem, val, src))
        for k in writes:
            w = self.lastw.get(k)
            if isinstance(w, list):
                deps.extend(w)
            elif w is not None and not (skip_same_waw and w[2] == eng):
                deps.append(w)
            for sem, (val, src) in self.readers.get(k, {}).items():
                deps.append((sem, val, src))
        for ev in deps:
            self._wait(eng, ev)

    def _record(self, ev, reads, writes):
        sem, val, src = ev
        for k in reads:
            self.readers.setdefault(k, {})[sem] = (val, src)
        for k in writes:
            self.lastw[k] = ev
            self.readers[k] = {}

    def op(self, eng, fn, reads=(), writes=(), acc=False):
        self._deps(eng, reads, writes, acc)
        ins = fn(self.e[eng])
        self.ccnt[eng] += 1
        ins.then_inc(self.csem[eng], 1)
        ev = (self.csem[eng], self.ccnt[eng], eng)
        self._record(ev, reads, writes)
        self.n += 1
        return ev

    def dma(self, q, fn, reads=(), writes=()):
        i = self.dnext[q]
        self.dnext[q] = (i + 1) % self.ndma
        sem = self.dsem[q][i]
        if self.dcnt[q][i] > 0:
            self._wait(q, (sem, self.dcnt[q][i], 'dma'))
        self._deps(q, reads, writes, False)
        ins = fn(self.e[q])
        ins.then_inc(sem, 16)
        self.dcnt[q][i] += 16
        ev = (sem, self.dcnt[q][i], 'dma')
        self._record(ev, reads, writes)
        self.n += 1
        return ev

    def fence(self, key, q):
        self.lastw[key] = [(self.dsem[q][i], self.dcnt[q][i], 'dma') for i in range(self.ndma) if self.dcnt[q][i] > 0]
        self.readers[key] = {}

    def collective(self, kind, op, ins, outs, reads=(), writes=()):
        if self.cccnt > 0:
            self._wait('pool', (self.ccsem, self.cccnt, 'cc'))
        self._deps('pool', reads, writes, False)
        ins_ = self.nc.gpsimd.collective_compute(kind, op, replica_groups=GROUPS, ins=ins, outs=outs)
        ins_.then_inc(self.ccsem, 1)
        self.cccnt += 1
        ev = (self.ccsem, self.cccnt, 'cc')
        self._record(ev, reads, writes)
        return ev

    def barrier(self, cc=False):
        evs = [(self.csem[k], self.ccnt[k], k) for k in self.CE if self.ccnt[k] > 0]
        for q in self.dsem:
            for i in range(self.ndma):
                if self.dcnt[q][i] > 0:
                    evs.append((self.dsem[q][i], self.dcnt[q][i], 'dma'))
        if self.cccnt and cc:
            evs.append((self.ccsem, self.cccnt, 'cc'))
        for eng in self.e:
            for ev in evs:
                self._wait(eng, ev)
        keepw = {k: v for k, v in self.lastw.items() if not isinstance(v, list) and v[2] == 'cc'}
        keepr = {k: {sm: vv for sm, vv in d.items() if vv[1] == 'cc'} for k, d in self.readers.items()}
        self.lastw = keepw if not cc else {}
        self.readers = {k: d for k, d in keepr.items() if d} if not cc else {}


def _const_tables():
    c = {}
    i128 = np.arange(128)
    i64 = np.arange(64)
    c['ident'] = np.eye(128)
    a = 2 * np.pi * np.outer(i128, i128) / 128.0
    c['C128'] = np.cos(a)
    c['S128'] = np.sin(a)
    c['nS128'] = -np.sin(a)
    c['CCs'] = np.cos(a) / 1024.0
    c['SCs'] = np.sin(a) / 1024.0
    tw = 2 * np.pi * np.outer(i128, i64) / 8192.0
    c['Tc'] = np.cos(tw)
    c['Ts'] = np.sin(tw)
    a64 = 2 * np.pi * np.outer(i64, i64) / 64.0
    c['C64'] = np.cos(a64)
    c['nS64'] = -np.sin(a64)
    s = i64[:, None]
    t = i64[None, :]
    c['TriF'] = (s <= t) * 1.0
    c['TriB'] = (s >= t) * 1.0
    c['SLoF'] = (s > t) * 1.0
    c['SLoB'] = (s < t) * 1.0
    c['SU64'] = (s < t) * 1.0
    c['ones16'] = np.ones((16, 16))
    p = i128[:, None]
    q = i128[None, :]
    c['L128'] = (p < q) * 1.0
    c['avg128'] = np.ones((128, 128)) / 128.0
    c['one128'] = np.ones((128, 128))
    eq = (i128 // 2) % 16
    c['G'] = (eq[:, None] == eq[None, :]) * 1.0
    lay = {}
    off = 0
    for k, v in c.items():
        lay[k] = (off, v.shape[0], v.shape[1])
        off += v.shape[1]
    pack = np.zeros((128, off), np.float32)
    for k, v in c.items():
        o, r, w = lay[k]
        pack[:r, o:o + w] = v.astype(np.float32)
    return pack, lay


CPACK, CLAY = _const_tables()

PLAY = {}


def _play():
    off = 0
    for name, w in [('ccT', 16), ('bada_fm', 16), ('gmix_fm', 8), ('grec_fm', 1), ('lbl_fm', 4),
                    ('lbl_row', 512), ('wr', 128), ('gffn_row', 1024), ('gfin_row', 1024)]:
        PLAY[name] = (off, w)
        off += w
    return off


PW = _play()


def build(dbg=None):
    nc = bass.Bass("TRN2", target_bir_lowering=False)

    def din(name, shape, dt=F32):
        return nc.dram_tensor(name, list(shape), dt, kind="ExternalInput").ap()

    x_b = din("x_b", [L, D])
    x_own = din("x_own", [2048, D])
    ctx_b = din("ctx_b", [LC, D])
    w_ada = din("w_ada", [D, 6 * D])
    w_in = din("w_in", [D, 768])
    w_f = din("w_f", [128, 128])
    w_out = din("w_out", [D, D])
    weg = din("weg", [4, D, DE])
    weu = din("weu", [4, D, DE])
    wed = din("wed", [4, DE, D])
    cpack_d = din("cpack", list(CPACK.shape))
    ppack_d = din("ppack", [128, PW])
    bada_rows_d = din("bada_rows", [128, 4096])
    idx_mix_d = din("idx_mix", [128, 8], I32)
    idx_mk_d = din("idx_mk", [64, 4], I32)
    out_d = nc.dram_tensor("out", [2048, D], F32, kind="ExternalOutput").ap()

    def dscr(name, shape, dt=F32):
        return nc.dram_tensor(name, list(shape), dt).ap()

    def ag_mix(i, keys):
        S.collective("AllGather", ALU.bypass, [mix_d[i * 256:(i + 1) * 256, :]], [mixall_d[i * 1024:(i + 1) * 1024, :]],
                     reads=keys, writes=['mixall'])

    uT_d = dscr("uT_d", [128, L])
    qT_d = dscr("qT_d", [128, L])
    sgT_d = dscr("sgT_d", [128, L])
    kT_d = [dscr("kfT_d", [128, TT]), dscr("kbT_d", [128, TT])]
    v_d = dscr("v_d", [TT, 128], BF16)
    k_d = [dscr("kf_d", [TT, 128]), dscr("kb_d", [TT, 128])]
    lf_d = [dscr("lff_d", [TT, 128]), dscr("lfb_d", [TT, 128])]
    Y_d = dscr("Y_d", [2, 128, 64, 128])
    mix_d = dscr("mix_d", [4 * 256, 2048], BF16)
    mixall_d = dscr("mixall_d", [16 * 256, 2048], BF16)
    x1_d = dscr("x1_d", [2048, D])
    h2_d = dscr("h2_d", [2048, D], BF16)
    h2all_d = dscr("h2all_d", [L, D], BF16)
    affT_d = dscr("affT_d", [16, 2048])
    affall_d = dscr("affall_d", [64, 2048])
    mk_d = dscr("mk_d", [128, 1024])
    wg_d = dscr("wg_d", [128, 1024])
    xs_d = [dscr("xs_d%d" % i, [CAP, D], BF16) for i in range(4)]
    ye_d = [dscr("ye_d%d" % i, [CAP, D]) for i in range(4)]
    op_d = dscr("op_d", [L, D], BF16)
    moe_d = dscr("moe_d", [2048, D], BF16)

    dbg_out = {}
    if dbg:
        for name, shape in dbg.items():
            if name.startswith('_'):
                continue
            dbg_out[name] = nc.dram_tensor("dbg_" + name, list(shape), F32, kind="ExternalOutput").ap()

    with ExitStack() as top:
        S = Sched(nc, top)
        sb = lambda st, name, shape, dt=F32: st.enter_context(nc.sbuf_tensor(name, list(shape), dt))
        P = [top.enter_context(nc.psum_tensor("P%d" % i, [128, 512], F32)) for i in range(8)]
        PK = ['P%d' % i for i in range(8)]
        cp = sb(top, "cpack_sb", CPACK.shape)
        pp = sb(top, "ppack_sb", [128, PW])
        S.dma('sp', lambda e: e.dma_start(out=cp[:], in_=cpack_d), writes=['cp'])
        S.dma('sp', lambda e: e.dma_start(out=pp[:], in_=ppack_d), writes=['pp'])

        def C(name, rows=None):
            o, r, w = CLAY[name]
            return cp[0:(rows or r), o:o + w]

        def PP(name, a=0, b=None):
            o, w = PLAY[name]
            return pp[:, o + a:o + (w if b is None else b)]

        ident = C('ident')
        modfm = sb(top, "modfm", [128, 16, 2])
        A1 = sb(top, "A1", [128, 8, 2])
        rows = sb(top, "rows", [128, 4096])
        lbs = sb(top, "lbs", [128, 4])
        omlrow = sb(top, "omlrow", [128, 256])
        scB = sb(top, "scB", [128, 8, 128])

        with ExitStack() as ph:
            scT = sb(ph, "scT", [128, 16])
            wa = [sb(ph, "wa%d" % i, [128, 8, 512]) for i in range(2)]
            S.op('act', lambda e: e.activation(out=scT[:], in_=PP('ccT'), func=AF.Exp, scale=-1.0), reads=['pp'], writes=['scT'])
            S.op('dve', lambda e: e.tensor_scalar(out=scT[:], in0=scT[:], scalar1=1.0, scalar2=None, op0=ALU.add), reads=['scT'], writes=['scT'])
            S.op('dve', lambda e: e.reciprocal(out=scT[:], in_=scT[:]), reads=['scT'], writes=['scT'])
            S.op('dve', lambda e: e.tensor_tensor(out=scT[:], in0=scT[:], in1=PP('ccT'), op=ALU.mult), reads=['scT', 'pp'], writes=['scT'])
            for k in range(8):
                S.op('dve', lambda e, k=k: e.tensor_scalar(out=scB[:, k, :], in0=C('one128'), scalar1=scT[:, 2 * k:2 * k + 1],
                                                            scalar2=None, op0=ALU.mult),
                     reads=['scT', 'cp'], writes=['scB'])
            w_ada_v = w_ada.rearrange("(k p) n -> p k n", p=128)
            for cb in range(4):
                wt = wa[cb % 2]
                wk = 'wa%d' % (cb % 2)
                S.dma('sp' if cb % 2 == 0 else 'act', lambda e, wt=wt, cb=cb: e.dma_start(out=wt[:], in_=w_ada_v[:, :, cb * 512:(cb + 1) * 512]),
                      writes=[wk])
                if cb < 4:
                    for bl in range(4):
                        blk = cb * 4 + bl
                        for k in range(8):
                            S.op('pe', lambda e, k=k, bl=bl, blk=blk, wt=wt: e.matmul(
                                P[0][:, blk * 2:blk * 2 + 2], lhsT=wt[:, k, bl * 128:(bl + 1) * 128],
                                rhs=scT[:, 2 * k:2 * k + 2], start=(k == 0), stop=(k == 7)),
                                reads=[wk, 'scT'], writes=['P0'], acc=True)
            for n in range(2):
                S.op('dve', lambda e, n=n: e.tensor_tensor(out=modfm[:, :, n], in0=P[0][:, 0:32].rearrange("p (b n) -> p b n", n=2)[:, :, n],
                                                           in1=PP('bada_fm'), op=ALU.add),
                     reads=['P0', 'pp'], writes=['modfm'])
            for n in range(2):
                S.op('dve', lambda e, n=n: e.scalar_tensor_tensor(out=A1[:, :, n], in0=modfm[:, 8:16, n], scalar=1.0, in1=PP('gmix_fm'),
                                                                  op0=ALU.add, op1=ALU.mult),
                     reads=['modfm', 'pp'], writes=['A1'])
            S.op('dve', lambda e: e.tensor_tensor(out=lbs[:, 2:4], in0=PP('lbl_fm', 2, 4), in1=PP('lbl_fm', 0, 2), op=ALU.subtract),
                 reads=['pp'], writes=['lbs'])
            S.op('act', lambda e: e.activation(out=lbs[:, 0:2], in_=lbs[:, 2:4], func=AF.Exp, scale=-1.0), reads=['lbs'], writes=['lbs'])
            S.op('dve', lambda e: e.tensor_scalar(out=lbs[:, 0:2], in0=lbs[:, 0:2], scalar1=1.0, scalar2=None, op0=ALU.add), reads=['lbs'], writes=['lbs'])
            S.op('dve', lambda e: e.tensor_tensor(out=omlrow[:], in0=PP('lbl_row', 256, 512), in1=PP('lbl_row', 0, 256), op=ALU.subtract),
                 reads=['pp'], writes=['omlrow'])
            S.op('act', lambda e: e.activation(out=omlrow[:], in_=omlrow[:], func=AF.Exp, scale=-1.0), reads=['omlrow'], writes=['omlrow'])
            S.op('dve', lambda e: e.tensor_scalar(out=omlrow[:], in0=omlrow[:], scalar1=1.0, scalar2=None, op0=ALU.add), reads=['omlrow'], writes=['omlrow'])
            if dbg and 'modfm' in dbg:
                S.dma('sp', lambda e: e.dma_start(out=dbg_out['modfm'], in_=modfm[:].rearrange("p a b -> p (a b)")), reads=['modfm'])
            S.barrier()

        with ExitStack() as ph:
            win = sb(ph, "win", [128, 8, 768], BF16)
            with ExitStack() as ph0:
                winf = sb(ph0, "winf", [128, 8, 768])
                S.dma('act', lambda e: e.dma_start(out=winf[:], in_=w_in.rearrange("(k p) n -> p k n", p=128)), writes=['winf'])
                for k in range(8):
                    if k % 2 == 0:
                        S.op('act', lambda e: e.copy(out=win[:, k, :], in_=winf[:, k, :]), reads=['winf'], writes=['win'])
                    else:
                        S.op('dve', lambda e: e.tensor_copy(out=win[:, k, :], in_=winf[:, k, :]), reads=['winf'], writes=['win'])
                S.barrier()
            xt = [sb(ph, "xt%d" % i, [128, D]) for i in range(4)]
            xn = [sb(ph, "xn%d" % i, [128, D], BF16) for i in range(2)]
            identb1 = sb(ph, "identb1", [128, 128], BF16)
            S.op('dve', lambda e: e.tensor_copy(out=identb1[:], in_=ident), reads=['cp'], writes=['identb1'])
            junk = sb(ph, "junk", [128, D])
            hT = [sb(ph, "hT%d" % i, [128, 8, 512], BF16) for i in range(2)]
            ofm = [sb(ph, "ofm%d" % i, [128, 512]) for i in range(4)]
            otm = [sb(ph, "otm%d" % i, [128, 128], BF16) for i in range(6)]
            ofm_i = [0]
            otm_i = [0]
            tcount = [0]

            st4 = sb(ph, "st4r", [128, 4, 4])
            otw = [sb(ph, "otw%d" % i, [128, 256]) for i in range(4)]
            otw_i = [0]

            def stageA(gi, src, ntile, n):
                hb = hT[gi % 2]
                hk = 'hT%d' % (gi % 2)
                for i in range(ntile):
                    ti = tcount[0]
                    tcount[0] += 1
                    xb, xk = xt[ti % 4], 'xt%d' % (ti % 4)
                    nb, nk = xn[ti % 2], 'xn%d' % (ti % 2)
                    sk = 'st4_%d' % i
                    S.dma('sp', lambda e: e.dma_start(out=xb[:], in_=src[i * 128:(i + 1) * 128, :]), writes=[xk])
                    S.op('act', lambda e: e.activation(out=junk[:], in_=xb[:], func=AF.Square, accum_out=st4[:, i, 0:1]),
                         reads=[xk], writes=['junk', sk])
                    S.op('act', lambda e: e.activation(out=st4[:, i, 1:2], in_=st4[:, i, 0:1], func=AF.Ln, scale=1.0 / D, bias=EPS),
                         reads=[sk], writes=[sk])
                    S.op('act', lambda e: e.activation(out=st4[:, i, 2:3], in_=st4[:, i, 1:2], func=AF.Exp, scale=-0.5),
                         reads=[sk], writes=[sk])
                    S.op('dve', lambda e: e.tensor_scalar(out=nb[:], in0=xb[:], scalar1=st4[:, i, 2:3], scalar2=None, op0=ALU.mult),
                         reads=[xk, sk], writes=[nk])
                    for half in range(2):
                        bi_ = half + 6 * (i % 2)
                        pb, pk = P[bi_][:, :].bitcast(BF16), PK[bi_]
                        for kk in range(4):
                            k = half * 4 + kk
                            S.op('pe', lambda e: e.transpose(pb[:, kk * 128:(kk + 1) * 128], nb[:, k * 128:(k + 1) * 128], identb1[:]),
                                 reads=[nk, 'identb1'], writes=[pk], acc=True)
                        for kk in range(4):
                            k = half * 4 + kk
                            if half == 0:
                                S.op('dve', lambda e: e.tensor_scalar(
                                    out=hb[:, k, i * 128:(i + 1) * 128], in0=pb[:, kk * 128:(kk + 1) * 128],
                                    scalar1=A1[:, k, n:n + 1], scalar2=modfm[:, k, n:n + 1], op0=ALU.mult, op1=ALU.add),
                                    reads=[pk, 'A1', 'modfm'], writes=[(hk, k)])
                            else:
                                S.op('act', lambda e: e.activation(
                                    out=hb[:, k, i * 128:(i + 1) * 128], in_=pb[:, kk * 128:(kk + 1) * 128], func=AF.Identity,
                                    scale=A1[:, k, n:n + 1], bias=modfm[:, k, n:n + 1]),
                                    reads=[pk, 'A1', 'modfm'], writes=[(hk, k)])
                    yield

            def stageB(gi, ntile, col0, latent):
                hb = hT[gi % 2]
                hk = 'hT%d' % (gi % 2)
                ncol = ntile * 128
                blks = [0, 1, 3, 4, 5] if latent else [3, 4]
                for bi, blk in enumerate(blks):
                    pb, pk = P[2 + bi % 2], PK[2 + bi % 2]
                    for k in range(8):
                        S.op('pe', lambda e: e.matmul(pb[:, 0:ncol], lhsT=win[:, k, blk * 128:(blk + 1) * 128],
                                                      rhs=hb[:, k, 0:ncol], start=(k == 0), stop=(k == 7)),
                             reads=['win', (hk, k)], writes=[pk], acc=True)
                    ob, ok = ofm[ofm_i[0] % 4], 'ofm%d' % (ofm_i[0] % 4)
                    ofm_i[0] += 1
                    if blk == 0:
                        S.op('act', lambda e: e.copy(out=ob[:, 0:ncol], in_=pb[:, 0:ncol]), reads=[pk], writes=[ok])
                        dst = uT_d[:, col0 - LC:col0 - LC + ncol]
                    elif blk in (1, 5):
                        S.op('act', lambda e: e.activation(out=ob[:, 0:ncol], in_=pb[:, 0:ncol], func=AF.Exp, scale=-1.0), reads=[pk], writes=[ok])
                        S.op('dve', lambda e: e.tensor_scalar(out=ob[:, 0:ncol], in0=ob[:, 0:ncol], scalar1=1.0, scalar2=None, op0=ALU.add),
                             reads=[ok], writes=[ok])
                        S.op('act', lambda e: e.activation(out=ob[:, 0:ncol], in_=ob[:, 0:ncol], func=AF.Ln), reads=[ok], writes=[ok])
                        S.op('act', lambda e: e.activation(out=ob[:, 0:ncol], in_=ob[:, 0:ncol], func=AF.Exp, scale=-1.0), reads=[ok], writes=[ok])
                        S.op('dve', lambda e: e.tensor_tensor(out=ob[:, 0:ncol], in0=ob[:, 0:ncol], in1=pb[:, 0:ncol], op=ALU.mult),
                             reads=[ok, pk], writes=[ok])
                        dst = (qT_d if blk == 1 else sgT_d)[:, col0 - LC:col0 - LC + ncol]
                    else:
                        d_ = blk - 3
                        S.op('act', lambda e: e.activation(out=ob[:, 0:ncol], in_=pb[:, 0:ncol], func=AF.Exp), reads=[pk], writes=[ok])
                        S.op('dve', lambda e: e.tensor_scalar(out=ob[:, 0:ncol], in0=ob[:, 0:ncol], scalar1=lbs[:, d_:d_ + 1], scalar2=lbs[:, d_:d_ + 1],
                                                              op0=ALU.mult, op1=ALU.add),
                             reads=[ok, 'lbs'], writes=[ok])
                        S.op('act', lambda e: e.activation(out=ob[:, 0:ncol], in_=ob[:, 0:ncol], func=AF.Ln), reads=[ok], writes=[ok])
                        S.op('act', lambda e: e.activation(out=ob[:, 0:ncol], in_=ob[:, 0:ncol], func=AF.Exp, scale=-1.0), reads=[ok], writes=[ok])
                        dst = kT_d[d_][:, col0:col0 + ncol]
                    S.dma('pool', lambda e: e.dma_start(out=dst, in_=ob[:, 0:ncol]), reads=[ok])
                    yield
                for i in range(ntile):
                    pb, pk = P[4 + i % 2], PK[4 + i % 2]
                    for k in range(8):
                        S.op('pe', lambda e: e.matmul(pb[:, 0:384], lhsT=hb[:, k, i * 128:(i + 1) * 128], rhs=win[:, k, 256:640],
                                                      start=(k == 0), stop=(k == 7)),
                             reads=['win', (hk, k)], writes=[pk], acc=True)
                    r0 = col0 + i * 128
                    ob, ok = otm[otm_i[0] % 6], 'otm%d' % (otm_i[0] % 6)
                    otm_i[0] += 1
                    S.op('act', lambda e: e.copy(out=ob[:], in_=pb[:, 0:128]), reads=[pk], writes=[ok])
                    S.dma('pool', lambda e: e.dma_start(out=v_d[r0:r0 + 128, :], in_=ob[:]), reads=[ok])
                    kb, kk_ = otw[otw_i[0] % 4], 'otw%d' % (otw_i[0] % 4)
                    lb_, lk_ = otw[(otw_i[0] + 1) % 4], 'otw%d' % ((otw_i[0] + 1) % 4)
                    otw_i[0] += 2
                    S.op('act', lambda e: e.activation(out=kb[:], in_=pb[:, 128:384], func=AF.Exp), reads=[pk], writes=[kk_])
                    S.op('dve', lambda e: e.scalar_tensor_tensor(out=kb[:], in0=kb[:], scalar=1.0, in1=omlrow[:], op0=ALU.add, op1=ALU.mult),
                         reads=[kk_, 'omlrow'], writes=[kk_])
                    S.op('act', lambda e: e.activation(out=kb[:], in_=kb[:], func=AF.Ln), reads=[kk_], writes=[kk_])
                    S.op('act', lambda e: e.activation(out=kb[:], in_=kb[:], func=AF.Exp, scale=-1.0), reads=[kk_], writes=[kk_])
                    S.op('act', lambda e: e.activation(out=lb_[:], in_=kb[:], func=AF.Ln, scale=-1.0, bias=1.0), reads=[kk_], writes=[lk_])
                    for d_ in range(2):
                        S.dma('pool', lambda e: e.dma_start(out=k_d[d_][r0:r0 + 128, :], in_=kb[:, d_ * 128:(d_ + 1) * 128]), reads=[kk_])
                        S.dma('pool', lambda e: e.dma_start(out=lf_d[d_][r0:r0 + 128, :], in_=lb_[:, d_ * 128:(d_ + 1) * 128]), reads=[lk_])
                    yield

            ngl = 16 if not (dbg and dbg.get('_short')) else 1
            plan = [(ctx_b, 2, 1, 0, False)] + [(x_b[g * 512:(g + 1) * 512, :], 4, 0, LC + g * 512, True) for g in range(ngl)]
            def drain(gen, n=10 ** 9):
                for _ in range(n):
                    try:
                        next(gen)
                    except StopIteration:
                        return True
                return False

            drain(stageA(0, plan[0][0], plan[0][1], plan[0][2]))
            for gi in range(len(plan)):
                ga = stageA(gi + 1, plan[gi + 1][0], plan[gi + 1][1], plan[gi + 1][2]) if gi + 1 < len(plan) else iter(())
                gb = stageB(gi, plan[gi][1], plan[gi][3], plan[gi][4])
                da = db = False
                while not (da and db):
                    if not da:
                        da = drain(ga, 1)
                    if not db:
                        db = drain(gb, 2)
            S.barrier()

        def dbgdump(name, src_ap, rows=None):
            if dbg and name in dbg:
                S.barrier(cc=True)
                S.dma('sp', lambda e: e.dma_start(out=dbg_out[name], in_=src_ap))
                S.barrier()

        stop = (dbg or {}).get('_stop', 99)
        if stop >= 2:
          with ExitStack() as ph:
            Xs = sb(ph, "Xs", [128, 64, 256])
            Wcat = sb(ph, "Wcat", [128, 256])
            wf = sb(ph, "wf", [128, 128])
            ft = [sb(ph, "ft%d" % i, [128, 512]) for i in range(6)]
            with ExitStack() as ph2:
                uT = sb(ph2, "uT", [128, L])
                for i in range(4):
                    S.dma('sp' if i % 2 == 0 else 'act', lambda e: e.dma_start(out=uT[:, i * 2048:(i + 1) * 2048], in_=uT_d[:, i * 2048:(i + 1) * 2048]),
                          writes=['uT'])
                S.dma('act', lambda e: e.dma_start(out=wf[:], in_=w_f), writes=['wf'])
                S.op('pe', lambda e: e.matmul(P[0][:, 0:128], lhsT=C('CCs'), rhs=wf[:], start=True, stop=True), reads=['cp', 'wf'], writes=['P0'], acc=True)
                S.op('pe', lambda e: e.matmul(P[0][:, 128:256], lhsT=C('SCs'), rhs=wf[:], start=True, stop=True), reads=['cp', 'wf'], writes=['P0'], acc=True)
                S.op('dve', lambda e: e.tensor_copy(out=Wcat[:], in_=P[0][:, 0:256]), reads=['P0'], writes=['Wcat'])
                uT3 = uT[:].rearrange("p (t1 t2) -> p t2 t1", t2=64)
                for t2p in range(32):
                    pb, pk = P[1 + t2p % 2], PK[1 + t2p % 2]
                    for h in range(2):
                        S.op('pe', lambda e: e.matmul(pb[:, h * 256:(h + 1) * 256], lhsT=uT3[:, 2 * t2p + h, :], rhs=Wcat[:], start=True, stop=True),
                             reads=['uT', 'Wcat'], writes=[pk], acc=True)
                    dst = Xs[:, 2 * t2p:2 * t2p + 2, :]
                    src = pb[:, 0:512].rearrange("p (a b) -> p a b", b=256)
                    if t2p % 2 == 0:
                        S.op('dve', lambda e: e.tensor_copy(out=dst, in_=src), reads=[pk], writes=['Xs0'])
                    else:
                        S.op('act', lambda e: e.copy(out=dst, in_=src), reads=[pk], writes=['Xs1'])
                S.barrier()
            yT = sb(ph, "yT", [128, L], BF16)
            for q in range(16):
                XA = Xs[:, 4 * q:4 * q + 4, 0:128]
                XB = Xs[:, 4 * q:4 * q + 4, 128:256]
                pr, prk = P[3 + (q % 2) * 2], PK[3 + (q % 2) * 2]
                pi, pik = P[4 + (q % 2) * 2], PK[4 + (q % 2) * 2]
                pr3 = pr[:, :].rearrange("p (a c) -> p a c", c=128)
                pi3 = pi[:, :].rearrange("p (a c) -> p a c", c=128)
                S.op('pe', lambda e: e.matmul(pr3, lhsT=C('C128'), rhs=XA, start=True, stop=False), reads=['cp', 'Xs0', 'Xs1'], writes=[prk], acc=True)
                S.op('pe', lambda e: e.matmul(pr3, lhsT=C('nS128'), rhs=XB, start=False, stop=True), reads=['cp', 'Xs0', 'Xs1'], writes=[prk], acc=True)
                S.op('pe', lambda e: e.matmul(pi3, lhsT=C('C128'), rhs=XB, start=True, stop=False), reads=['cp', 'Xs0', 'Xs1'], writes=[pik], acc=True)
                S.op('pe', lambda e: e.matmul(pi3, lhsT=C('S128'), rhs=XA, start=False, stop=True), reads=['cp', 'Xs0', 'Xs1'], writes=[pik], acc=True)
                Tcb = C('Tc')[:, 4 * q:4 * q + 4].unsqueeze(2).to_broadcast([128, 4, 128])
                Tsb = C('Ts')[:, 4 * q:4 * q + 4].unsqueeze(2).to_broadcast([128, 4, 128])
                f3 = [t[:].rearrange("p (a c) -> p a c", c=128) for t in ft]
                S.op('dve', lambda e: e.tensor_tensor(out=f3[0], in0=pr3, in1=Tcb, op=ALU.mult), reads=[prk, 'cp'], writes=['ft0'])
                S.op('dve', lambda e: e.tensor_tensor(out=f3[1], in0=pi3, in1=Tsb, op=ALU.mult), reads=[pik, 'cp'], writes=['ft1'])
                S.op('pool', lambda e: e.tensor_tensor(out=f3[2], in0=f3[0], in1=f3[1], op=ALU.subtract), reads=['ft0', 'ft1'], writes=['ft2'])
                S.op('dve', lambda e: e.tensor_tensor(out=f3[3], in0=pi3, in1=Tcb, op=ALU.mult), reads=[pik, 'cp'], writes=['ft3'])
                S.op('dve', lambda e: e.tensor_tensor(out=f3[4], in0=pr3, in1=Tsb, op=ALU.mult), reads=[prk, 'cp'], writes=['ft4'])
                S.op('pool', lambda e: e.tensor_tensor(out=f3[5], in0=f3[3], in1=f3[4], op=ALU.add), reads=['ft3', 'ft4'], writes=['ft5'])
                S.dma('pool', lambda e: e.dma_start(out=Y_d[0][:, 4 * q:4 * q + 4, :], in_=f3[2]), reads=['ft2'])
                S.dma('pool', lambda e: e.dma_start(out=Y_d[1][:, 4 * q:4 * q + 4, :], in_=f3[5]), reads=['ft5'])
            S.barrier()
            yr = [sb(ph, "yr%d" % i, [64, 8, 128]) for i in range(2)]
            yi = [sb(ph, "yi%d" % i, [64, 8, 128]) for i in range(2)]
            Yv = [Y_d[i].rearrange("k t c -> t k c") for i in range(2)]
            yTv = yT[:].rearrange("p (k2 k1) -> p k1 k2", k1=128)
            for g in range(16):
                a_, ak = yr[g % 2], 'yr%d' % (g % 2)
                b_, bk = yi[g % 2], 'yi%d' % (g % 2)
                S.dma('sp', lambda e: e.dma_start(out=a_[:], in_=Yv[0][:, 8 * g:8 * g + 8, :]), writes=[ak])
                S.dma('sp', lambda e: e.dma_start(out=b_[:], in_=Yv[1][:, 8 * g:8 * g + 8, :]), writes=[bk])
                pb, pk = P[g % 2], PK[g % 2]
                for kk in range(8):
                    S.op('pe', lambda e: e.matmul(pb[:, kk * 64:(kk + 1) * 64], lhsT=a_[:, kk, :], rhs=C('C64'), start=True, stop=False),
                         reads=[ak, 'cp'], writes=[pk], acc=True)
                    S.op('pe', lambda e: e.matmul(pb[:, kk * 64:(kk + 1) * 64], lhsT=b_[:, kk, :], rhs=C('nS64'), start=False, stop=True),
                         reads=[bk, 'cp'], writes=[pk], acc=True)
                k1a = 8 * g
                src = pb[:, 0:512].rearrange("p (a b) -> p a b", b=64)
                if g % 2 == 0:
                    S.op('dve', lambda e: e.tensor_copy(out=yTv[:, k1a:k1a + 8, :], in_=src), reads=[pk], writes=['yT0'])
                else:
                    S.op('act', lambda e: e.copy(out=yTv[:, k1a:k1a + 8, :], in_=src), reads=[pk], writes=['yT1'])
            for jj in range(4):
                S.dma('sp', lambda e: e.dma_start(out=mix_d[jj * 128:jj * 128 + 128, :], in_=yT[:, jj * 2048:(jj + 1) * 2048]), reads=['yT0', 'yT1'],
                      writes=['mixf%d' % jj])
            if stop >= 3.5:
                ag_mix(0, ['mixf0', 'mixf1'])
                ag_mix(1, ['mixf2', 'mixf3'])
            S.barrier()

        if stop >= 3:
          with ExitStack() as ph:
            oT = sb(ph, "oT", [128, L])
            Sst = [sb(ph, "Sst%d" % i, [128, 128]) for i in range(2)]
            qTg = [sb(ph, "qTg%d" % i, [128, 512]) for i in range(2)]
            kTg = [sb(ph, "kTg%d" % i, [128, 512]) for i in range(2)]
            vg = [sb(ph, "vg%d" % i, [64, 8, 128], BF16) for i in range(2)]
            Sb = [sb(ph, "Sb%d" % i, [128, 128], BF16) for i in range(2)]
            lfg = [sb(ph, "lfg%d" % i, [64, 8, 128]) for i in range(2)]
            ktg = [sb(ph, "ktg%d" % i, [64, 8, 128]) for i in range(2)]
            eB = [sb(ph, "eB%d" % i, [128, 512]) for i in range(2)]
            enB = [sb(ph, "enB%d" % i, [128, 512]) for i in range(2)]
            eR = [sb(ph, "eR%d" % i, [64, 8, 128]) for i in range(2)]
            Qt = [sb(ph, "Qt%d" % i, [128, 512], BF16) for i in range(2)]
            Kt = [sb(ph, "Kt%d" % i, [128, 512], BF16) for i in range(2)]
            Kh = [sb(ph, "Kh%d" % i, [64, 8, 128], BF16) for i in range(2)]
            ATm = [sb(ph, "ATm%d" % i, [64, 8, 64], BF16) for i in range(2)]
            ngl = 16 if not (dbg and dbg.get('_short')) else 1
            scur = [0]

            def pre1(g):
                gi, nch, latent, tok0, d_ = g['gi'], g['nch'], g['latent'], g['tok0'], g['d']
                nt = nch * 64
                tri = C('TriF') if d_ == 0 else C('TriB')
                slo = C('SLoF') if d_ == 0 else C('SLoB')
                S.dma('sp', lambda e: e.dma_start(out=lfg[gi][:, 0:nch, :], in_=lf_d[d_][tok0:tok0 + nt, :].rearrange("(c s) d -> s c d", s=64)),
                      writes=['lfg%d' % gi])
                S.dma('sp', lambda e: e.dma_start(out=ktg[gi][:, 0:nch, :], in_=k_d[d_][tok0:tok0 + nt, :].rearrange("(c s) d -> s c d", s=64)),
                      writes=['ktg%d' % gi])
                S.dma('sp', lambda e: e.dma_start(out=vg[gi][:, 0:nch, :], in_=v_d[tok0:tok0 + nt, :].rearrange("(c s) d -> s c d", s=64)),
                      writes=['vg%d' % gi])
                if latent:
                    S.dma('sp', lambda e: e.dma_start(out=kTg[gi][:, 0:nt], in_=kT_d[d_][:, tok0:tok0 + nt]), writes=['kTg%d' % gi])
                    S.dma('sp', lambda e: e.dma_start(out=qTg[gi][:, 0:nt], in_=qT_d[:, tok0 - LC:tok0 - LC + nt]), writes=['qTg%d' % gi])
                for ch in range(nch):
                    S.op('pe', lambda e: e.matmul(P[0][:, ch * 64:(ch + 1) * 64], lhsT=lfg[gi][:, ch, :], rhs=tri, start=True, stop=True),
                         reads=['lfg%d' % gi, 'cp'], writes=['P0'], acc=True)
                S.op('act', lambda e: e.activation(out=eB[gi][:, 0:nt], in_=P[0][:, 0:nt], func=AF.Exp), reads=['P0'], writes=['eB%d' % gi])
                if latent:
                    S.op('act', lambda e: e.activation(out=enB[gi][:, 0:nt], in_=P[0][:, 0:nt], func=AF.Exp, scale=-1.0), reads=['P0'], writes=['enB%d' % gi])
                for hf in range((nch + 3) // 4):
                    n4 = min(4, nch - hf * 4)
                    for c4 in range(n4):
                        S.op('pe', lambda e: e.matmul(P[1][0:64, c4 * 128:(c4 + 1) * 128], lhsT=slo, rhs=lfg[gi][:, hf * 4 + c4, :], start=True, stop=True),
                             reads=['lfg%d' % gi, 'cp'], writes=['P1'], acc=True)
                    S.op('act', lambda e: e.activation(out=eR[gi][:, hf * 4:hf * 4 + n4, :],
                                                       in_=P[1][0:64, 0:n4 * 128].rearrange("p (a b) -> p a b", b=128), func=AF.Exp),
                         reads=['P1'], writes=['eR%d' % gi])
                S.op('dve', lambda e: e.tensor_tensor(out=Kh[gi][:, 0:nch, :], in0=ktg[gi][:, 0:nch, :], in1=eR[gi][:, 0:nch, :], op=ALU.mult),
                     reads=['ktg%d' % gi, 'eR%d' % gi], writes=['Kh%d' % gi])
                if latent:
                    S.op('dve', lambda e: e.tensor_tensor(out=Qt[gi][:, 0:nt], in0=qTg[gi][:, 0:nt], in1=eB[gi][:, 0:nt], op=ALU.mult),
                         reads=['qTg%d' % gi, 'eB%d' % gi], writes=['Qt%d' % gi])
                    S.op('dve', lambda e: e.tensor_tensor(out=Kt[gi][:, 0:nt], in0=kTg[gi][:, 0:nt], in1=enB[gi][:, 0:nt], op=ALU.mult),
                         reads=['kTg%d' % gi, 'enB%d' % gi], writes=['Kt%d' % gi])

            def pre2(g):
                gi, nch, latent, d_ = g['gi'], g['nch'], g['latent'], g['d']
                tri = C('TriF') if d_ == 0 else C('TriB')
                if latent:
                    for ch in range(nch):
                        cs = slice(ch * 64, (ch + 1) * 64)
                        S.op('pe', lambda e: e.matmul(P[2][0:64, cs], lhsT=Kt[gi][:, cs], rhs=Qt[gi][:, cs], start=True, stop=True),
                             reads=['Kt%d' % gi, 'Qt%d' % gi], writes=['P2'], acc=True)
                    S.op('dve', lambda e: e.tensor_tensor(out=ATm[gi][:, 0:nch, :], in0=P[2][0:64, 0:nch * 64].rearrange("p (a b) -> p a b", b=64),
                                                          in1=tri.unsqueeze(1).to_broadcast([64, nch, 64]), op=ALU.mult),
                         reads=['P2', 'cp'], writes=['ATm%d' % gi])
                ub = g['ub']
                for ch in range(nch):
                    S.op('pe', lambda e: e.matmul(P[ub + ch // 4][:, (ch % 4) * 128:(ch % 4 + 1) * 128], lhsT=Kh[gi][:, ch, :], rhs=vg[gi][:, ch, :],
                                                  start=True, stop=True),
                         reads=['Kh%d' % gi, 'vg%d' % gi], writes=[PK[ub + ch // 4]], acc=True)

            def seq(g, chs):
                gi, latent, d_, ub = g['gi'], g['latent'], g['d'], g['ub']
                for ch in chs:
                    cs = slice(ch * 64, (ch + 1) * 64)
                    cur = scur[0]
                    if latent:
                        S.op('act', lambda e: e.copy(out=Sb[cur][:], in_=Sst[cur][:]), reads=['S%d' % cur], writes=['Sb%d' % cur])
                        S.op('pe', lambda e: e.matmul(P[7][:, cs], lhsT=Sb[cur][:], rhs=Qt[gi][:, cs], start=True, stop=False),
                             reads=['Sb%d' % cur, 'Qt%d' % gi], writes=['P7'], acc=True)
                        S.op('pe', lambda e: e.matmul(P[7][:, cs], lhsT=vg[gi][:, ch, :], rhs=ATm[gi][:, ch, :], start=False, stop=True),
                             reads=['vg%d' % gi, 'ATm%d' % gi], writes=['P7'], acc=True)
                    dc = ch * 64 + (63 if d_ == 0 else 0)
                    S.op('dve', lambda e: e.scalar_tensor_tensor(out=Sst[1 - cur][:], in0=Sst[cur][:], scalar=eB[gi][:, dc:dc + 1],
                                                                 in1=P[ub + ch // 4][:, (ch % 4) * 128:(ch % 4 + 1) * 128], op0=ALU.mult, op1=ALU.add),
                         reads=['S%d' % cur, 'eB%d' % gi, PK[ub + ch // 4]], writes=['S%d' % (1 - cur)])
                    scur[0] = 1 - cur

            def fin(g):
                if not g['latent']:
                    return
                nt = g['nch'] * 64
                c0 = g['tok0'] - LC
                if g['d'] == 0:
                    S.op('act', lambda e: e.copy(out=oT[:, c0:c0 + nt], in_=P[7][:, 0:nt]), reads=['P7'], writes=['oT'])
                else:
                    S.op('dve', lambda e: e.tensor_tensor(out=oT[:, c0:c0 + nt], in0=oT[:, c0:c0 + nt], in1=P[7][:, 0:nt], op=ALU.add),
                         reads=['P7', 'oT'], writes=['oT'])

            gcount = 0
            for d_ in range(2):
                S.op('dve', lambda e: e.memset(Sst[scur[0]][:], 0.0), writes=['S%d' % scur[0]])
                glist = []
                for (tok0, nch, latent) in [(0, 4, False)] + [(LC + g * 512, 8, True) for g in (range(ngl) if d_ == 0 else range(ngl - 1, -1, -1))]:
                    glist.append(dict(gi=gcount % 2, ub=3 + 2 * (gcount % 2), nch=nch, latent=latent, tok0=tok0, d=d_))
                    gcount += 1
                pre1(glist[0])
                pre2(glist[0])
                for ix, g in enumerate(glist):
                    order = list(range(g['nch'])) if d_ == 0 else list(range(g['nch'] - 1, -1, -1))
                    hlf = len(order) // 2
                    nxt = glist[ix + 1] if ix + 1 < len(glist) else None
                    if nxt:
                        pre1(nxt)
                    seq(g, order[:hlf])
                    if nxt:
                        pre2(nxt)
                    seq(g, order[hlf:])
                    fin(g)
            rt = [sb(ph, "rt%d" % i, [128, 512]) for i in range(3)]
            rb = [sb(ph, "rb%d" % i, [128, 512], BF16) for i in range(2)]
            sgt = [sb(ph, "sgt%d" % i, [128, 512]) for i in range(2)]
            for g in range(ngl):
                cs = slice(g * 512, (g + 1) * 512)
                S.dma('sp', lambda e: e.dma_start(out=sgt[g % 2][:], in_=sgT_d[:, cs]), writes=['sgt%d' % (g % 2)])
                S.op('act', lambda e: e.activation(out=rt[0][:], in_=oT[:, cs], func=AF.Square), reads=['oT'], writes=['rt0'])
                S.op('pe', lambda e: e.matmul(P[5][:, :], lhsT=C('avg128'), rhs=rt[0][:], start=True, stop=True), reads=['cp', 'rt0'], writes=['P5'], acc=True)
                S.op('act', lambda e: e.activation(out=rt[1][:], in_=P[5][:, :], func=AF.Ln, bias=EPS, scale=1.0), reads=['P5'], writes=['rt1'])
                S.op('act', lambda e: e.activation(out=rt[1][:], in_=rt[1][:], func=AF.Exp, scale=-0.5), reads=['rt1'], writes=['rt1'])
                S.op('dve', lambda e: e.tensor_tensor(out=rt[2][:], in0=oT[:, cs], in1=rt[1][:], op=ALU.mult), reads=['oT', 'rt1'], writes=['rt2'])
                ob = rb[g % 2]
                okk = 'rb%d' % (g % 2)
                S.op('dve', lambda e: e.scalar_tensor_tensor(out=ob[:], in0=rt[2][:], scalar=PP('grec_fm'), in1=sgt[g % 2][:], op0=ALU.mult, op1=ALU.mult),
                     reads=['rt2', 'pp', 'sgt%d' % (g % 2)], writes=[okk])
                jj = g // 4
                S.dma('sp', lambda e: e.dma_start(out=mix_d[512 + jj * 128:512 + jj * 128 + 128, (g % 4) * 512:(g % 4 + 1) * 512], in_=ob[:]),
                      reads=[okk], writes=['mixr%d' % g])
                if stop >= 3.5 and g % 8 == 7:
                    ag_mix(2 + g // 8, ['mixr%d' % gg for gg in range(g - 7, g + 1)])
            S.barrier()

        if stop >= 4:
          with ExitStack() as ph:
            wo = sb(ph, "wo", [128, 8, D], BF16)
            imix = sb(ph, "imix", [128, 8], I32)
            mT = sb(ph, "mT", [128, 8, 2048], BF16)
            affT = sb(ph, "affT", [16, 2048])
            xt3 = [sb(ph, "x3t%d" % i, [128, D]) for i in range(2)]
            x1t = [sb(ph, "x1t%d" % i, [128, D]) for i in range(2)]
            h2t = [sb(ph, "h2t%d" % i, [128, D]) for i in range(2)]
            h2b = [sb(ph, "h2b%d" % i, [128, D], BF16) for i in range(2)]
            h2T = sb(ph, "h2T", [128, 8, 128])
            junk3 = sb(ph, "junk3", [128, D])
            st3 = sb(ph, "st3", [128, 8])
            eT = sb(ph, "eT", [16, 128])
            rs16 = sb(ph, "rs16", [16, 128])
            S.dma('sp', lambda e: e.dma_start(out=imix[:], in_=idx_mix_d), writes=['imix'])
            with ExitStack() as ph0:
                wa = [sb(ph0, "wa3%d" % i, [128, 8, 512]) for i in range(2)]
                brows = sb(ph0, "brows", [128, 4096])
                S.dma('act', lambda e: e.dma_start(out=brows[:], in_=bada_rows_d), writes=['brows'])
                w_ada_v = w_ada.rearrange("(k p) n -> p k n", p=128)
                for cb in range(4, 12):
                    wt = wa[cb % 2]
                    wk = 'wa%d' % (cb % 2)
                    S.dma('sp' if cb % 2 == 0 else 'act', lambda e: e.dma_start(out=wt[:], in_=w_ada_v[:, :, cb * 512:(cb + 1) * 512]), writes=[wk])
                    pb = P[1 + (cb % 2)]
                    pk = PK[1 + (cb % 2)]
                    for k in range(8):
                        S.op('pe', lambda e: e.matmul(pb[:, :], lhsT=scB[:, k, :], rhs=wt[:, k, :], start=(k == 0), stop=(k == 7)),
                             reads=[wk, 'scB'], writes=[pk], acc=True)
                    o = (cb - 4) * 512
                    S.op('dve', lambda e: e.tensor_tensor(out=rows[:, o:o + 512], in0=pb[:, :], in1=brows[:, o:o + 512], op=ALU.add),
                         reads=[pk, 'brows'], writes=['rows'])
                S.op('dve', lambda e: e.scalar_tensor_tensor(out=rows[:, 2048:3072], in0=rows[:, 2048:3072], scalar=1.0, in1=PP('gffn_row'),
                                                             op0=ALU.add, op1=ALU.mult),
                     reads=['rows', 'pp'], writes=['rows'])
                S.barrier()
            with ExitStack() as ph0:
                wof = sb(ph0, "wof", [128, 8, D])
                S.dma('sp', lambda e: e.dma_start(out=wof[:], in_=w_out.rearrange("(k p) n -> p k n", p=128)), writes=['wof'])
                for k in range(8):
                    if k % 2 == 0:
                        S.op('act', lambda e: e.copy(out=wo[:, k, :], in_=wof[:, k, :]), reads=['wof'], writes=['wo'])
                    else:
                        S.op('dve', lambda e: e.tensor_copy(out=wo[:, k, :], in_=wof[:, k, :]), reads=['wof'], writes=['wo'])
                S.barrier()
            for k in range(8):
                S.dma('pool', lambda e: e.indirect_dma_start(out=mT[:, k, :], out_offset=None, in_=mixall_d,
                                                            in_offset=bass.IndirectOffsetOnAxis(ap=imix[:, k:k + 1], axis=0)),
                      reads=['imix', 'mixall'], writes=['mT'])
            wr = PP('wr').rearrange("p (k e) -> p k e", e=16)
            def p3A(i):
                xb, xk = xt3[i % 2], 'x3t%d' % (i % 2)
                x1, x1k = x1t[i % 2], 'x1t%d' % (i % 2)
                h2, h2k = h2t[i % 2], 'h2t%d' % (i % 2)
                sk = 'st3_%d' % (i % 2)
                st = st3[:, (i % 2) * 4:(i % 2) * 4 + 4]
                ts_ = slice(i * 128, (i + 1) * 128)
                S.dma('sp', lambda e: e.dma_start(out=xb[:], in_=x_own[ts_, :]), writes=[xk])
                for half in range(2):
                    bo = half + 6 * (i % 2)
                    for k in range(8):
                        S.op('pe', lambda e: e.matmul(P[bo][:, :], lhsT=mT[:, k, ts_], rhs=wo[:, k, half * 512:(half + 1) * 512],
                                                      start=(k == 0), stop=(k == 7)),
                             reads=['mT', 'wo'], writes=[PK[bo]], acc=True)
                    hs = slice(half * 512, (half + 1) * 512)
                    S.op('dve', lambda e: e.tensor_tensor(out=x1[:, hs], in0=P[bo][:, :], in1=rows[:, half * 512:(half + 1) * 512], op=ALU.mult),
                         reads=[PK[bo], 'rows'], writes=[x1k])
                S.op('dve', lambda e: e.tensor_tensor(out=x1[:], in0=x1[:], in1=xb[:], op=ALU.add), reads=[x1k, xk], writes=[x1k])
                S.dma('sp', lambda e: e.dma_start(out=x1_d[ts_, :], in_=x1[:]), reads=[x1k])
                S.op('act', lambda e: e.activation(out=junk3[:], in_=x1[:], func=AF.Square, accum_out=st[:, 0:1]), reads=[x1k], writes=['junk3', sk])
                S.op('act', lambda e: e.activation(out=st[:, 1:2], in_=st[:, 0:1], func=AF.Ln, scale=1.0 / D, bias=EPS), reads=[sk], writes=[sk])
                S.op('act', lambda e: e.activation(out=st[:, 2:3], in_=st[:, 1:2], func=AF.Exp, scale=-0.5), reads=[sk], writes=[sk])
                S.op('dve', lambda e: e.scalar_tensor_tensor(out=h2[:], in0=x1[:], scalar=st[:, 2:3], in1=rows[:, 2048:3072], op0=ALU.mult, op1=ALU.mult),
                     reads=[x1k, sk, 'rows'], writes=[h2k])
                S.op('dve', lambda e: e.tensor_tensor(out=h2[:], in0=h2[:], in1=rows[:, 1024:2048], op=ALU.add), reads=[h2k, 'rows'], writes=[h2k])
                S.op('act', lambda e: e.copy(out=h2b[i % 2][:], in_=h2[:]), reads=[h2k], writes=['h2b%d' % (i % 2)])
                S.dma('sp', lambda e: e.dma_start(out=h2_d[ts_, :], in_=h2b[i % 2][:]), reads=['h2b%d' % (i % 2)], writes=['h2_dt%d' % i])
                if stop >= 5 and i % 4 == 3:
                    S.collective("AllGather", ALU.bypass, [h2_d[(i // 4) * 512:(i // 4 + 1) * 512, :]],
                                 [h2all_d[(i // 4) * 2048:(i // 4 + 1) * 2048, :]], reads=['h2_dt%d' % ii for ii in range(i - 3, i + 1)],
                                 writes=['h2all%d' % (i // 4)])

            def p3B(i):
                h2, h2k = h2t[i % 2], 'h2t%d' % (i % 2)
                ts_ = slice(i * 128, (i + 1) * 128)
                for half in range(2):
                    pb, pk = P[2 + half], PK[2 + half]
                    for kk in range(4):
                        k = half * 4 + kk
                        S.op('pe', lambda e: e.transpose(pb[:, kk * 128:(kk + 1) * 128], h2[:, k * 128:(k + 1) * 128], ident),
                             reads=[h2k, 'cp'], writes=[pk], acc=True)
                    dst = h2T[:, half * 4:(half + 1) * 4, :]
                    src = pb[:, :].rearrange("p (a b) -> p a b", b=128)
                    if half == 0:
                        S.op('act', lambda e: e.copy(out=dst, in_=src), reads=[pk], writes=['h2T0'])
                    else:
                        S.op('dve', lambda e: e.tensor_copy(out=dst, in_=src), reads=[pk], writes=['h2T1'])
                for k in range(8):
                    S.op('pe', lambda e: e.matmul(P[4][0:16, 0:128], lhsT=wr[:, k, :], rhs=h2T[:, k, :], start=(k == 0), stop=(k == 7)),
                         reads=['pp', 'h2T%d' % (k // 4)], writes=['P4'], acc=True)
                S.op('act', lambda e: e.activation(out=eT[:], in_=P[4][0:16, 0:128], func=AF.Exp), reads=['P4'], writes=['eT'])
                S.op('pe', lambda e: e.matmul(P[5][0:16, 0:128], lhsT=C('ones16'), rhs=eT[:], start=True, stop=True), reads=['cp', 'eT'], writes=['P5'], acc=True)
                S.op('dve', lambda e: e.reciprocal(out=rs16[:], in_=P[5][0:16, 0:128]), reads=['P5'], writes=['rs16'])
                S.op('dve', lambda e: e.tensor_tensor(out=affT[:, ts_], in0=eT[:], in1=rs16[:], op=ALU.mult), reads=['eT', 'rs16'], writes=['affT'])

            p3A(0)
            for i in range(16):
                if i + 1 < 16:
                    p3A(i + 1)
                p3B(i)
            S.dma('sp', lambda e: e.dma_start(out=affT_d, in_=affT[:]), reads=['affT'], writes=['affT_d'])
            if stop >= 5:
                S.collective("AllGather", ALU.bypass, [affT_d], [affall_d], reads=['affT_d'], writes=['affall'])
            S.barrier()
        dbgdump('x1', x1_d)
        dbgdump('h2', h2_d)
        dbgdump('affT', affT_d)

        si = [sb(top, "si%d" % i, [128, 64], I32) for i in range(4)]
        gidx = [sb(top, "gi%d" % i, [128, 64], I32) for i in range(4)]
        wpc = [sb(top, "wpc%d" % i, [128, 64]) for i in range(4)]
        if stop >= 5:
          with ExitStack() as ph:
            af = sb(ph, "af", [128, 1024])
            junk4 = sb(ph, "junk4", [128, 1024])
            bis = sb(ph, "bis", [128, 8])
            lo, hi, mid, cnt, ge, dd = [bis[:, i:i + 1] for i in range(6)]
            S.dma('sp', lambda e: e.dma_start(out=af[:], in_=affall_d.rearrange("a (h t) -> (a h) t", h=2)), reads=['affall'], writes=['af'])
            S.op('dve', lambda e: e.memset(lo, 0.0), writes=['bis'])
            S.op('dve', lambda e: e.memset(hi, 1.5), writes=['bis'])
            for it in range(28):
                S.op('dve', lambda e: e.tensor_tensor(out=mid, in0=lo, in1=hi, op=ALU.add), reads=['bis'], writes=['bis'])
                S.op('dve', lambda e: e.tensor_scalar(out=mid, in0=mid, scalar1=0.5, scalar2=None, op0=ALU.mult), reads=['bis'], writes=['bis'])
                S.op('dve', lambda e: e.tensor_scalar(out=junk4[:], in0=af[:], scalar1=mid, scalar2=0.0, op0=ALU.is_ge, op1=ALU.add, accum_out=cnt),
                     reads=['bis', 'af'], writes=['junk4', 'bis'])
                S.op('pe', lambda e: e.matmul(P[0][:, 0:1], lhsT=C('G'), rhs=cnt, start=True, stop=True), reads=['cp', 'bis'], writes=['P0'], acc=True)
                S.op('dve', lambda e: e.tensor_scalar(out=ge, in0=P[0][:, 0:1], scalar1=CAP - 0.5, scalar2=None, op0=ALU.is_ge), reads=['P0'], writes=['bis'])
                S.op('dve', lambda e: e.tensor_tensor(out=dd, in0=mid, in1=lo, op=ALU.subtract), reads=['bis'], writes=['bis'])
                S.op('dve', lambda e: e.scalar_tensor_tensor(out=lo, in0=dd, scalar=ge, in1=lo, op0=ALU.mult, op1=ALU.add), reads=['bis'], writes=['bis'])
                S.op('dve', lambda e: e.tensor_tensor(out=dd, in0=hi, in1=mid, op=ALU.subtract), reads=['bis'], writes=['bis'])
                S.op('dve', lambda e: e.scalar_tensor_tensor(out=hi, in0=dd, scalar=ge, in1=mid, op0=ALU.mult, op1=ALU.add), reads=['bis'], writes=['bis'])
            S.op('dve', lambda e: e.tensor_scalar(out=junk4[:], in0=af[:], scalar1=lo, scalar2=None, op0=ALU.is_ge), reads=['bis', 'af'], writes=['junk4'])
            S.op('dve', lambda e: e.tensor_tensor(out=af[:], in0=af[:], in1=junk4[:], op=ALU.mult), reads=['junk4', 'af'], writes=['af'])
            S.dma('sp', lambda e: e.dma_start(out=mk_d, in_=junk4[:]), reads=['junk4'], writes=['mk_d'])
            S.dma('act', lambda e: e.dma_start(out=wg_d, in_=af[:]), reads=['af'], writes=['wg_d'])
            imk = sb(ph, "imk", [64, 4], I32)
            S.dma('sp', lambda e: e.dma_start(out=imk[:], in_=idx_mk_d), writes=['imk'])
            mkT = sb(ph, "mkT", [64, 128])
            wgT = sb(ph, "wgT", [64, 128])
            mk = sb(ph, "mk", [128, 64])
            totb = sb(ph, "totb", [128, 64])
            slf = sb(ph, "slf", [128, 64])
            sl2 = sb(ph, "sl2", [128, 64])
            tot = sb(ph, "tot", [128, 1])
            mkv = mk_d.rearrange("q (c p) -> (q c) p", p=128)
            wgv = wg_d.rearrange("q (c p) -> (q c) p", p=128)
            for el in range(4):
                S.dma('pool', lambda e: e.indirect_dma_start(out=mkT[:, :], out_offset=None, in_=mkv,
                                                            in_offset=bass.IndirectOffsetOnAxis(ap=imk[:, el:el + 1], axis=0)),
                      reads=['imk', 'mk_d'], writes=['mkT'])
                S.dma('pool', lambda e: e.indirect_dma_start(out=wgT[:, :], out_offset=None, in_=wgv,
                                                            in_offset=bass.IndirectOffsetOnAxis(ap=imk[:, el:el + 1], axis=0)),
                      reads=['imk', 'wg_d'], writes=['wgT'])
                S.op('pe', lambda e: e.transpose(P[1][:, 0:64], mkT[:, :], C('ident', 64)[:, 0:64]), reads=['mkT', 'cp'], writes=['P1'], acc=True)
                S.op('pe', lambda e: e.transpose(P[2][:, 0:64], wgT[:, :], C('ident', 64)[:, 0:64]), reads=['wgT', 'cp'], writes=['P2'], acc=True)
                S.op('dve', lambda e: e.tensor_copy(out=mk[:], in_=P[1][:, 0:64]), reads=['P1'], writes=['mk'])
                S.op('act', lambda e: e.copy(out=wpc[el][:], in_=P[2][:, 0:64]), reads=['P2'], writes=['wpc%d' % el])
                S.op('dve', lambda e: e.reduce_sum(out=tot[:], in_=mk[:], axis=AX.X), reads=['mk'], writes=['tot'])
                S.op('dve', lambda e: e.tensor_scalar(out=totb[:], in0=C('one128')[:, 0:64], scalar1=tot[:, 0:1], scalar2=None, op0=ALU.mult),
                     reads=['tot', 'cp'], writes=['totb'])
                S.op('pe', lambda e: e.matmul(P[3][:, 0:64], lhsT=mkT[:, :], rhs=C('SU64'), start=True, stop=False), reads=['mkT', 'cp'], writes=['P3'], acc=True)
                S.op('pe', lambda e: e.matmul(P[3][:, 0:64], lhsT=C('L128'), rhs=totb[:], start=False, stop=True), reads=['totb', 'cp'], writes=['P3'], acc=True)
                S.op('dve', lambda e: e.tensor_copy(out=slf[:], in_=P[3][:, 0:64]), reads=['P3'], writes=['slf'])
                S.op('dve', lambda e: e.tensor_tensor(out=sl2[:], in0=slf[:], in1=mk[:], op=ALU.mult), reads=['slf', 'mk'], writes=['sl2'])
                S.op('dve', lambda e: e.tensor_scalar(out=sl2[:], in0=sl2[:], scalar1=float(CAP - 1), scalar2=None, op0=ALU.min), reads=['sl2'], writes=['sl2'])
                S.op('dve', lambda e: e.tensor_copy(out=gidx[el][:], in_=sl2[:]), reads=['sl2'], writes=['gi%d' % el])
                S.op('dve', lambda e: e.tensor_scalar(out=sl2[:], in0=slf[:], scalar1=-5000.0, scalar2=None, op0=ALU.add), reads=['slf'], writes=['sl2'])
                S.op('dve', lambda e: e.tensor_tensor(out=sl2[:], in0=sl2[:], in1=mk[:], op=ALU.mult), reads=['sl2', 'mk'], writes=['sl2'])
                S.op('dve', lambda e: e.tensor_scalar(out=sl2[:], in0=sl2[:], scalar1=5000.0, scalar2=None, op0=ALU.add), reads=['sl2'], writes=['sl2'])
                S.op('dve', lambda e: e.tensor_copy(out=si[el][:], in_=sl2[:]), reads=['sl2'], writes=['si%d' % el])
            if dbg and 'slots' in dbg:
                for el in range(4):
                    S.op('dve', lambda e: e.tensor_copy(out=junk4[:, el * 64:(el + 1) * 64], in_=si[el][:]), reads=['si%d' % el], writes=['junk4'])
                    S.op('dve', lambda e: e.tensor_copy(out=junk4[:, 256 + el * 64:256 + (el + 1) * 64], in_=wpc[el][:]), reads=['wpc%d' % el], writes=['junk4'])
                S.dma('sp', lambda e: e.dma_start(out=dbg_out['slots'], in_=junk4[:, 0:512]), reads=['junk4'])
            S.barrier()

        if stop >= 7:
          with ExitStack() as ph:
            xsT = sb(ph, "xsT", [128, 8, CAP], BF16)
            hid = sb(ph, "hid", [128, NFC, CAP], BF16)
            wgf = [sb(ph, "wgf%d" % i, [128, 8, 256]) for i in range(2)]
            wuf = [sb(ph, "wuf%d" % i, [128, 8, 256]) for i in range(2)]
            wgb = [sb(ph, "wgb%d" % i, [128, 8, 256], BF16) for i in range(2)]
            wub = [sb(ph, "wub%d" % i, [128, 8, 256], BF16) for i in range(2)]
            wdf = [sb(ph, "wdf%d" % i, [128, D]) for i in range(3)]
            wdb = [sb(ph, "wdb%d" % i, [128, D], BF16) for i in range(3)]
            xr = [sb(ph, "xr%d" % i, [128, D], BF16) for i in range(2)]
            yt = [sb(ph, "yt%d" % i, [128, D]) for i in range(2)]
            tmp6 = [sb(ph, "tmp6%d" % i, [128, 512]) for i in range(2)]
            identb = sb(ph, "identb", [128, 128], BF16)
            S.op('dve', lambda e: e.tensor_copy(out=identb[:], in_=ident), reads=['cp'], writes=['identb'])
            nexp = 4 if not (dbg and dbg.get('_short')) else 1
            hb = [sb(ph, "hb%d" % i, [128, D], BF16) for i in range(4)]
            breg = nc.gpsimd.to_reg(CAP - 1)
            hcnt = [0]

            def dispatch(el):
                def load(blk):
                    i_ = (hcnt[0] + blk) % 4
                    S.dma('pool', lambda e: e.dma_start(out=hb[i_][:], in_=h2all_d[blk * 128:(blk + 1) * 128, :]),
                          reads=['h2all%d' % (blk // 16)], writes=['hb%d' % i_])
                load(0)
                load(1)
                for blk in range(64):
                    if blk + 2 < 64:
                        load(blk + 2)
                    i_ = (hcnt[0] + blk) % 4
                    tb = ((blk % 16) // 4) * 16 + (blk // 16) * 4 + (blk % 4)
                    S.dma('pool', lambda e: e.indirect_dma_start(out=xs_d[el], out_offset=bass.IndirectOffsetOnAxis(ap=si[el][:, tb:tb + 1], axis=0),
                                                                in_=hb[i_][:, :], in_offset=None, bounds_check=breg, oob_is_err=False),
                          reads=['hb%d' % i_, 'si%d' % el])
                hcnt[0] += 64
                S.fence('xsd%d' % el, 'pool')

            dispatch(0)
            wcnt = 0
            dcnt = 0
            tcnt = 0
            xcnt = 0
            for el in range(nexp):
                if el + 1 < nexp:
                    dispatch(el + 1)
                wgv_ = weg[el].rearrange("(k p) f -> p k f", p=128)
                wuv_ = weu[el].rearrange("(k p) f -> p k f", p=128)
                for st in range(8):
                    r0 = st * 128
                    xb, xk = xr[xcnt % 2], 'xr%d' % (xcnt % 2)
                    xcnt += 1
                    S.dma('sp', lambda e: e.dma_start(out=xb[:], in_=xs_d[el][r0:r0 + 128, :]), reads=['xsd%d' % el], writes=[xk])
                    for h2_ in range(2):
                        pb, pk = P[4 + h2_][:, :].bitcast(BF16), PK[4 + h2_]
                        for kk in range(4):
                            k = h2_ * 4 + kk
                            S.op('pe', lambda e: e.transpose(pb[:, kk * 128:(kk + 1) * 128], xb[:, k * 128:(k + 1) * 128], identb[:]),
                                 reads=[xk, 'identb'], writes=[pk], acc=True)
                        dst = xsT[:, h2_ * 4:(h2_ + 1) * 4, st * 128:(st + 1) * 128]
                        src = pb[:, 0:512].rearrange("p (a b) -> p a b", b=128)
                        if h2_ == 0:
                            S.op('act', lambda e: e.copy(out=dst, in_=src), reads=[pk], writes=['xsT0'])
                        else:
                            S.op('dve', lambda e: e.tensor_copy(out=dst, in_=src), reads=[pk], writes=['xsT1'])
                for fg in range(11):
                    wi = wcnt % 2
                    wcnt += 1
                    S.dma('sp', lambda e: e.dma_start(out=wgf[wi][:], in_=wgv_[:, :, fg * 256:(fg + 1) * 256]), writes=['wgf%d' % wi])
                    S.dma('sp', lambda e: e.dma_start(out=wuf[wi][:], in_=wuv_[:, :, fg * 256:(fg + 1) * 256]), writes=['wuf%d' % wi])
                    S.op('act', lambda e: e.copy(out=wgb[wi][:], in_=wgf[wi][:]), reads=['wgf%d' % wi], writes=['wgb%d' % wi])
                    S.op('dve', lambda e: e.tensor_copy(out=wub[wi][:], in_=wuf[wi][:]), reads=['wuf%d' % wi], writes=['wub%d' % wi])
                    for fc in range(2):
                        for half in range(2):
                            pg, pgk = P[half], PK[half]
                            pu, puk = P[2 + half], PK[2 + half]
                            hs = slice(half * 512, (half + 1) * 512)
                            for k in range(8):
                                S.op('pe', lambda e: e.matmul(pg[:, :], lhsT=wgb[wi][:, k, fc * 128:(fc + 1) * 128], rhs=xsT[:, k, hs],
                                                              start=(k == 0), stop=(k == 7)),
                                     reads=['wgb%d' % wi, 'xsT%d' % (k // 4)], writes=[pgk], acc=True)
                            for k in range(8):
                                S.op('pe', lambda e: e.matmul(pu[:, :], lhsT=wub[wi][:, k, fc * 128:(fc + 1) * 128], rhs=xsT[:, k, hs],
                                                              start=(k == 0), stop=(k == 7)),
                                     reads=['wub%d' % wi, 'xsT%d' % (k // 4)], writes=[puk], acc=True)
                            ti = tcnt % 2
                            tcnt += 1
                            S.op('act', lambda e: e.activation(out=tmp6[ti][:], in_=pg[:, :], func=AF.Silu), reads=[pgk], writes=['tmp6%d' % ti])
                            S.op('dve', lambda e: e.tensor_tensor(out=hid[:, fg * 2 + fc, hs], in0=tmp6[ti][:], in1=pu[:, :], op=ALU.mult),
                                 reads=['tmp6%d' % ti, puk], writes=['hid'])
                for tg in range(2):
                    for fch in range(NFC):
                        di = dcnt % 3
                        dcnt += 1
                        S.dma('sp', lambda e: e.dma_start(out=wdf[di][:], in_=wed[el][fch * 128:(fch + 1) * 128, :]), writes=['wdf%d' % di])
                        if fch % 2 == 0:
                            S.op('act', lambda e: e.copy(out=wdb[di][:], in_=wdf[di][:]), reads=['wdf%d' % di], writes=['wdb%d' % di])
                        else:
                            S.op('dve', lambda e: e.tensor_copy(out=wdb[di][:], in_=wdf[di][:]), reads=['wdf%d' % di], writes=['wdb%d' % di])
                        for st in range(4):
                            for dh in range(2):
                                S.op('pe', lambda e: e.matmul(P[st * 2 + dh][:, :], lhsT=hid[:, fch, (tg * 4 + st) * 128:(tg * 4 + st + 1) * 128],
                                                              rhs=wdb[di][:, dh * 512:(dh + 1) * 512], start=(fch == 0), stop=(fch == NFC - 1)),
                                     reads=['hid', 'wdb%d' % di], writes=[PK[st * 2 + dh]], acc=True)
                    for st in range(4):
                        yb, yk = yt[st % 2], 'yt%d' % (st % 2)
                        S.op('act', lambda e: e.copy(out=yb[:, 0:512], in_=P[st * 2][:, :]), reads=[PK[st * 2]], writes=[yk])
                        S.op('dve', lambda e: e.tensor_copy(out=yb[:, 512:1024], in_=P[st * 2 + 1][:, :]), reads=[PK[st * 2 + 1]], writes=[yk])
                        r0 = (tg * 4 + st) * 128
                        S.dma('act', lambda e: e.dma_start(out=ye_d[el][r0:r0 + 128, :], in_=yb[:]), reads=[yk])
            S.barrier()
        dbgdump('ye0', ye_d[0])

        if stop >= 8:
          with ExitStack() as ph:
            gt_ = [sb(ph, "gt%d" % i, [128, D]) for i in range(4)]
            acc = [sb(ph, "acc%d" % i, [128, D]) for i in range(2)]
            accb = [sb(ph, "accb%d" % i, [128, D], BF16) for i in range(2)]
            gc = 0
            breg7 = nc.gpsimd.to_reg(CAP - 1)
            for i in range(4):
                S.op('pool', lambda e: e.memset(gt_[i][:], 0.0), writes=['gt%d' % i])
            for blk in range(64):
                ab, ak = acc[blk % 2], 'acc%d' % (blk % 2)
                tb7 = ((blk % 32) // 8) * 16 + (blk // 32) * 8 + (blk % 8)
                for el in range(4):
                    g_, gk = gt_[gc % 4], 'gt%d' % (gc % 4)
                    gc += 1
                    S.dma('pool', lambda e: e.indirect_dma_start(out=g_[:, :], out_offset=None, in_=ye_d[el],
                                                                in_offset=bass.IndirectOffsetOnAxis(ap=si[el][:, tb7:tb7 + 1], axis=0),
                                                                bounds_check=breg7, oob_is_err=False),
                          reads=['si%d' % el], writes=[gk])
                    if el == 0:
                        S.op('dve', lambda e: e.tensor_scalar(out=ab[:], in0=g_[:], scalar1=wpc[el][:, tb7:tb7 + 1], scalar2=None, op0=ALU.mult),
                             reads=[gk, 'wpc%d' % el], writes=[ak])
                    elif el < 3:
                        S.op('dve', lambda e: e.scalar_tensor_tensor(out=ab[:], in0=g_[:], scalar=wpc[el][:, tb7:tb7 + 1], in1=ab[:], op0=ALU.mult, op1=ALU.add),
                             reads=[gk, 'wpc%d' % el, ak], writes=[ak])
                    else:
                        S.op('dve', lambda e: e.scalar_tensor_tensor(out=accb[blk % 2][:], in0=g_[:], scalar=wpc[el][:, tb7:tb7 + 1], in1=ab[:],
                                                                     op0=ALU.mult, op1=ALU.add),
                             reads=[gk, 'wpc%d' % el, ak], writes=['accb%d' % (blk % 2)])
                S.dma('sp', lambda e: e.dma_start(out=op_d[blk * 128:(blk + 1) * 128, :], in_=accb[blk % 2][:]), reads=['accb%d' % (blk % 2)],
                      writes=['op_b%d' % blk])
                if blk % 32 == 31:
                    c_ = blk // 32
                    S.collective("ReduceScatter", ALU.add, [op_d[c_ * 4096:(c_ + 1) * 4096, :]], [moe_d[c_ * 1024:(c_ + 1) * 1024, :]],
                                 reads=['op_b%d' % bb for bb in range(blk - 31, blk + 1)], writes=['moe%d' % c_])
            S.barrier()
        dbgdump('moe', moe_d)

        if stop >= 9:
          with ExitStack() as ph:
            a8 = [sb(ph, "a8%d" % i, [128, D]) for i in range(2)]
            m8 = [sb(ph, "m8%d" % i, [128, D], BF16) for i in range(2)]
            m8f = [sb(ph, "m8f%d" % i, [128, D]) for i in range(2)]
            o8 = [sb(ph, "o8%d" % i, [128, D]) for i in range(2)]
            junk8 = sb(ph, "junk8", [128, D])
            st8 = sb(ph, "st8", [128, 8])
            for i in range(16):
                ts_ = slice(i * 128, (i + 1) * 128)
                a_, ak = a8[i % 2], 'a8%d' % (i % 2)
                m_, mk_ = m8[i % 2], 'm8%d' % (i % 2)
                o_, ok_ = o8[i % 2], 'o8%d' % (i % 2)
                S.dma('sp', lambda e: e.dma_start(out=a_[:], in_=x1_d[ts_, :]), writes=[ak])
                S.dma('act', lambda e: e.dma_start(out=m_[:], in_=moe_d[ts_, :]), reads=['moe%d' % (i // 8)], writes=[mk_])
                mf_, mfk = m8f[i % 2], 'm8f%d' % (i % 2)
                S.op('dve', lambda e: e.tensor_tensor(out=mf_[:], in0=m_[:], in1=rows[:, 3072:4096], op=ALU.mult), reads=[mk_, 'rows'], writes=[mfk])
                S.op('dve', lambda e: e.tensor_tensor(out=a_[:], in0=a_[:], in1=mf_[:], op=ALU.add), reads=[ak, mfk], writes=[ak])
                S.op('act', lambda e: e.activation(out=junk8[:], in_=a_[:], func=AF.Square, accum_out=st8[:, 0:1]), reads=[ak], writes=['junk8', 'st8'])
                S.op('act', lambda e: e.activation(out=st8[:, 1:2], in_=st8[:, 0:1], func=AF.Ln, scale=1.0 / D, bias=EPS), reads=['st8'], writes=['st8'])
                S.op('act', lambda e: e.activation(out=st8[:, 2:3], in_=st8[:, 1:2], func=AF.Exp, scale=-0.5), reads=['st8'], writes=['st8'])
                S.op('dve', lambda e: e.scalar_tensor_tensor(out=o_[:], in0=a_[:], scalar=st8[:, 2:3], in1=PP('gfin_row'), op0=ALU.mult, op1=ALU.mult),
                     reads=[ak, 'st8', 'pp'], writes=[ok_])
                S.dma('sp', lambda e: e.dma_start(out=out_d[ts_, :], in_=o_[:]), reads=[ok_], writes=['out'])
        S.barrier(cc=True)
    return nc


def _make_inputs(x, c, ctx, c_ctx, w_ada, b_ada, g_mix, w_in, w_fourier, lb_logits, g_rec, w_out,
                 g_ffn, w_router, w_exp_gate, w_exp_up, w_exp_down, g_final):
    f = lambda a: np.ascontiguousarray(np.asarray(a, dtype=np.float32))
    x, c, ctx, c_ctx = f(x), f(c), f(ctx), f(c_ctx)
    w_ada, b_ada, g_mix, w_in = f(w_ada)[0], f(b_ada)[0], f(g_mix)[0], f(w_in)[0]
    w_fourier, lb_logits, g_rec, w_out = f(w_fourier)[0], f(lb_logits), f(g_rec)[0], f(w_out)[0]
    g_ffn, w_router = f(g_ffn)[0], f(w_router)[0]
    weg, weu, wed, g_final = f(w_exp_gate)[0], f(w_exp_up)[0], f(w_exp_down)[0], f(g_final)
    perm = np.concatenate([np.concatenate([np.arange(r * 128, (r + 1) * 128), 512 + np.arange(r * 128, (r + 1) * 128)]) for r in range(4)])
    w_out_p = np.ascontiguousarray(w_out[perm, :])
    in_maps = []
    for core in range(8):
        b, j = core // 4, core % 4
        cols = np.concatenate([np.arange(j * 128, (j + 1) * 128)] + [512 + i * 512 + np.arange(j * 128, (j + 1) * 128) for i in range(5)])
        pk = np.zeros((128, PW), np.float32)

        def put(name, arr):
            o, w = PLAY[name]
            pk[:, o:o + w] = arr.reshape(128, w)
        cc = np.stack([c[b], c_ctx], 0)
        put('ccT', cc.reshape(2, 8, 128).transpose(2, 1, 0))
        put('bada_fm', b_ada[:2048].reshape(16, 128).T)
        put('gmix_fm', g_mix.reshape(8, 128).T)
        put('grec_fm', g_rec[j * 128:(j + 1) * 128].reshape(128, 1))
        lbl = lb_logits[:, :, j * 128:(j + 1) * 128]
        put('lbl_fm', lbl.reshape(4, 128).T)
        put('lbl_row', np.broadcast_to(lbl.reshape(1, 512), (128, 512)))
        put('wr', w_router.reshape(8, 128, 16).transpose(1, 0, 2))
        put('gffn_row', np.broadcast_to(g_ffn.reshape(1, 1024), (128, 1024)))
        put('gfin_row', np.broadcast_to(g_final.reshape(1, 1024), (128, 1024)))
        feat = np.arange(1024)
        r_, fh_, p_ = feat // 256, (feat // 128) % 2, feat % 128
        rowidx = (fh_ * 2 + j // 2) * 1024 + r_ * 256 + (j % 2) * 128 + p_
        idx_mix = np.ascontiguousarray(rowidx.reshape(8, 128).T.astype(np.int32))
        idx_mk = np.zeros((64, 4), np.int32)
        for el in range(4):
            e_ = 4 * j + el
            cidx = np.arange(64)
            rr, hh, cc_ = cidx // 16, (cidx // 8) % 2, cidx % 8
            idx_mk[:, el] = (rr * 32 + e_ * 2 + hh) * 8 + cc_
        in_maps.append({
            "x_b": x[b], "x_own": np.ascontiguousarray(x[b, j * 2048:(j + 1) * 2048]), "ctx_b": ctx[b],
            "w_ada": w_ada, "w_in": np.ascontiguousarray(w_in[:, cols]), "w_f": w_fourier[j],
            "w_out": w_out_p, "weg": np.ascontiguousarray(weg[4 * j:4 * j + 4]), "weu": np.ascontiguousarray(weu[4 * j:4 * j + 4]),
            "wed": np.ascontiguousarray(wed[4 * j:4 * j + 4]), "cpack": CPACK, "ppack": pk,
            "bada_rows": np.ascontiguousarray(np.broadcast_to(b_ada[2048:].reshape(1, 4096), (128, 4096))),
            "idx_mix": idx_mix, "idx_mk": idx_mk,
        })
    return in_maps


def kernel(**inputs):
    in_maps = _make_inputs(**inputs)
    nc = build()
    res = run_bass_kernel_spmd(nc, in_maps, core_ids=list(range(8)))
    out = np.zeros((2, L, D), np.float32)
    for core in range(8):
        b, j = core // 4, core % 4
        out[b, j * 2048:(j + 1) * 2048] = res.results[core]["out"]
    return out
```

```python
import os
import numpy as np
from contextlib import ExitStack
import concourse.bass as bass
import concourse.mybir as mybir
from concourse.bass_utils import run_bass_kernel_spmd

F32 = mybir.dt.float32
I32 = mybir.dt.int32
BF16 = mybir.dt.bfloat16
AF = mybir.ActivationFunctionType
ALU = mybir.AluOpType
AX = mybir.AxisListType

D = 1024
L = 8192
LC = 256
TT = LC + L
NE = 16
CAP = 1024
DE = 2816
NFC = DE // 128
EPS = 1e-6
GROUPS = [[0, 1, 2, 3], [4, 5, 6, 7]]


class Sched:
    CE = ('pe', 'dve', 'act', 'pool')

    def __init__(self, nc, stack, ndma=8):
        self.nc = nc
        self.e = dict(pe=nc.tensor, dve=nc.vector, act=nc.scalar, pool=nc.gpsimd, sp=nc.sync)
        self.csem = {k: stack.enter_context(nc.semaphore('cs_' + k)) for k in self.CE}
        self.ccnt = {k: 0 for k in self.CE}
        self.dsem = {q: [stack.enter_context(nc.semaphore('ds_%s%d' % (q, i))) for i in range(ndma)]
                     for q in ('sp', 'pool', 'act')}
        self.dcnt = {q: [0] * ndma for q in self.dsem}
        self.dnext = {q: 0 for q in self.dsem}
        self.ndma = ndma
        self.seen = {k: {} for k in self.e}
        self.lastw = {}
        self.readers = {}
        self.ccsem = stack.enter_context(nc.semaphore('ccsem'))
        self.cccnt = 0
        self.n = 0

    def _wait(self, eng, ev):
        sem, val, src = ev
        d = self.seen[eng]
        if d.get(sem, 0) >= val:
            return
        self.e[eng].wait_ge(sem, val)
        d[sem] = val

    def _deps(self, eng, reads, writes, skip_same_waw):
        deps = []
        for k in reads:
            w = self.lastw.get(k)
            if isinstance(w, list):
                deps.extend(w)
            elif w is not None:
                deps.append(w)
            if isinstance(k, str) and len(k) == 2 and k[0] == 'P' and k[1].isdigit():
                for sem, (val, src) in self.readers.get(k, {}).items():
                    if src != eng:
                        deps.append((sem, val, src))
        for k in writes:
            w = self.lastw.get(k)
            if isinstance(w, list):
                deps.extend(w)
            elif w is not None and not (skip_same_waw and w[2] == eng):
                deps.append(w)
            for sem, (val, src) in self.readers.get(k, {}).items():
                deps.append((sem, val, src))
        for ev in deps:
            self._wait(eng, ev)

    def _record(self, ev, reads, writes):
        sem, val, src = ev
        for k in reads:
            self.readers.setdefault(k, {})[sem] = (val, src)
        for k in writes:
            self.lastw[k] = ev
            self.readers[k] = {}

    def op(self, eng, fn, reads=(), writes=(), acc=False):
        self._deps(eng, reads, writes, acc)
        ins = fn(self.e[eng])
        self.ccnt[eng] += 1
        ins.then_inc(self.csem[eng], 1)
        ev = (self.csem[eng], self.ccnt[eng], eng)
        self._record(ev, reads, writes)
        self.n += 1
        return ev

    def dma(self, q, fn, reads=(), writes=()):
        i = self.dnext[q]
        self.dnext[q] = (i + 1) % self.ndma
        sem = self.dsem[q][i]
        if self.dcnt[q][i] > 0:
            self._wait(q, (sem, self.dcnt[q][i], 'dma'))
        self._deps(q, reads, writes, False)
        ins = fn(self.e[q])
        ins.then_inc(sem, 16)
        self.dcnt[q][i] += 16
        ev = (sem, self.dcnt[q][i], 'dma')
        self._record(ev, reads, writes)
        self.n += 1
        return ev

    def fence(self, key, q):
        self.lastw[key] = [(self.dsem[q][i], self.dcnt[q][i], 'dma') for i in range(self.ndma) if self.dcnt[q][i] > 0]
        self.readers[key] = {}

    def collective(self, kind, op, ins, outs, reads=(), writes=()):
        if self.cccnt > 0:
            self._wait('pool', (self.ccsem, self.cccnt, 'cc'))
        self._deps('pool', reads, writes, False)
        ins_ = self.nc.gpsimd.collective_compute(kind, op, replica_groups=GROUPS, ins=ins, outs=outs)
        ins_.then_inc(self.ccsem, 1)
        self.cccnt += 1
        ev = (self.ccsem, self.cccnt, 'cc')
        self._record(ev, reads, writes)
        return ev

    def barrier(self, cc=False):
        evs = [(self.csem[k], self.ccnt[k], k) for k in self.CE if self.ccnt[k] > 0]
        for q in self.dsem:
            for i in range(self.ndma):
                if self.dcnt[q][i] > 0:
                    evs.append((self.dsem[q][i], self.dcnt[q][i], 'dma'))
        if self.cccnt and cc:
            evs.append((self.ccsem, self.cccnt, 'cc'))
        for eng in self.e:
            for ev in evs:
                self._wait(eng, ev)
        keepw = {k: v for k, v in self.lastw.items() if not isinstance(v, list) and v[2] == 'cc'}
        keepr = {k: {sm: vv for sm, vv in d.items() if vv[1] == 'cc'} for k, d in self.readers.items()}
        self.lastw = keepw if not cc else {}
        self.readers = {k: d for k, d in keepr.items() if d} if not cc else {}


def _const_tables():
    c = {}
    i128 = np.arange(128)
    i64 = np.arange(64)
    c['ident'] = np.eye(128)
    a = 2 * np.pi * np.outer(i128, i128) / 128.0
    c['C128'] = np.cos(a)
    c['S128'] = np.sin(a)
    c['nS128'] = -np.sin(a)
    c['CCs'] = np.cos(a) / 1024.0
    c['SCs'] = np.sin(a) / 1024.0
    tw = 2 * np.pi * np.outer(i128, i64) / 8192.0
    c['Tc'] = np.cos(tw)
    c['Ts'] = np.sin(tw)
    a64 = 2 * np.pi * np.outer(i64, i64) / 64.0
    c['C64'] = np.cos(a64)
    c['nS64'] = -np.sin(a64)
    s = i64[:, None]
    t = i64[None, :]
    c['TriF'] = (s <= t) * 1.0
    c['TriB'] = (s >= t) * 1.0
    c['SLoF'] = (s > t) * 1.0
    c['SLoB'] = (s < t) * 1.0
    c['SU64'] = (s < t) * 1.0
    c['ones16'] = np.ones((16, 16))
    p = i128[:, None]
    q = i128[None, :]
    c['L128'] = (p < q) * 1.0
    c['avg128'] = np.ones((128, 128)) / 128.0
    c['one128'] = np.ones((128, 128))
    eq = (i128 // 2) % 16
    c['G'] = (eq[:, None] == eq[None, :]) * 1.0
    lay = {}
    off = 0
    for k, v in c.items():
        lay[k] = (off, v.shape[0], v.shape[1])
        off += v.shape[1]
    pack = np.zeros((128, off), np.float32)
    for k, v in c.items():
        o, r, w = lay[k]
        pack[:r, o:o + w] = v.astype(np.float32)
    return pack, lay


CPACK, CLAY = _const_tables()

PLAY = {}


def _play():
    off = 0
    for name, w in [('ccT', 16), ('bada_fm', 16), ('gmix_fm', 8), ('grec_fm', 1), ('lbl_fm', 4),
                    ('lbl_row', 512), ('wr', 128), ('gffn_row', 1024), ('gfin_row', 1024)]:
        PLAY[name] = (off, w)
        off += w
    return off


PW = _play()


def build(dbg=None):
    nc = bass.Bass("TRN2", target_bir_lowering=False)

    def din(name, shape, dt=F32):
        return nc.dram_tensor(name, list(shape), dt, kind="ExternalInput").ap()

    x_b = din("x_b", [L, D])
    x_own = din("x_own", [2048, D])
    ctx_b = din("ctx_b", [LC, D])
    w_ada = din("w_ada", [D, 6 * D])
    w_in = din("w_in", [D, 768])
    w_f = din("w_f", [128, 128])
    w_out = din("w_out", [D, D])
    weg = din("weg", [4, D, DE])
    weu = din("weu", [4, D, DE])
    wed = din("wed", [4, DE, D])
    cpack_d = din("cpack", list(CPACK.shape))
    ppack_d = din("ppack", [128, PW])
    bada_rows_d = din("bada_rows", [128, 4096])
    idx_mix_d = din("idx_mix", [128, 8], I32)
    idx_mk_d = din("idx_mk", [64, 4], I32)
    out_d = nc.dram_tensor("out", [2048, D], F32, kind="ExternalOutput").ap()

    def dscr(name, shape, dt=F32):
        return nc.dram_tensor(name, list(shape), dt).ap()

    def ag_mix(i, keys):
        S.collective("AllGather", ALU.bypass, [mix_d[i * 256:(i + 1) * 256, :]], [mixall_d[i * 1024:(i + 1) * 1024, :]],
                     reads=keys, writes=['mixall'])

    uT_d = dscr("uT_d", [128, L])
    qT_d = dscr("qT_d", [128, L])
    sgT_d = dscr("sgT_d", [128, L])
    kT_d = [dscr("kfT_d", [128, TT]), dscr("kbT_d", [128, TT])]
    v_d = dscr("v_d", [TT, 128], BF16)
    k_d = [dscr("kf_d", [TT, 128]), dscr("kb_d", [TT, 128])]
    lf_d = [dscr("lff_d", [TT, 128]), dscr("lfb_d", [TT, 128])]
    Y_d = dscr("Y_d", [2, 128, 64, 128])
    mix_d = dscr("mix_d", [4 * 256, 2048], BF16)
    mixall_d = dscr("mixall_d", [16 * 256, 2048], BF16)
    x1_d = dscr("x1_d", [2048, D])
    h2_d = dscr("h2_d", [2048, D], BF16)
    h2all_d = dscr("h2all_d", [L, D], BF16)
    affT_d = dscr("affT_d", [16, 2048])
    affall_d = dscr("affall_d", [64, 2048])
    mk_d = dscr("mk_d", [128, 1024])
    wg_d = dscr("wg_d", [128, 1024])
    xs_d = [dscr("xs_d%d" % i, [CAP, D], BF16) for i in range(4)]
    ye_d = [dscr("ye_d%d" % i, [CAP, D]) for i in range(4)]
    op_d = dscr("op_d", [L, D], BF16)
    moe_d = dscr("moe_d", [2048, D], BF16)

    dbg_out = {}
    if dbg:
        for name, shape in dbg.items():
            if name.startswith('_'):
                continue
            dbg_out[name] = nc.dram_tensor("dbg_" + name, list(shape), F32, kind="ExternalOutput").ap()

    with ExitStack() as top:
        S = Sched(nc, top)
        sb = lambda st, name, shape, dt=F32: st.enter_context(nc.sbuf_tensor(name, list(shape), dt))
        P = [top.enter_context(nc.psum_tensor("P%d" % i, [128, 512], F32)) for i in range(8)]
        PK = ['P%d' % i for i in range(8)]
        cp = sb(top, "cpack_sb", CPACK.shape)
        pp = sb(top, "ppack_sb", [128, PW])
        S.dma('sp', lambda e: e.dma_start(out=cp[:], in_=cpack_d), writes=['cp'])
        S.dma('sp', lambda e: e.dma_start(out=pp[:], in_=ppack_d), writes=['pp'])

        def C(name, rows=None):
            o, r, w = CLAY[name]
            return cp[0:(rows or r), o:o + w]

        def PP(name, a=0, b=None):
            o, w = PLAY[name]
            return pp[:, o + a:o + (w if b is None else b)]

        ident = C('ident')
        modfm = sb(top, "modfm", [128, 16, 2])
        A1 = sb(top, "A1", [128, 8, 2])
        rows = sb(top, "rows", [128, 4096])
        lbs = sb(top, "lbs", [128, 4])
        omlrow = sb(top, "omlrow", [128, 256])
        scB = sb(top, "scB", [128, 8, 128])

        with ExitStack() as ph:
            scT = sb(ph, "scT", [128, 16])
            wa = [sb(ph, "wa%d" % i, [128, 8, 512]) for i in range(2)]
            S.op('act', lambda e: e.activation(out=scT[:], in_=PP('ccT'), func=AF.Exp, scale=-1.0), reads=['pp'], writes=['scT'])
            S.op('dve', lambda e: e.tensor_scalar(out=scT[:], in0=scT[:], scalar1=1.0, scalar2=None, op0=ALU.add), reads=['scT'], writes=['scT'])
            S.op('dve', lambda e: e.reciprocal(out=scT[:], in_=scT[:]), reads=['scT'], writes=['scT'])
            S.op('dve', lambda e: e.tensor_tensor(out=scT[:], in0=scT[:], in1=PP('ccT'), op=ALU.mult), reads=['scT', 'pp'], writes=['scT'])
            for k in range(8):
                S.op('dve', lambda e, k=k: e.tensor_scalar(out=scB[:, k, :], in0=C('one128'), scalar1=scT[:, 2 * k:2 * k + 1],
                                                            scalar2=None, op0=ALU.mult),
                     reads=['scT', 'cp'], writes=['scB'])
            w_ada_v = w_ada.rearrange("(k p) n -> p k n", p=128)
            for cb in range(4):
                wt = wa[cb % 2]
                wk = 'wa%d' % (cb % 2)
                S.dma('sp' if cb % 2 == 0 else 'act', lambda e, wt=wt, cb=cb: e.dma_start(out=wt[:], in_=w_ada_v[:, :, cb * 512:(cb + 1) * 512]),
                      writes=[wk])
                if cb < 4:
                    for bl in range(4):
                        blk = cb * 4 + bl
                        for k in range(8):
                            S.op('pe', lambda e, k=k, bl=bl, blk=blk, wt=wt: e.matmul(
                                P[0][:, blk * 2:blk * 2 + 2], lhsT=wt[:, k, bl * 128:(bl + 1) * 128],
                                rhs=scT[:, 2 * k:2 * k + 2], start=(k == 0), stop=(k == 7)),
                                reads=[wk, 'scT'], writes=['P0'], acc=True)
            for n in range(2):
                S.op('dve', lambda e, n=n: e.tensor_tensor(out=modfm[:, :, n], in0=P[0][:, 0:32].rearrange("p (b n) -> p b n", n=2)[:, :, n],
                                                           in1=PP('bada_fm'), op=ALU.add),
                     reads=['P0', 'pp'], writes=['modfm'])
            for n in range(2):
                S.op('dve', lambda e, n=n: e.scalar_tensor_tensor(out=A1[:, :, n], in0=modfm[:, 8:16, n], scalar=1.0, in1=PP('gmix_fm'),
                                                                  op0=ALU.add, op1=ALU.mult),
                     reads=['modfm', 'pp'], writes=['A1'])
            S.op('dve', lambda e: e.tensor_tensor(out=lbs[:, 2:4], in0=PP('lbl_fm', 2, 4), in1=PP('lbl_fm', 0, 2), op=ALU.subtract),
                 reads=['pp'], writes=['lbs'])
            S.op('act', lambda e: e.activation(out=lbs[:, 0:2], in_=lbs[:, 2:4], func=AF.Exp, scale=-1.0), reads=['lbs'], writes=['lbs'])
            S.op('dve', lambda e: e.tensor_scalar(out=lbs[:, 0:2], in0=lbs[:, 0:2], scalar1=1.0, scalar2=None, op0=ALU.add), reads=['lbs'], writes=['lbs'])
            S.op('dve', lambda e: e.tensor_tensor(out=omlrow[:], in0=PP('lbl_row', 256, 512), in1=PP('lbl_row', 0, 256), op=ALU.subtract),
                 reads=['pp'], writes=['omlrow'])
            S.op('act', lambda e: e.activation(out=omlrow[:], in_=omlrow[:], func=AF.Exp, scale=-1.0), reads=['omlrow'], writes=['omlrow'])
            S.op('dve', lambda e: e.tensor_scalar(out=omlrow[:], in0=omlrow[:], scalar1=1.0, scalar2=None, op0=ALU.add), reads=['omlrow'], writes=['omlrow'])
            if dbg and 'modfm' in dbg:
                S.dma('sp', lambda e: e.dma_start(out=dbg_out['modfm'], in_=modfm[:].rearrange("p a b -> p (a b)")), reads=['modfm'])
            S.barrier()

        with ExitStack() as ph:
            win = sb(ph, "win", [128, 8, 768], BF16)
            with ExitStack() as ph0:
                winf = sb(ph0, "winf", [128, 8, 768])
                S.dma('act', lambda e: e.dma_start(out=winf[:], in_=w_in.rearrange("(k p) n -> p k n", p=128)), writes=['winf'])
                for k in range(8):
                    if k % 2 == 0:
                        S.op('act', lambda e: e.copy(out=win[:, k, :], in_=winf[:, k, :]), reads=['winf'], writes=['win'])
                    else:
                        S.op('dve', lambda e: e.tensor_copy(out=win[:, k, :], in_=winf[:, k, :]), reads=['winf'], writes=['win'])
                S.barrier()
            xt = [sb(ph, "xt%d" % i, [128, D]) for i in range(4)]
            xn = [sb(ph, "xn%d" % i, [128, D], BF16) for i in range(2)]
            identb1 = sb(ph, "identb1", [128, 128], BF16)
            S.op('dve', lambda e: e.tensor_copy(out=identb1[:], in_=ident), reads=['cp'], writes=['identb1'])
            junk = sb(ph, "junk", [128, D])
            hT = [sb(ph, "hT%d" % i, [128, 8, 512], BF16) for i in range(2)]
            ofm = [sb(ph, "ofm%d" % i, [128, 512]) for i in range(4)]
            otm = [sb(ph, "otm%d" % i, [128, 128], BF16) for i in range(6)]
            ofm_i = [0]
            otm_i = [0]
            tcount = [0]

            st4 = sb(ph, "st4r", [128, 4, 4])
            otw = [sb(ph, "otw%d" % i, [128, 256]) for i in range(4)]
            otw_i = [0]

            def stageA(gi, src, ntile, n):
                hb = hT[gi % 2]
                hk = 'hT%d' % (gi % 2)
                for i in range(ntile):
                    ti = tcount[0]
                    tcount[0] += 1
                    xb, xk = xt[ti % 4], 'xt%d' % (ti % 4)
                    nb, nk = xn[ti % 2], 'xn%d' % (ti % 2)
                    sk = 'st4_%d' % i
                    S.dma('sp', lambda e: e.dma_start(out=xb[:], in_=src[i * 128:(i + 1) * 128, :]), writes=[xk])
                    S.op('act', lambda e: e.activation(out=junk[:], in_=xb[:], func=AF.Square, accum_out=st4[:, i, 0:1]),
                         reads=[xk], writes=['junk', sk])
                    S.op('act', lambda e: e.activation(out=st4[:, i, 1:2], in_=st4[:, i, 0:1], func=AF.Ln, scale=1.0 / D, bias=EPS),
                         reads=[sk], writes=[sk])
                    S.op('act', lambda e: e.activation(out=st4[:, i, 2:3], in_=st4[:, i, 1:2], func=AF.Exp, scale=-0.5),
                         reads=[sk], writes=[sk])
                    S.op('dve', lambda e: e.tensor_scalar(out=nb[:], in0=xb[:], scalar1=st4[:, i, 2:3], scalar2=None, op0=ALU.mult),
                         reads=[xk, sk], writes=[nk])
                    for half in range(2):
                        bi_ = half + 6 * (i % 2)
                        pb, pk = P[bi_][:, :].bitcast(BF16), PK[bi_]
                        for kk in range(4):
                            k = half * 4 + kk
                            S.op('pe', lambda e: e.transpose(pb[:, kk * 128:(kk + 1) * 128], nb[:, k * 128:(k + 1) * 128], identb1[:]),
                                 reads=[nk, 'identb1'], writes=[pk], acc=True)
                        for kk in range(4):
                            k = half * 4 + kk
                            if half == 0:
                                S.op('dve', lambda e: e.tensor_scalar(
                                    out=hb[:, k, i * 128:(i + 1) * 128], in0=pb[:, kk * 128:(kk + 1) * 128],
                                    scalar1=A1[:, k, n:n + 1], scalar2=modfm[:, k, n:n + 1], op0=ALU.mult, op1=ALU.add),
                                    reads=[pk, 'A1', 'modfm'], writes=[(hk, k)])
                            else:
                                S.op('act', lambda e: e.activation(
                                    out=hb[:, k, i * 128:(i + 1) * 128], in_=pb[:, kk * 128:(kk + 1) * 128], func=AF.Identity,
                                    scale=A1[:, k, n:n + 1], bias=modfm[:, k, n:n + 1]),
                                    reads=[pk, 'A1', 'modfm'], writes=[(hk, k)])
                    yield

            def stageB(gi, ntile, col0, latent):
                hb = hT[gi % 2]
                hk = 'hT%d' % (gi % 2)
                ncol = ntile * 128
                blks = [0, 1, 3, 4, 5] if latent else [3, 4]
                for bi, blk in enumerate(blks):
                    pb, pk = P[2 + bi % 2], PK[2 + bi % 2]
                    for k in range(8):
                        S.op('pe', lambda e: e.matmul(pb[:, 0:ncol], lhsT=win[:, k, blk * 128:(blk + 1) * 128],
                                                      rhs=hb[:, k, 0:ncol], start=(k == 0), stop=(k == 7)),
                             reads=['win', (hk, k)], writes=[pk], acc=True)
                    ob, ok = ofm[ofm_i[0] % 4], 'ofm%d' % (ofm_i[0] % 4)
                    ofm_i[0] += 1
                    if blk == 0:
                        S.op('act', lambda e: e.copy(out=ob[:, 0:ncol], in_=pb[:, 0:ncol]), reads=[pk], writes=[ok])
                        dst = uT_d[:, col0 - LC:col0 - LC + ncol]
                    elif blk in (1, 5):
                        S.op('act', lambda e: e.activation(out=ob[:, 0:ncol], in_=pb[:, 0:ncol], func=AF.Exp, scale=-1.0), reads=[pk], writes=[ok])
                        S.op('dve', lambda e: e.tensor_scalar(out=ob[:, 0:ncol], in0=ob[:, 0:ncol], scalar1=1.0, scalar2=None, op0=ALU.add),
                             reads=[ok], writes=[ok])
                        S.op('act', lambda e: e.activation(out=ob[:, 0:ncol], in_=ob[:, 0:ncol], func=AF.Ln), reads=[ok], writes=[ok])
                        S.op('act', lambda e: e.activation(out=ob[:, 0:ncol], in_=ob[:, 0:ncol], func=AF.Exp, scale=-1.0), reads=[ok], writes=[ok])
                        S.op('dve', lambda e: e.tensor_tensor(out=ob[:, 0:ncol], in0=ob[:, 0:ncol], in1=pb[:, 0:ncol], op=ALU.mult),
                             reads=[ok, pk], writes=[ok])
                        dst = (qT_d if blk == 1 else sgT_d)[:, col0 - LC:col0 - LC + ncol]
                    else:
                        d_ = blk - 3
                        S.op('act', lambda e: e.activation(out=ob[:, 0:ncol], in_=pb[:, 0:ncol], func=AF.Exp), reads=[pk], writes=[ok])
                        S.op('dve', lambda e: e.tensor_scalar(out=ob[:, 0:ncol], in0=ob[:, 0:ncol], scalar1=lbs[:, d_:d_ + 1], scalar2=lbs[:, d_:d_ + 1],
                                                              op0=ALU.mult, op1=ALU.add),
                             reads=[ok, 'lbs'], writes=[ok])
                        S.op('act', lambda e: e.activation(out=ob[:, 0:ncol], in_=ob[:, 0:ncol], func=AF.Ln), reads=[ok], writes=[ok])
                        S.op('act', lambda e: e.activation(out=ob[:, 0:ncol], in_=ob[:, 0:ncol], func=AF.Exp, scale=-1.0), reads=[ok], writes=[ok])
                        dst = kT_d[d_][:, col0:col0 + ncol]
                    S.dma('pool', lambda e: e.dma_start(out=dst, in_=ob[:, 0:ncol]), reads=[ok])
                    yield
                for i in range(ntile):
                    pb, pk = P[4 + i % 2], PK[4 + i % 2]
                    for k in range(8):
                        S.op('pe', lambda e: e.matmul(pb[:, 0:384], lhsT=hb[:, k, i * 128:(i + 1) * 128], rhs=win[:, k, 256:640],
                                                      start=(k == 0), stop=(k == 7)),
                             reads=['win', (hk, k)], writes=[pk], acc=True)
                    r0 = col0 + i * 128
                    ob, ok = otm[otm_i[0] % 6], 'otm%d' % (otm_i[0] % 6)
                    otm_i[0] += 1
                    S.op('act', lambda e: e.copy(out=ob[:], in_=pb[:, 0:128]), reads=[pk], writes=[ok])
                    S.dma('pool', lambda e: e.dma_start(out=v_d[r0:r0 + 128, :], in_=ob[:]), reads=[ok])
                    kb, kk_ = otw[otw_i[0] % 4], 'otw%d' % (otw_i[0] % 4)
                    lb_, lk_ = otw[(otw_i[0] + 1) % 4], 'otw%d' % ((otw_i[0] + 1) % 4)
                    otw_i[0] += 2
                    S.op('act', lambda e: e.activation(out=kb[:], in_=pb[:, 128:384], func=AF.Exp), reads=[pk], writes=[kk_])
                    S.op('dve', lambda e: e.scalar_tensor_tensor(out=kb[:], in0=kb[:], scalar=1.0, in1=omlrow[:], op0=ALU.add, op1=ALU.mult),
                         reads=[kk_, 'omlrow'], writes=[kk_])
                    S.op('act', lambda e: e.activation(out=kb[:], in_=kb[:], func=AF.Ln), reads=[kk_], writes=[kk_])
                    S.op('act', lambda e: e.activation(out=kb[:], in_=kb[:], func=AF.Exp, scale=-1.0), reads=[kk_], writes=[kk_])
                    S.op('act', lambda e: e.activation(out=lb_[:], in_=kb[:], func=AF.Ln, scale=-1.0, bias=1.0), reads=[kk_], writes=[lk_])
                    for d_ in range(2):
                        S.dma('pool', lambda e: e.dma_start(out=k_d[d_][r0:r0 + 128, :], in_=kb[:, d_ * 128:(d_ + 1) * 128]), reads=[kk_])
                        S.dma('pool', lambda e: e.dma_start(out=lf_d[d_][r0:r0 + 128, :], in_=lb_[:, d_ * 128:(d_ + 1) * 128]), reads=[lk_])
                    yield

            ngl = 16 if not (dbg and dbg.get('_short')) else 1
            plan = [(ctx_b, 2, 1, 0, False)] + [(x_b[g * 512:(g + 1) * 512, :], 4, 0, LC + g * 512, True) for g in range(ngl)]
            def drain(gen, n=10 ** 9):
                for _ in range(n):
                    try:
                        next(gen)
                    except StopIteration:
                        return True
                return False

            drain(stageA(0, plan[0][0], plan[0][1], plan[0][2]))
            for gi in range(len(plan)):
                ga = stageA(gi + 1, plan[gi + 1][0], plan[gi + 1][1], plan[gi + 1][2]) if gi + 1 < len(plan) else iter(())
                gb = stageB(gi, plan[gi][1], plan[gi][3], plan[gi][4])
                da = db = False
                while not (da and db):
                    if not db:
                        db = drain(gb, 2)
                    if not da:
                        da = drain(ga, 1)
            S.barrier()

        def dbgdump(name, src_ap, rows=None):
            if dbg and name in dbg:
                S.barrier(cc=True)
                S.dma('sp', lambda e: e.dma_start(out=dbg_out[name], in_=src_ap))
                S.barrier()

        stop = (dbg or {}).get('_stop', 99)
        if stop >= 2:
          with ExitStack() as ph:
            Xs = sb(ph, "Xs", [128, 64, 256])
            Wcat = sb(ph, "Wcat", [128, 256])
            wf = sb(ph, "wf", [128, 128])
            ft = [sb(ph, "ft%d" % i, [128, 512]) for i in range(6)]
            with ExitStack() as ph2:
                uT = sb(ph2, "uT", [128, L])
                for i in range(4):
                    S.dma('sp' if i % 2 == 0 else 'act', lambda e: e.dma_start(out=uT[:, i * 2048:(i + 1) * 2048], in_=uT_d[:, i * 2048:(i + 1) * 2048]),
                          writes=['uT'])
                S.dma('act', lambda e: e.dma_start(out=wf[:], in_=w_f), writes=['wf'])
                S.op('pe', lambda e: e.matmul(P[0][:, 0:128], lhsT=C('CCs'), rhs=wf[:], start=True, stop=True), reads=['cp', 'wf'], writes=['P0'], acc=True)
                S.op('pe', lambda e: e.matmul(P[0][:, 128:256], lhsT=C('SCs'), rhs=wf[:], start=True, stop=True), reads=['cp', 'wf'], writes=['P0'], acc=True)
                S.op('dve', lambda e: e.tensor_copy(out=Wcat[:], in_=P[0][:, 0:256]), reads=['P0'], writes=['Wcat'])
                uT3 = uT[:].rearrange("p (t1 t2) -> p t2 t1", t2=64)
                for t2p in range(32):
                    pb, pk = P[1 + t2p % 2], PK[1 + t2p % 2]
                    for h in range(2):
                        S.op('pe', lambda e: e.matmul(pb[:, h * 256:(h + 1) * 256], lhsT=uT3[:, 2 * t2p + h, :], rhs=Wcat[:], start=True, stop=True),
                             reads=['uT', 'Wcat'], writes=[pk], acc=True)
                    dst = Xs[:, 2 * t2p:2 * t2p + 2, :]
                    src = pb[:, 0:512].rearrange("p (a b) -> p a b", b=256)
                    if t2p % 2 == 0:
                        S.op('dve', lambda e: e.tensor_copy(out=dst, in_=src), reads=[pk], writes=['Xs0'])
                    else:
                        S.op('act', lambda e: e.copy(out=dst, in_=src), reads=[pk], writes=['Xs1'])
                S.barrier()
            yT = sb(ph, "yT", [128, L], BF16)
            for q in range(16):
                XA = Xs[:, 4 * q:4 * q + 4, 0:128]
                XB = Xs[:, 4 * q:4 * q + 4, 128:256]
                pr, prk = P[3 + (q % 2) * 2], PK[3 + (q % 2) * 2]
                pi, pik = P[4 + (q % 2) * 2], PK[4 + (q % 2) * 2]
                pr3 = pr[:, :].rearrange("p (a c) -> p a c", c=128)
                pi3 = pi[:, :].rearrange("p (a c) -> p a c", c=128)
                S.op('pe', lambda e: e.matmul(pr3, lhsT=C('C128'), rhs=XA, start=True, stop=False), reads=['cp', 'Xs0', 'Xs1'], writes=[prk], acc=True)
                S.op('pe', lambda e: e.matmul(pr3, lhsT=C('nS128'), rhs=XB, start=False, stop=True), reads=['cp', 'Xs0', 'Xs1'], writes=[prk], acc=True)
                S.op('pe', lambda e: e.matmul(pi3, lhsT=C('C128'), rhs=XB, start=True, stop=False), reads=['cp', 'Xs0', 'Xs1'], writes=[pik], acc=True)
                S.op('pe', lambda e: e.matmul(pi3, lhsT=C('S128'), rhs=XA, start=False, stop=True), reads=['cp', 'Xs0', 'Xs1'], writes=[pik], acc=True)
                Tcb = C('Tc')[:, 4 * q:4 * q + 4].unsqueeze(2).to_broadcast([128, 4, 128])
                Tsb = C('Ts')[:, 4 * q:4 * q + 4].unsqueeze(2).to_broadcast([128, 4, 128])
                f3 = [t[:].rearrange("p (a c) -> p a c", c=128) for t in ft]
                S.op('dve', lambda e: e.tensor_tensor(out=f3[0], in0=pr3, in1=Tcb, op=ALU.mult), reads=[prk, 'cp'], writes=['ft0'])
                S.op('dve', lambda e: e.tensor_tensor(out=f3[1], in0=pi3, in1=Tsb, op=ALU.mult), reads=[pik, 'cp'], writes=['ft1'])
                S.op('pool', lambda e: e.tensor_tensor(out=f3[2], in0=f3[0], in1=f3[1], op=ALU.subtract), reads=['ft0', 'ft1'], writes=['ft2'])
                S.op('dve', lambda e: e.tensor_tensor(out=f3[3], in0=pi3, in1=Tcb, op=ALU.mult), reads=[pik, 'cp'], writes=['ft3'])
                S.op('dve', lambda e: e.tensor_tensor(out=f3[4], in0=pr3, in1=Tsb, op=ALU.mult), reads=[prk, 'cp'], writes=['ft4'])
                S.op('pool', lambda e: e.tensor_tensor(out=f3[5], in0=f3[3], in1=f3[4], op=ALU.add), reads=['ft3', 'ft4'], writes=['ft5'])
                S.dma('pool', lambda e: e.dma_start(out=Y_d[0][:, 4 * q:4 * q + 4, :], in_=f3[2]), reads=['ft2'])
                S.dma('pool', lambda e: e.dma_start(out=Y_d[1][:, 4 * q:4 * q + 4, :], in_=f3[5]), reads=['ft5'])
            S.barrier()
            yr = [sb(ph, "yr%d" % i, [64, 8, 128]) for i in range(2)]
            yi = [sb(ph, "yi%d" % i, [64, 8, 128]) for i in range(2)]
            Yv = [Y_d[i].rearrange("k t c -> t k c") for i in range(2)]
            yTv = yT[:].rearrange("p (k2 k1) -> p k1 k2", k1=128)
            for g in range(16):
                a_, ak = yr[g % 2], 'yr%d' % (g % 2)
                b_, bk = yi[g % 2], 'yi%d' % (g % 2)
                S.dma('sp', lambda e: e.dma_start(out=a_[:], in_=Yv[0][:, 8 * g:8 * g + 8, :]), writes=[ak])
                S.dma('sp', lambda e: e.dma_start(out=b_[:], in_=Yv[1][:, 8 * g:8 * g + 8, :]), writes=[bk])
                pb, pk = P[g % 2], PK[g % 2]
                for kk in range(8):
                    S.op('pe', lambda e: e.matmul(pb[:, kk * 64:(kk + 1) * 64], lhsT=a_[:, kk, :], rhs=C('C64'), start=True, stop=False),
                         reads=[ak, 'cp'], writes=[pk], acc=True)
                    S.op('pe', lambda e: e.matmul(pb[:, kk * 64:(kk + 1) * 64], lhsT=b_[:, kk, :], rhs=C('nS64'), start=False, stop=True),
                         reads=[bk, 'cp'], writes=[pk], acc=True)
                k1a = 8 * g
                src = pb[:, 0:512].rearrange("p (a b) -> p a b", b=64)
                if g % 2 == 0:
                    S.op('dve', lambda e: e.tensor_copy(out=yTv[:, k1a:k1a + 8, :], in_=src), reads=[pk], writes=['yT0'])
                else:
                    S.op('act', lambda e: e.copy(out=yTv[:, k1a:k1a + 8, :], in_=src), reads=[pk], writes=['yT1'])
            for jj in range(4):
                S.dma('sp', lambda e: e.dma_start(out=mix_d[jj * 128:jj * 128 + 128, :], in_=yT[:, jj * 2048:(jj + 1) * 2048]), reads=['yT0', 'yT1'],
                      writes=['mixf%d' % jj])
            if stop >= 3.5:
                ag_mix(0, ['mixf0', 'mixf1'])
                ag_mix(1, ['mixf2', 'mixf3'])
            S.barrier()

        if stop >= 3:
          with ExitStack() as ph:
            oT = sb(ph, "oT", [128, L])
            Sst = [sb(ph, "Sst%d" % i, [128, 128]) for i in range(2)]
            qTg = [sb(ph, "qTg%d" % i, [128, 512]) for i in range(2)]
            kTg = [sb(ph, "kTg%d" % i, [128, 512]) for i in range(2)]
            vg = [sb(ph, "vg%d" % i, [64, 8, 128], BF16) for i in range(2)]
            Sb = [sb(ph, "Sb%d" % i, [128, 128], BF16) for i in range(2)]
            lfg = [sb(ph, "lfg%d" % i, [64, 8, 128]) for i in range(2)]
            ktg = [sb(ph, "ktg%d" % i, [64, 8, 128]) for i in range(2)]
            eB = [sb(ph, "eB%d" % i, [128, 512]) for i in range(2)]
            enB = [sb(ph, "enB%d" % i, [128, 512]) for i in range(2)]
            eR = [sb(ph, "eR%d" % i, [64, 8, 128]) for i in range(2)]
            Qt = [sb(ph, "Qt%d" % i, [128, 512], BF16) for i in range(2)]
            Kt = [sb(ph, "Kt%d" % i, [128, 512], BF16) for i in range(2)]
            Kh = [sb(ph, "Kh%d" % i, [64, 8, 128], BF16) for i in range(2)]
            ATm = [sb(ph, "ATm%d" % i, [64, 8, 64], BF16) for i in range(2)]
            ngl = 16 if not (dbg and dbg.get('_short')) else 1
            scur = [0]

            def pre1(g):
                gi, nch, latent, tok0, d_ = g['gi'], g['nch'], g['latent'], g['tok0'], g['d']
                nt = nch * 64
                tri = C('TriF') if d_ == 0 else C('TriB')
                slo = C('SLoF') if d_ == 0 else C('SLoB')
                S.dma('sp', lambda e: e.dma_start(out=lfg[gi][:, 0:nch, :], in_=lf_d[d_][tok0:tok0 + nt, :].rearrange("(c s) d -> s c d", s=64)),
                      writes=['lfg%d' % gi])
                S.dma('sp', lambda e: e.dma_start(out=ktg[gi][:, 0:nch, :], in_=k_d[d_][tok0:tok0 + nt, :].rearrange("(c s) d -> s c d", s=64)),
                      writes=['ktg%d' % gi])
                S.dma('sp', lambda e: e.dma_start(out=vg[gi][:, 0:nch, :], in_=v_d[tok0:tok0 + nt, :].rearrange("(c s) d -> s c d", s=64)),
                      writes=['vg%d' % gi])
                if latent:
                    S.dma('sp', lambda e: e.dma_start(out=kTg[gi][:, 0:nt], in_=kT_d[d_][:, tok0:tok0 + nt]), writes=['kTg%d' % gi])
                    S.dma('sp', lambda e: e.dma_start(out=qTg[gi][:, 0:nt], in_=qT_d[:, tok0 - LC:tok0 - LC + nt]), writes=['qTg%d' % gi])
                for ch in range(nch):
                    S.op('pe', lambda e: e.matmul(P[0][:, ch * 64:(ch + 1) * 64], lhsT=lfg[gi][:, ch, :], rhs=tri, start=True, stop=True),
                         reads=['lfg%d' % gi, 'cp'], writes=['P0'], acc=True)
                S.op('act', lambda e: e.activation(out=eB[gi][:, 0:nt], in_=P[0][:, 0:nt], func=AF.Exp), reads=['P0'], writes=['eB%d' % gi])
                if latent:
                    S.op('act', lambda e: e.activation(out=enB[gi][:, 0:nt], in_=P[0][:, 0:nt], func=AF.Exp, scale=-1.0), reads=['P0'], writes=['enB%d' % gi])
                for hf in range((nch + 3) // 4):
                    n4 = min(4, nch - hf * 4)
                    for c4 in range(n4):
                        S.op('pe', lambda e: e.matmul(P[1][0:64, c4 * 128:(c4 + 1) * 128], lhsT=slo, rhs=lfg[gi][:, hf * 4 + c4, :], start=True, stop=True),
                             reads=['lfg%d' % gi, 'cp'], writes=['P1'], acc=True)
                    S.op('act', lambda e: e.activation(out=eR[gi][:, hf * 4:hf * 4 + n4, :],
                                                       in_=P[1][0:64, 0:n4 * 128].rearrange("p (a b) -> p a b", b=128), func=AF.Exp),
                         reads=['P1'], writes=['eR%d' % gi])
                S.op('dve', lambda e: e.tensor_tensor(out=Kh[gi][:, 0:nch, :], in0=ktg[gi][:, 0:nch, :], in1=eR[gi][:, 0:nch, :], op=ALU.mult),
                     reads=['ktg%d' % gi, 'eR%d' % gi], writes=['Kh%d' % gi])
                if latent:
                    S.op('dve', lambda e: e.tensor_tensor(out=Qt[gi][:, 0:nt], in0=qTg[gi][:, 0:nt], in1=eB[gi][:, 0:nt], op=ALU.mult),
                         reads=['qTg%d' % gi, 'eB%d' % gi], writes=['Qt%d' % gi])
                    S.op('dve', lambda e: e.tensor_tensor(out=Kt[gi][:, 0:nt], in0=kTg[gi][:, 0:nt], in1=enB[gi][:, 0:nt], op=ALU.mult),
                         reads=['kTg%d' % gi, 'enB%d' % gi], writes=['Kt%d' % gi])

            def pre2(g):
                gi, nch, latent, d_ = g['gi'], g['nch'], g['latent'], g['d']
                tri = C('TriF') if d_ == 0 else C('TriB')
                if latent:
                    for ch in range(nch):
                        cs = slice(ch * 64, (ch + 1) * 64)
                        S.op('pe', lambda e: e.matmul(P[2][0:64, cs], lhsT=Kt[gi][:, cs], rhs=Qt[gi][:, cs], start=True, stop=True),
                             reads=['Kt%d' % gi, 'Qt%d' % gi], writes=['P2'], acc=True)
                    S.op('dve', lambda e: e.tensor_tensor(out=ATm[gi][:, 0:nch, :], in0=P[2][0:64, 0:nch * 64].rearrange("p (a b) -> p a b", b=64),
                                                          in1=tri.unsqueeze(1).to_broadcast([64, nch, 64]), op=ALU.mult),
                         reads=['P2', 'cp'], writes=['ATm%d' % gi])
                ub = g['ub']
                for ch in range(nch):
                    S.op('pe', lambda e: e.matmul(P[ub + ch // 4][:, (ch % 4) * 128:(ch % 4 + 1) * 128], lhsT=Kh[gi][:, ch, :], rhs=vg[gi][:, ch, :],
                                                  start=True, stop=True),
                         reads=['Kh%d' % gi, 'vg%d' % gi], writes=[PK[ub + ch // 4]], acc=True)

            def seq(g, chs):
                gi, latent, d_, ub = g['gi'], g['latent'], g['d'], g['ub']
                for ch in chs:
                    cs = slice(ch * 64, (ch + 1) * 64)
                    cur = scur[0]
                    if latent:
                        S.op('act', lambda e: e.copy(out=Sb[cur][:], in_=Sst[cur][:]), reads=['S%d' % cur], writes=['Sb%d' % cur])
                        S.op('pe', lambda e: e.matmul(P[7][:, cs], lhsT=Sb[cur][:], rhs=Qt[gi][:, cs], start=True, stop=False),
                             reads=['Sb%d' % cur, 'Qt%d' % gi], writes=['P7'], acc=True)
                        S.op('pe', lambda e: e.matmul(P[7][:, cs], lhsT=vg[gi][:, ch, :], rhs=ATm[gi][:, ch, :], start=False, stop=True),
                             reads=['vg%d' % gi, 'ATm%d' % gi], writes=['P7'], acc=True)
                    dc = ch * 64 + (63 if d_ == 0 else 0)
                    S.op('dve', lambda e: e.scalar_tensor_tensor(out=Sst[1 - cur][:], in0=Sst[cur][:], scalar=eB[gi][:, dc:dc + 1],
                                                                 in1=P[ub + ch // 4][:, (ch % 4) * 128:(ch % 4 + 1) * 128], op0=ALU.mult, op1=ALU.add),
                         reads=['S%d' % cur, 'eB%d' % gi, PK[ub + ch // 4]], writes=['S%d' % (1 - cur)])
                    scur[0] = 1 - cur

            def fin(g):
                if not g['latent']:
                    return
                nt = g['nch'] * 64
                c0 = g['tok0'] - LC
                if g['d'] == 0:
                    S.op('act', lambda e: e.copy(out=oT[:, c0:c0 + nt], in_=P[7][:, 0:nt]), reads=['P7'], writes=['oT'])
                else:
                    S.op('dve', lambda e: e.tensor_tensor(out=oT[:, c0:c0 + nt], in0=oT[:, c0:c0 + nt], in1=P[7][:, 0:nt], op=ALU.add),
                         reads=['P7', 'oT'], writes=['oT'])

            gcount = 0
            for d_ in range(2):
                S.op('dve', lambda e: e.memset(Sst[scur[0]][:], 0.0), writes=['S%d' % scur[0]])
                glist = []
                for (tok0, nch, latent) in [(0, 4, False)] + [(LC + g * 512, 8, True) for g in (range(ngl) if d_ == 0 else range(ngl - 1, -1, -1))]:
                    glist.append(dict(gi=gcount % 2, ub=3 + 2 * (gcount % 2), nch=nch, latent=latent, tok0=tok0, d=d_))
                    gcount += 1
                pre1(glist[0])
                pre2(glist[0])
                for ix, g in enumerate(glist):
                    order = list(range(g['nch'])) if d_ == 0 else list(range(g['nch'] - 1, -1, -1))
                    hlf = len(order) // 2
                    nxt = glist[ix + 1] if ix + 1 < len(glist) else None
                    if nxt:
                        pre1(nxt)
                    seq(g, order[:hlf])
                    if nxt:
                        pre2(nxt)
                    seq(g, order[hlf:])
                    fin(g)
            rt = [sb(ph, "rt%d" % i, [128, 512]) for i in range(3)]
            rb = [sb(ph, "rb%d" % i, [128, 512], BF16) for i in range(2)]
            sgt = [sb(ph, "sgt%d" % i, [128, 512]) for i in range(2)]
            for g in range(ngl):
                cs = slice(g * 512, (g + 1) * 512)
                S.dma('sp', lambda e: e.dma_start(out=sgt[g % 2][:], in_=sgT_d[:, cs]), writes=['sgt%d' % (g % 2)])
                S.op('act', lambda e: e.activation(out=rt[0][:], in_=oT[:, cs], func=AF.Square), reads=['oT'], writes=['rt0'])
                S.op('pe', lambda e: e.matmul(P[5][:, :], lhsT=C('avg128'), rhs=rt[0][:], start=True, stop=True), reads=['cp', 'rt0'], writes=['P5'], acc=True)
                S.op('act', lambda e: e.activation(out=rt[1][:], in_=P[5][:, :], func=AF.Ln, bias=EPS, scale=1.0), reads=['P5'], writes=['rt1'])
                S.op('act', lambda e: e.activation(out=rt[1][:], in_=rt[1][:], func=AF.Exp, scale=-0.5), reads=['rt1'], writes=['rt1'])
                S.op('dve', lambda e: e.tensor_tensor(out=rt[2][:], in0=oT[:, cs], in1=rt[1][:], op=ALU.mult), reads=['oT', 'rt1'], writes=['rt2'])
                ob = rb[g % 2]
                okk = 'rb%d' % (g % 2)
                S.op('dve', lambda e: e.scalar_tensor_tensor(out=ob[:], in0=rt[2][:], scalar=PP('grec_fm'), in1=sgt[g % 2][:], op0=ALU.mult, op1=ALU.mult),
                     reads=['rt2', 'pp', 'sgt%d' % (g % 2)], writes=[okk])
                jj = g // 4
                S.dma('sp', lambda e: e.dma_start(out=mix_d[512 + jj * 128:512 + jj * 128 + 128, (g % 4) * 512:(g % 4 + 1) * 512], in_=ob[:]),
                      reads=[okk], writes=['mixr%d' % g])
                if stop >= 3.5 and g % 8 == 7:
                    ag_mix(2 + g // 8, ['mixr%d' % gg for gg in range(g - 7, g + 1)])
            S.barrier()

        if stop >= 4:
          with ExitStack() as ph:
            wo = sb(ph, "wo", [128, 8, D], BF16)
            imix = sb(ph, "imix", [128, 8], I32)
            mT = sb(ph, "mT", [128, 8, 2048], BF16)
            affT = sb(ph, "affT", [16, 2048])
            xt3 = [sb(ph, "x3t%d" % i, [128, D]) for i in range(2)]
            x1t = [sb(ph, "x1t%d" % i, [128, D]) for i in range(2)]
            h2t = [sb(ph, "h2t%d" % i, [128, D]) for i in range(2)]
            h2b = [sb(ph, "h2b%d" % i, [128, D], BF16) for i in range(2)]
            h2T = sb(ph, "h2T", [128, 8, 128])
            junk3 = sb(ph, "junk3", [128, D])
            st3 = sb(ph, "st3", [128, 8])
            eT = sb(ph, "eT", [16, 128])
            rs16 = sb(ph, "rs16", [16, 128])
            S.dma('sp', lambda e: e.dma_start(out=imix[:], in_=idx_mix_d), writes=['imix'])
            with ExitStack() as ph0:
                wa = [sb(ph0, "wa3%d" % i, [128, 8, 512]) for i in range(2)]
                brows = sb(ph0, "brows", [128, 4096])
                S.dma('act', lambda e: e.dma_start(out=brows[:], in_=bada_rows_d), writes=['brows'])
                w_ada_v = w_ada.rearrange("(k p) n -> p k n", p=128)
                for cb in range(4, 12):
                    wt = wa[cb % 2]
                    wk = 'wa%d' % (cb % 2)
                    S.dma('sp' if cb % 2 == 0 else 'act', lambda e: e.dma_start(out=wt[:], in_=w_ada_v[:, :, cb * 512:(cb + 1) * 512]), writes=[wk])
                    pb = P[1 + (cb % 2)]
                    pk = PK[1 + (cb % 2)]
                    for k in range(8):
                        S.op('pe', lambda e: e.matmul(pb[:, :], lhsT=scB[:, k, :], rhs=wt[:, k, :], start=(k == 0), stop=(k == 7)),
                             reads=[wk, 'scB'], writes=[pk], acc=True)
                    o = (cb - 4) * 512
                    S.op('dve', lambda e: e.tensor_tensor(out=rows[:, o:o + 512], in0=pb[:, :], in1=brows[:, o:o + 512], op=ALU.add),
                         reads=[pk, 'brows'], writes=['rows'])
                S.op('dve', lambda e: e.scalar_tensor_tensor(out=rows[:, 2048:3072], in0=rows[:, 2048:3072], scalar=1.0, in1=PP('gffn_row'),
                                                             op0=ALU.add, op1=ALU.mult),
                     reads=['rows', 'pp'], writes=['rows'])
                S.barrier()
            with ExitStack() as ph0:
                wof = sb(ph0, "wof", [128, 8, D])
                S.dma('sp', lambda e: e.dma_start(out=wof[:], in_=w_out.rearrange("(k p) n -> p k n", p=128)), writes=['wof'])
                for k in range(8):
                    if k % 2 == 0:
                        S.op('act', lambda e: e.copy(out=wo[:, k, :], in_=wof[:, k, :]), reads=['wof'], writes=['wo'])
                    else:
                        S.op('dve', lambda e: e.tensor_copy(out=wo[:, k, :], in_=wof[:, k, :]), reads=['wof'], writes=['wo'])
                S.barrier()
            for k in range(8):
                S.dma('pool', lambda e: e.indirect_dma_start(out=mT[:, k, :], out_offset=None, in_=mixall_d,
                                                            in_offset=bass.IndirectOffsetOnAxis(ap=imix[:, k:k + 1], axis=0)),
                      reads=['imix', 'mixall'], writes=['mT'])
            wr = PP('wr').rearrange("p (k e) -> p k e", e=16)
            def p3A(i):
                xb, xk = xt3[i % 2], 'x3t%d' % (i % 2)
                x1, x1k = x1t[i % 2], 'x1t%d' % (i % 2)
                h2, h2k = h2t[i % 2], 'h2t%d' % (i % 2)
                sk = 'st3_%d' % (i % 2)
                st = st3[:, (i % 2) * 4:(i % 2) * 4 + 4]
                ts_ = slice(i * 128, (i + 1) * 128)
                S.dma('sp', lambda e: e.dma_start(out=xb[:], in_=x_own[ts_, :]), writes=[xk])
                for half in range(2):
                    for k in range(8):
                        S.op('pe', lambda e: e.matmul(P[half][:, :], lhsT=mT[:, k, ts_], rhs=wo[:, k, half * 512:(half + 1) * 512],
                                                      start=(k == 0), stop=(k == 7)),
                             reads=['mT', 'wo'], writes=[PK[half]], acc=True)
                    hs = slice(half * 512, (half + 1) * 512)
                    S.op('dve', lambda e: e.tensor_tensor(out=x1[:, hs], in0=P[half][:, :], in1=rows[:, half * 512:(half + 1) * 512], op=ALU.mult),
                         reads=[PK[half], 'rows'], writes=[x1k])
                S.op('dve', lambda e: e.tensor_tensor(out=x1[:], in0=x1[:], in1=xb[:], op=ALU.add), reads=[x1k, xk], writes=[x1k])
                S.dma('sp', lambda e: e.dma_start(out=x1_d[ts_, :], in_=x1[:]), reads=[x1k])
                S.op('act', lambda e: e.activation(out=junk3[:], in_=x1[:], func=AF.Square, accum_out=st[:, 0:1]), reads=[x1k], writes=['junk3', sk])
                S.op('act', lambda e: e.activation(out=st[:, 1:2], in_=st[:, 0:1], func=AF.Ln, scale=1.0 / D, bias=EPS), reads=[sk], writes=[sk])
                S.op('act', lambda e: e.activation(out=st[:, 2:3], in_=st[:, 1:2], func=AF.Exp, scale=-0.5), reads=[sk], writes=[sk])
                S.op('dve', lambda e: e.scalar_tensor_tensor(out=h2[:], in0=x1[:], scalar=st[:, 2:3], in1=rows[:, 2048:3072], op0=ALU.mult, op1=ALU.mult),
                     reads=[x1k, sk, 'rows'], writes=[h2k])
                S.op('dve', lambda e: e.tensor_tensor(out=h2[:], in0=h2[:], in1=rows[:, 1024:2048], op=ALU.add), reads=[h2k, 'rows'], writes=[h2k])
                S.op('act', lambda e: e.copy(out=h2b[i % 2][:], in_=h2[:]), reads=[h2k], writes=['h2b%d' % (i % 2)])
                S.dma('sp', lambda e: e.dma_start(out=h2_d[ts_, :], in_=h2b[i % 2][:]), reads=['h2b%d' % (i % 2)], writes=['h2_dt%d' % i])
                if stop >= 5 and i % 4 == 3:
                    S.collective("AllGather", ALU.bypass, [h2_d[(i // 4) * 512:(i // 4 + 1) * 512, :]],
                                 [h2all_d[(i // 4) * 2048:(i // 4 + 1) * 2048, :]], reads=['h2_dt%d' % ii for ii in range(i - 3, i + 1)],
                                 writes=['h2all%d' % (i // 4)])

            def p3B(i):
                h2, h2k = h2t[i % 2], 'h2t%d' % (i % 2)
                ts_ = slice(i * 128, (i + 1) * 128)
                for half in range(2):
                    pb, pk = P[2 + half], PK[2 + half]
                    for kk in range(4):
                        k = half * 4 + kk
                        S.op('pe', lambda e: e.transpose(pb[:, kk * 128:(kk + 1) * 128], h2[:, k * 128:(k + 1) * 128], ident),
                             reads=[h2k, 'cp'], writes=[pk], acc=True)
                    dst = h2T[:, half * 4:(half + 1) * 4, :]
                    src = pb[:, :].rearrange("p (a b) -> p a b", b=128)
                    if half == 0:
                        S.op('act', lambda e: e.copy(out=dst, in_=src), reads=[pk], writes=['h2T0'])
                    else:
                        S.op('dve', lambda e: e.tensor_copy(out=dst, in_=src), reads=[pk], writes=['h2T1'])
                for k in range(8):
                    S.op('pe', lambda e: e.matmul(P[4][0:16, 0:128], lhsT=wr[:, k, :], rhs=h2T[:, k, :], start=(k == 0), stop=(k == 7)),
                         reads=['pp', 'h2T%d' % (k // 4)], writes=['P4'], acc=True)
                S.op('act', lambda e: e.activation(out=eT[:], in_=P[4][0:16, 0:128], func=AF.Exp), reads=['P4'], writes=['eT'])
                S.op('pe', lambda e: e.matmul(P[5][0:16, 0:128], lhsT=C('ones16'), rhs=eT[:], start=True, stop=True), reads=['cp', 'eT'], writes=['P5'], acc=True)
                S.op('dve', lambda e: e.reciprocal(out=rs16[:], in_=P[5][0:16, 0:128]), reads=['P5'], writes=['rs16'])
                S.op('dve', lambda e: e.tensor_tensor(out=affT[:, ts_], in0=eT[:], in1=rs16[:], op=ALU.mult), reads=['eT', 'rs16'], writes=['affT'])

            p3A(0)
            for i in range(16):
                if i + 1 < 16:
                    p3A(i + 1)
                p3B(i)
            S.dma('sp', lambda e: e.dma_start(out=affT_d, in_=affT[:]), reads=['affT'], writes=['affT_d'])
            if stop >= 5:
                S.collective("AllGather", ALU.bypass, [affT_d], [affall_d], reads=['affT_d'], writes=['affall'])
            S.barrier()
        dbgdump('x1', x1_d)
        dbgdump('h2', h2_d)
        dbgdump('affT', affT_d)

        si = [sb(top, "si%d" % i, [128, 64], I32) for i in range(4)]
        gidx = [sb(top, "gi%d" % i, [128, 64], I32) for i in range(4)]
        wpc = [sb(top, "wpc%d" % i, [128, 64]) for i in range(4)]
        if stop >= 5:
          with ExitStack() as ph:
            af = sb(ph, "af", [128, 1024])
            junk4 = sb(ph, "junk4", [128, 1024])
            bis = sb(ph, "bis", [128, 8])
            lo, hi, mid, cnt, ge, dd = [bis[:, i:i + 1] for i in range(6)]
            S.dma('sp', lambda e: e.dma_start(out=af[:], in_=affall_d.rearrange("a (h t) -> (a h) t", h=2)), reads=['affall'], writes=['af'])
            S.op('dve', lambda e: e.memset(lo, 0.0), writes=['bis'])
            S.op('dve', lambda e: e.memset(hi, 1.5), writes=['bis'])
            for it in range(28):
                S.op('dve', lambda e: e.tensor_tensor(out=mid, in0=lo, in1=hi, op=ALU.add), reads=['bis'], writes=['bis'])
                S.op('dve', lambda e: e.tensor_scalar(out=mid, in0=mid, scalar1=0.5, scalar2=None, op0=ALU.mult), reads=['bis'], writes=['bis'])
                S.op('dve', lambda e: e.tensor_scalar(out=junk4[:], in0=af[:], scalar1=mid, scalar2=0.0, op0=ALU.is_ge, op1=ALU.add, accum_out=cnt),
                     reads=['bis', 'af'], writes=['junk4', 'bis'])
                S.op('pe', lambda e: e.matmul(P[0][:, 0:1], lhsT=C('G'), rhs=cnt, start=True, stop=True), reads=['cp', 'bis'], writes=['P0'], acc=True)
                S.op('dve', lambda e: e.tensor_scalar(out=ge, in0=P[0][:, 0:1], scalar1=CAP - 0.5, scalar2=None, op0=ALU.is_ge), reads=['P0'], writes=['bis'])
                S.op('dve', lambda e: e.tensor_tensor(out=dd, in0=mid, in1=lo, op=ALU.subtract), reads=['bis'], writes=['bis'])
                S.op('dve', lambda e: e.scalar_tensor_tensor(out=lo, in0=dd, scalar=ge, in1=lo, op0=ALU.mult, op1=ALU.add), reads=['bis'], writes=['bis'])
                S.op('dve', lambda e: e.tensor_tensor(out=dd, in0=hi, in1=mid, op=ALU.subtract), reads=['bis'], writes=['bis'])
                S.op('dve', lambda e: e.scalar_tensor_tensor(out=hi, in0=dd, scalar=ge, in1=mid, op0=ALU.mult, op1=ALU.add), reads=['bis'], writes=['bis'])
            S.op('dve', lambda e: e.tensor_scalar(out=junk4[:], in0=af[:], scalar1=lo, scalar2=None, op0=ALU.is_ge), reads=['bis', 'af'], writes=['junk4'])
            S.op('dve', lambda e: e.tensor_tensor(out=af[:], in0=af[:], in1=junk4[:], op=ALU.mult), reads=['junk4', 'af'], writes=['af'])
            S.dma('sp', lambda e: e.dma_start(out=mk_d, in_=junk4[:]), reads=['junk4'], writes=['mk_d'])
            S.dma('act', lambda e: e.dma_start(out=wg_d, in_=af[:]), reads=['af'], writes=['wg_d'])
            imk = sb(ph, "imk", [64, 4], I32)
            S.dma('sp', lambda e: e.dma_start(out=imk[:], in_=idx_mk_d), writes=['imk'])
            mkT = sb(ph, "mkT", [64, 128])
            wgT = sb(ph, "wgT", [64, 128])
            mk = sb(ph, "mk", [128, 64])
            totb = sb(ph, "totb", [128, 64])
            slf = sb(ph, "slf", [128, 64])
            sl2 = sb(ph, "sl2", [128, 64])
            tot = sb(ph, "tot", [128, 1])
            mkv = mk_d.rearrange("q (c p) -> (q c) p", p=128)
            wgv = wg_d.rearrange("q (c p) -> (q c) p", p=128)
            for el in range(4):
                S.dma('pool', lambda e: e.indirect_dma_start(out=mkT[:, :], out_offset=None, in_=mkv,
                                                            in_offset=bass.IndirectOffsetOnAxis(ap=imk[:, el:el + 1], axis=0)),
                      reads=['imk', 'mk_d'], writes=['mkT'])
                S.dma('pool', lambda e: e.indirect_dma_start(out=wgT[:, :], out_offset=None, in_=wgv,
                                                            in_offset=bass.IndirectOffsetOnAxis(ap=imk[:, el:el + 1], axis=0)),
                      reads=['imk', 'wg_d'], writes=['wgT'])
                S.op('pe', lambda e: e.transpose(P[1][:, 0:64], mkT[:, :], C('ident', 64)[:, 0:64]), reads=['mkT', 'cp'], writes=['P1'], acc=True)
                S.op('pe', lambda e: e.transpose(P[2][:, 0:64], wgT[:, :], C('ident', 64)[:, 0:64]), reads=['wgT', 'cp'], writes=['P2'], acc=True)
                S.op('dve', lambda e: e.tensor_copy(out=mk[:], in_=P[1][:, 0:64]), reads=['P1'], writes=['mk'])
                S.op('act', lambda e: e.copy(out=wpc[el][:], in_=P[2][:, 0:64]), reads=['P2'], writes=['wpc%d' % el])
                S.op('dve', lambda e: e.reduce_sum(out=tot[:], in_=mk[:], axis=AX.X), reads=['mk'], writes=['tot'])
                S.op('dve', lambda e: e.tensor_scalar(out=totb[:], in0=C('one128')[:, 0:64], scalar1=tot[:, 0:1], scalar2=None, op0=ALU.mult),
                     reads=['tot', 'cp'], writes=['totb'])
                S.op('pe', lambda e: e.matmul(P[3][:, 0:64], lhsT=mkT[:, :], rhs=C('SU64'), start=True, stop=False), reads=['mkT', 'cp'], writes=['P3'], acc=True)
                S.op('pe', lambda e: e.matmul(P[3][:, 0:64], lhsT=C('L128'), rhs=totb[:], start=False, stop=True), reads=['totb', 'cp'], writes=['P3'], acc=True)
                S.op('dve', lambda e: e.tensor_copy(out=slf[:], in_=P[3][:, 0:64]), reads=['P3'], writes=['slf'])
                S.op('dve', lambda e: e.tensor_tensor(out=sl2[:], in0=slf[:], in1=mk[:], op=ALU.mult), reads=['slf', 'mk'], writes=['sl2'])
                S.op('dve', lambda e: e.tensor_scalar(out=sl2[:], in0=sl2[:], scalar1=float(CAP - 1), scalar2=None, op0=ALU.min), reads=['sl2'], writes=['sl2'])
                S.op('dve', lambda e: e.tensor_copy(out=gidx[el][:], in_=sl2[:]), reads=['sl2'], writes=['gi%d' % el])
                S.op('dve', lambda e: e.tensor_scalar(out=sl2[:], in0=slf[:], scalar1=-5000.0, scalar2=None, op0=ALU.add), reads=['slf'], writes=['sl2'])
                S.op('dve', lambda e: e.tensor_tensor(out=sl2[:], in0=sl2[:], in1=mk[:], op=ALU.mult), reads=['sl2', 'mk'], writes=['sl2'])
                S.op('dve', lambda e: e.tensor_scalar(out=sl2[:], in0=sl2[:], scalar1=5000.0, scalar2=None, op0=ALU.add), reads=['sl2'], writes=['sl2'])
                S.op('dve', lambda e: e.tensor_copy(out=si[el][:], in_=sl2[:]), reads=['sl2'], writes=['si%d' % el])
            if dbg and 'slots' in dbg:
                for el in range(4):
                    S.op('dve', lambda e: e.tensor_copy(out=junk4[:, el * 64:(el + 1) * 64], in_=si[el][:]), reads=['si%d' % el], writes=['junk4'])
                    S.op('dve', lambda e: e.tensor_copy(out=junk4[:, 256 + el * 64:256 + (el + 1) * 64], in_=wpc[el][:]), reads=['wpc%d' % el], writes=['junk4'])
                S.dma('sp', lambda e: e.dma_start(out=dbg_out['slots'], in_=junk4[:, 0:512]), reads=['junk4'])
            S.barrier()

        if stop >= 7:
          with ExitStack() as ph:
            xsT = sb(ph, "xsT", [128, 8, CAP], BF16)
            hid = sb(ph, "hid", [128, NFC, CAP], BF16)
            wgf = [sb(ph, "wgf%d" % i, [128, 8, 256]) for i in range(2)]
            wuf = [sb(ph, "wuf%d" % i, [128, 8, 256]) for i in range(2)]
            wgb = [sb(ph, "wgb%d" % i, [128, 8, 256], BF16) for i in range(2)]
            wub = [sb(ph, "wub%d" % i, [128, 8, 256], BF16) for i in range(2)]
            wdf = [sb(ph, "wdf%d" % i, [128, D]) for i in range(3)]
            wdb = [sb(ph, "wdb%d" % i, [128, D], BF16) for i in range(3)]
            xr = [sb(ph, "xr%d" % i, [128, D], BF16) for i in range(2)]
            yt = [sb(ph, "yt%d" % i, [128, D]) for i in range(2)]
            tmp6 = [sb(ph, "tmp6%d" % i, [128, 512]) for i in range(2)]
            identb = sb(ph, "identb", [128, 128], BF16)
            S.op('dve', lambda e: e.tensor_copy(out=identb[:], in_=ident), reads=['cp'], writes=['identb'])
            nexp = 4 if not (dbg and dbg.get('_short')) else 1
            hb = [sb(ph, "hb%d" % i, [128, D], BF16) for i in range(4)]
            breg = nc.gpsimd.to_reg(CAP - 1)
            hcnt = [0]

            def dispatch(el):
                def load(blk):
                    i_ = (hcnt[0] + blk) % 4
                    S.dma('pool', lambda e: e.dma_start(out=hb[i_][:], in_=h2all_d[blk * 128:(blk + 1) * 128, :]),
                          reads=['h2all%d' % (blk // 16)], writes=['hb%d' % i_])
                load(0)
                load(1)
                for blk in range(64):
                    if blk + 2 < 64:
                        load(blk + 2)
                    i_ = (hcnt[0] + blk) % 4
                    tb = ((blk % 16) // 4) * 16 + (blk // 16) * 4 + (blk % 4)
                    S.dma('pool', lambda e: e.indirect_dma_start(out=xs_d[el], out_offset=bass.IndirectOffsetOnAxis(ap=si[el][:, tb:tb + 1], axis=0),
                                                                in_=hb[i_][:, :], in_offset=None, bounds_check=breg, oob_is_err=False),
                          reads=['hb%d' % i_, 'si%d' % el])
                hcnt[0] += 64
                S.fence('xsd%d' % el, 'pool')

            dispatch(0)
            wcnt = 0
            dcnt = 0
            tcnt = 0
            xcnt = 0
            for el in range(nexp):
                if el + 1 < nexp:
                    dispatch(el + 1)
                wgv_ = weg[el].rearrange("(k p) f -> p k f", p=128)
                wuv_ = weu[el].rearrange("(k p) f -> p k f", p=128)
                for st in range(8):
                    r0 = st * 128
                    xb, xk = xr[xcnt % 2], 'xr%d' % (xcnt % 2)
                    xcnt += 1
                    S.dma('sp', lambda e: e.dma_start(out=xb[:], in_=xs_d[el][r0:r0 + 128, :]), reads=['xsd%d' % el], writes=[xk])
                    for h2_ in range(2):
                        pb, pk = P[4 + h2_][:, :].bitcast(BF16), PK[4 + h2_]
                        for kk in range(4):
                            k = h2_ * 4 + kk
                            S.op('pe', lambda e: e.transpose(pb[:, kk * 128:(kk + 1) * 128], xb[:, k * 128:(k + 1) * 128], identb[:]),
                                 reads=[xk, 'identb'], writes=[pk], acc=True)
                        dst = xsT[:, h2_ * 4:(h2_ + 1) * 4, st * 128:(st + 1) * 128]
                        src = pb[:, 0:512].rearrange("p (a b) -> p a b", b=128)
                        if h2_ == 0:
                            S.op('act', lambda e: e.copy(out=dst, in_=src), reads=[pk], writes=['xsT0'])
                        else:
                            S.op('dve', lambda e: e.tensor_copy(out=dst, in_=src), reads=[pk], writes=['xsT1'])
                for fg in range(11):
                    wi = wcnt % 2
                    wcnt += 1
                    S.dma('sp', lambda e: e.dma_start(out=wgf[wi][:], in_=wgv_[:, :, fg * 256:(fg + 1) * 256]), writes=['wgf%d' % wi])
                    S.dma('sp', lambda e: e.dma_start(out=wuf[wi][:], in_=wuv_[:, :, fg * 256:(fg + 1) * 256]), writes=['wuf%d' % wi])
                    S.op('act', lambda e: e.copy(out=wgb[wi][:], in_=wgf[wi][:]), reads=['wgf%d' % wi], writes=['wgb%d' % wi])
                    S.op('dve', lambda e: e.tensor_copy(out=wub[wi][:], in_=wuf[wi][:]), reads=['wuf%d' % wi], writes=['wub%d' % wi])
                    for fc in range(2):
                        for half in range(2):
                            pg, pgk = P[half], PK[half]
                            pu, puk = P[2 + half], PK[2 + half]
                            hs = slice(half * 512, (half + 1) * 512)
                            for k in range(8):
                                S.op('pe', lambda e: e.matmul(pg[:, :], lhsT=wgb[wi][:, k, fc * 128:(fc + 1) * 128], rhs=xsT[:, k, hs],
                                                              start=(k == 0), stop=(k == 7)),
                                     reads=['wgb%d' % wi, 'xsT%d' % (k // 4)], writes=[pgk], acc=True)
                            for k in range(8):
                                S.op('pe', lambda e: e.matmul(pu[:, :], lhsT=wub[wi][:, k, fc * 128:(fc + 1) * 128], rhs=xsT[:, k, hs],
                                                              start=(k == 0), stop=(k == 7)),
                                     reads=['wub%d' % wi, 'xsT%d' % (k // 4)], writes=[puk], acc=True)
                            ti = tcnt % 2
                            tcnt += 1
                            S.op('act', lambda e: e.activation(out=tmp6[ti][:], in_=pg[:, :], func=AF.Silu), reads=[pgk], writes=['tmp6%d' % ti])
                            S.op('dve', lambda e: e.tensor_tensor(out=hid[:, fg * 2 + fc, hs], in0=tmp6[ti][:], in1=pu[:, :], op=ALU.mult),
                                 reads=['tmp6%d' % ti, puk], writes=['hid'])
                for tg in range(2):
                    for fch in range(NFC):
                        di = dcnt % 3
                        dcnt += 1
                        S.dma('sp', lambda e: e.dma_start(out=wdf[di][:], in_=wed[el][fch * 128:(fch + 1) * 128, :]), writes=['wdf%d' % di])
                        if fch % 2 == 0:
                            S.op('act', lambda e: e.copy(out=wdb[di][:], in_=wdf[di][:]), reads=['wdf%d' % di], writes=['wdb%d' % di])
                        else:
                            S.op('dve', lambda e: e.tensor_copy(out=wdb[di][:], in_=wdf[di][:]), reads=['wdf%d' % di], writes=['wdb%d' % di])
                        for st in range(4):
                            for dh in range(2):
                                S.op('pe', lambda e: e.matmul(P[st * 2 + dh][:, :], lhsT=hid[:, fch, (tg * 4 + st) * 128:(tg * 4 + st + 1) * 128],
                                                              rhs=wdb[di][:, dh * 512:(dh + 1) * 512], start=(fch == 0), stop=(fch == NFC - 1)),
                                     reads=['hid', 'wdb%d' % di], writes=[PK[st * 2 + dh]], acc=True)
                    for st in range(4):
                        yb, yk = yt[st % 2], 'yt%d' % (st % 2)
                        S.op('act', lambda e: e.copy(out=yb[:, 0:512], in_=P[st * 2][:, :]), reads=[PK[st * 2]], writes=[yk])
                        S.op('dve', lambda e: e.tensor_copy(out=yb[:, 512:1024], in_=P[st * 2 + 1][:, :]), reads=[PK[st * 2 + 1]], writes=[yk])
                        r0 = (tg * 4 + st) * 128
                        S.dma('act', lambda e: e.dma_start(out=ye_d[el][r0:r0 + 128, :], in_=yb[:]), reads=[yk])
            S.barrier()
        dbgdump('ye0', ye_d[0])

        if stop >= 8:
          with ExitStack() as ph:
            gt_ = [sb(ph, "gt%d" % i, [128, D]) for i in range(4)]
            acc = [sb(ph, "acc%d" % i, [128, D]) for i in range(2)]
            accb = [sb(ph, "accb%d" % i, [128, D], BF16) for i in range(2)]
            gc = 0
            breg7 = nc.gpsimd.to_reg(CAP - 1)
            for i in range(4):
                S.op('pool', lambda e: e.memset(gt_[i][:], 0.0), writes=['gt%d' % i])
            for blk in range(64):
                ab, ak = acc[blk % 2], 'acc%d' % (blk % 2)
                tb7 = ((blk % 32) // 8) * 16 + (blk // 32) * 8 + (blk % 8)
                for el in range(4):
                    g_, gk = gt_[gc % 4], 'gt%d' % (gc % 4)
                    gc += 1
                    S.dma('pool', lambda e: e.indirect_dma_start(out=g_[:, :], out_offset=None, in_=ye_d[el],
                                                                in_offset=bass.IndirectOffsetOnAxis(ap=si[el][:, tb7:tb7 + 1], axis=0),
                                                                bounds_check=breg7, oob_is_err=False),
                          reads=['si%d' % el], writes=[gk])
                    if el == 0:
                        S.op('dve', lambda e: e.tensor_scalar(out=ab[:], in0=g_[:], scalar1=wpc[el][:, tb7:tb7 + 1], scalar2=None, op0=ALU.mult),
                             reads=[gk, 'wpc%d' % el], writes=[ak])
                    elif el < 3:
                        S.op('dve', lambda e: e.scalar_tensor_tensor(out=ab[:], in0=g_[:], scalar=wpc[el][:, tb7:tb7 + 1], in1=ab[:], op0=ALU.mult, op1=ALU.add),
                             reads=[gk, 'wpc%d' % el, ak], writes=[ak])
                    else:
                        S.op('dve', lambda e: e.scalar_tensor_tensor(out=accb[blk % 2][:], in0=g_[:], scalar=wpc[el][:, tb7:tb7 + 1], in1=ab[:],
                                                                     op0=ALU.mult, op1=ALU.add),
                             reads=[gk, 'wpc%d' % el, ak], writes=['accb%d' % (blk % 2)])
                S.dma('sp', lambda e: e.dma_start(out=op_d[blk * 128:(blk + 1) * 128, :], in_=accb[blk % 2][:]), reads=['accb%d' % (blk % 2)],
                      writes=['op_b%d' % blk])
                if blk % 32 == 31:
                    c_ = blk // 32
                    S.collective("ReduceScatter", ALU.add, [op_d[c_ * 4096:(c_ + 1) * 4096, :]], [moe_d[c_ * 1024:(c_ + 1) * 1024, :]],
                                 reads=['op_b%d' % bb for bb in range(blk - 31, blk + 1)], writes=['moe%d' % c_])
            S.barrier()
        dbgdump('moe', moe_d)

        if stop >= 9:
          with ExitStack() as ph:
            a8 = [sb(ph, "a8%d" % i, [128, D]) for i in range(2)]
            m8 = [sb(ph, "m8%d" % i, [128, D], BF16) for i in range(2)]
            m8f = [sb(ph, "m8f%d" % i, [128, D]) for i in range(2)]
            o8 = [sb(ph, "o8%d" % i, [128, D]) for i in range(2)]
            junk8 = sb(ph, "junk8", [128, D])
            st8 = sb(ph, "st8", [128, 8])
            for i in range(16):
                ts_ = slice(i * 128, (i + 1) * 128)
                a_, ak = a8[i % 2], 'a8%d' % (i % 2)
                m_, mk_ = m8[i % 2], 'm8%d' % (i % 2)
                o_, ok_ = o8[i % 2], 'o8%d' % (i % 2)
                S.dma('sp', lambda e: e.dma_start(out=a_[:], in_=x1_d[ts_, :]), writes=[ak])
                S.dma('act', lambda e: e.dma_start(out=m_[:], in_=moe_d[ts_, :]), reads=['moe%d' % (i // 8)], writes=[mk_])
                mf_, mfk = m8f[i % 2], 'm8f%d' % (i % 2)
                S.op('dve', lambda e: e.tensor_tensor(out=mf_[:], in0=m_[:], in1=rows[:, 3072:4096], op=ALU.mult), reads=[mk_, 'rows'], writes=[mfk])
                S.op('dve', lambda e: e.tensor_tensor(out=a_[:], in0=a_[:], in1=mf_[:], op=ALU.add), reads=[ak, mfk], writes=[ak])
                S.op('act', lambda e: e.activation(out=junk8[:], in_=a_[:], func=AF.Square, accum_out=st8[:, 0:1]), reads=[ak], writes=['junk8', 'st8'])
                S.op('act', lambda e: e.activation(out=st8[:, 1:2], in_=st8[:, 0:1], func=AF.Ln, scale=1.0 / D, bias=EPS), reads=['st8'], writes=['st8'])
                S.op('act', lambda e: e.activation(out=st8[:, 2:3], in_=st8[:, 1:2], func=AF.Exp, scale=-0.5), reads=['st8'], writes=['st8'])
                S.op('dve', lambda e: e.scalar_tensor_tensor(out=o_[:], in0=a_[:], scalar=st8[:, 2:3], in1=PP('gfin_row'), op0=ALU.mult, op1=ALU.mult),
                     reads=[ak, 'st8', 'pp'], writes=[ok_])
                S.dma('sp', lambda e: e.dma_start(out=out_d[ts_, :], in_=o_[:]), reads=[ok_], writes=['out'])
        S.barrier(cc=True)
    return nc


def _make_inputs(x, c, ctx, c_ctx, w_ada, b_ada, g_mix, w_in, w_fourier, lb_logits, g_rec, w_out,
                 g_ffn, w_router, w_exp_gate, w_exp_up, w_exp_down, g_final):
    f = lambda a: np.ascontiguousarray(np.asarray(a, dtype=np.float32))
    x, c, ctx, c_ctx = f(x), f(c), f(ctx), f(c_ctx)
    w_ada, b_ada, g_mix, w_in = f(w_ada)[0], f(b_ada)[0], f(g_mix)[0], f(w_in)[0]
    w_fourier, lb_logits, g_rec, w_out = f(w_fourier)[0], f(lb_logits), f(g_rec)[0], f(w_out)[0]
    g_ffn, w_router = f(g_ffn)[0], f(w_router)[0]
    weg, weu, wed, g_final = f(w_exp_gate)[0], f(w_exp_up)[0], f(w_exp_down)[0], f(g_final)
    perm = np.concatenate([np.concatenate([np.arange(r * 128, (r + 1) * 128), 512 + np.arange(r * 128, (r + 1) * 128)]) for r in range(4)])
    w_out_p = np.ascontiguousarray(w_out[perm, :])
    in_maps = []
    for core in range(8):
        b, j = core // 4, core % 4
        cols = np.concatenate([np.arange(j * 128, (j + 1) * 128)] + [512 + i * 512 + np.arange(j * 128, (j + 1) * 128) for i in range(5)])
        pk = np.zeros((128, PW), np.float32)

        def put(name, arr):
            o, w = PLAY[name]
            pk[:, o:o + w] = arr.reshape(128, w)
        cc = np.stack([c[b], c_ctx], 0)
        put('ccT', cc.reshape(2, 8, 128).transpose(2, 1, 0))
        put('bada_fm', b_ada[:2048].reshape(16, 128).T)
        put('gmix_fm', g_mix.reshape(8, 128).T)
        put('grec_fm', g_rec[j * 128:(j + 1) * 128].reshape(128, 1))
        lbl = lb_logits[:, :, j * 128:(j + 1) * 128]
        put('lbl_fm', lbl.reshape(4, 128).T)
        put('lbl_row', np.broadcast_to(lbl.reshape(1, 512), (128, 512)))
        put('wr', w_router.reshape(8, 128, 16).transpose(1, 0, 2))
        put('gffn_row', np.broadcast_to(g_ffn.reshape(1, 1024), (128, 1024)))
        put('gfin_row', np.broadcast_to(g_final.reshape(1, 1024), (128, 1024)))
        feat = np.arange(1024)
        r_, fh_, p_ = feat // 256, (feat // 128) % 2, feat % 128
        rowidx = (fh_ * 2 + j // 2) * 1024 + r_ * 256 + (j % 2) * 128 + p_
        idx_mix = np.ascontiguousarray(rowidx.reshape(8, 128).T.astype(np.int32))
        idx_mk = np.zeros((64, 4), np.int32)
        for el in range(4):
            e_ = 4 * j + el
            cidx = np.arange(64)
            rr, hh, cc_ = cidx // 16, (cidx // 8) % 2, cidx % 8
            idx_mk[:, el] = (rr * 32 + e_ * 2 + hh) * 8 + cc_
        in_maps.append({
            "x_b": x[b], "x_own": np.ascontiguousarray(x[b, j * 2048:(j + 1) * 2048]), "ctx_b": ctx[b],
            "w_ada": w_ada, "w_in": np.ascontiguousarray(w_in[:, cols]), "w_f": w_fourier[j],
            "w_out": w_out_p, "weg": np.ascontiguousarray(weg[4 * j:4 * j + 4]), "weu": np.ascontiguousarray(weu[4 * j:4 * j + 4]),
            "wed": np.ascontiguousarray(wed[4 * j:4 * j + 4]), "cpack": CPACK, "ppack": pk,
            "bada_rows": np.ascontiguousarray(np.broadcast_to(b_ada[2048:].reshape(1, 4096), (128, 4096))),
            "idx_mix": idx_mix, "idx_mk": idx_mk,
        })
    return in_maps


def kernel(**inputs):
    in_maps = _make_inputs(**inputs)
    nc = build()
    res = run_bass_kernel_spmd(nc, in_maps, core_ids=list(range(8)))
    out = np.zeros((2, L, D), np.float32)
    for core in range(8):
        b, j = core // 4, core % 4
        out[b, j * 2048:(j + 1) * 2048] = res.results[core]["out"]
    return out
```
